# Optimizing a Trainium2 kernel written in Bass

```python
import jax, jax.numpy as jnp
from jax import lax
import numpy as np

D_MODEL = 1024
BATCH = 4
SEQ = 8192
DEPTH = 1

N_META = 16
D_MIX = D_MODEL
D_SC = D_MIX // 2
D_CF = D_MIX - D_SC
N_GROUPS_SC = 8
N_GROUPS_CF = 8
SC_WIDTH = 3
CF_WIDTH = 31
D_IN = 3 * D_SC + 2 * D_CF
PEER_HEADS = 8
PEER_N_KEYS = 128
PEER_N_EXPERTS = PEER_N_KEYS * PEER_N_KEYS
PEER_D_KEY = 256
PEER_D_HALF = PEER_D_KEY // 2
PEER_TOPK = 16
PEER_CHUNK = 256
EPS = 1e-6

kernel_name = "hymba_conv_peer_block"


def rms_norm(x, g):
    xf = x.astype(jnp.float32)
    y = xf * lax.rsqrt(jnp.mean(xf * xf, axis=-1, keepdims=True) + EPS)
    return (y * g.astype(jnp.float32)).astype(x.dtype)


def layer_norm(x, g, b):
    xf = x.astype(jnp.float32)
    mu = jnp.mean(xf, axis=-1, keepdims=True)
    var = jnp.mean(jnp.square(xf - mu), axis=-1, keepdims=True)
    y = (xf - mu) * lax.rsqrt(var + EPS)
    return (y * g.astype(jnp.float32) + b.astype(jnp.float32)).astype(x.dtype)


def causal_depthwise_conv(x, w):
    k, c = w.shape
    return lax.conv_general_dilated(
        x, w[:, None, :].astype(x.dtype), window_strides=(1,), padding=[(k - 1, 0)],
        dimension_numbers=("NWC", "WIO", "NWC"), feature_group_count=c)


def short_conv_mixer(xv, bg, cg, w_conv):
    return bg * causal_depthwise_conv(cg * xv, w_conv)


def conformer_conv_mixer(a, gate, w_dw, b_dw, ln_g, ln_b):
    u = a * jax.nn.sigmoid(gate)
    u = causal_depthwise_conv(u, w_dw) + b_dw
    return jax.nn.silu(layer_norm(u, ln_g, ln_b))


def peer_route(q, sub_keys):
    s1 = jnp.einsum("thd,hnd->thn", q[..., :PEER_D_HALF], sub_keys[:, 0])
    s2 = jnp.einsum("thd,hnd->thn", q[..., PEER_D_HALF:], sub_keys[:, 1])
    v1, i1 = lax.top_k(s1, PEER_TOPK)
    v2, i2 = lax.top_k(s2, PEER_TOPK)
    cand = (v1[..., :, None] + v2[..., None, :]).reshape(q.shape[:-1] + (PEER_TOPK * PEER_TOPK,))
    vals, pos = lax.top_k(cand, PEER_TOPK)
    e1 = jnp.take_along_axis(i1, pos // PEER_TOPK, axis=-1)
    e2 = jnp.take_along_axis(i2, pos % PEER_TOPK, axis=-1)
    idx = e1 * PEER_N_KEYS + e2
    gates = jax.nn.softmax(vals.astype(jnp.float32), axis=-1).astype(q.dtype)
    return idx, gates


def peer_experts(xc, idx, gates, u, v):
    u_sel = jnp.take(u, idx, axis=0)
    act = jax.nn.gelu(jnp.einsum("chkd,cd->chk", u_sel, xc))
    v_sel = jnp.take(v, idx, axis=0)
    return jnp.einsum("chk,chkd->cd", act * gates, v_sel)


def peer_layer(x, w_q, sub_keys, u, v):
    b, t, d = x.shape
    n_tok = b * t
    pad = (-n_tok) % PEER_CHUNK
    xf = jnp.pad(x.reshape(n_tok, d), ((0, pad), (0, 0)))
    q = (xf @ w_q).reshape(n_tok + pad, PEER_HEADS, PEER_D_KEY)
    idx, gates = peer_route(q, sub_keys)
    n_chunks = (n_tok + pad) // PEER_CHUNK
    xc = xf.reshape(n_chunks, PEER_CHUNK, d)
    idxc = idx.reshape(n_chunks, PEER_CHUNK, PEER_HEADS, PEER_TOPK)
    gc = gates.reshape(n_chunks, PEER_CHUNK, PEER_HEADS, PEER_TOPK)
    out = lax.map(lambda args: peer_experts(args[0], args[1], args[2], u, v), (xc, idxc, gc))
    return out.reshape(n_tok + pad, d)[:n_tok].reshape(b, t, d)


def setup_inputs(seed: int = 0) -> dict:
    key = jax.random.key(seed)
    ks = jax.random.split(key, 20)
    f32 = jnp.float32
    nrm = lambda k, shape, s: (jax.random.normal(k, shape, f32) * s)
    gain = lambda k, shape: 1.0 + 0.02 * jax.random.normal(k, shape, f32)
    return {
        "x": nrm(ks[0], (BATCH, SEQ, D_MODEL), 1.0),
        "meta_tokens": nrm(ks[1], (N_META, D_MODEL), 1.0),
        "norm_mix_g": gain(ks[2], (DEPTH, D_MODEL)),
        "w_in": nrm(ks[3], (DEPTH, D_MODEL, D_IN), D_MODEL ** -0.5),
        "sc_conv_w": nrm(ks[4], (DEPTH, SC_WIDTH, D_SC), SC_WIDTH ** -0.5),
        "cf_conv_w": nrm(ks[5], (DEPTH, CF_WIDTH, D_CF), CF_WIDTH ** -0.5),
        "cf_conv_b": nrm(ks[6], (DEPTH, D_CF), 0.02),
        "cf_ln_g": gain(ks[7], (DEPTH, D_CF)),
        "cf_ln_b": nrm(ks[8], (DEPTH, D_CF), 0.02),
        "out_norm_g_sc": gain(ks[9], (DEPTH, D_SC)),
        "out_norm_g_cf": gain(ks[10], (DEPTH, D_CF)),
        "w_out": nrm(ks[11], (DEPTH, D_MIX, D_MODEL), D_MIX ** -0.5),
        "norm_ffn_g": gain(ks[12], (DEPTH, D_MODEL)),
        "peer_w_q": nrm(ks[13], (DEPTH, D_MODEL, PEER_HEADS * PEER_D_KEY), D_MODEL ** -0.5),
        "peer_sub_keys": nrm(ks[14], (DEPTH, PEER_HEADS, 2, PEER_N_KEYS, PEER_D_HALF), PEER_D_HALF ** -0.5),
        "peer_u": nrm(ks[15], (DEPTH, PEER_N_EXPERTS, D_MODEL), D_MODEL ** -0.5),
        "peer_v": nrm(ks[16], (DEPTH, PEER_N_EXPERTS, D_MODEL), PEER_HEADS ** -0.5),
        "final_norm_g": gain(ks[17], (D_MODEL,)),
    }


def reference(x, meta_tokens, norm_mix_g, w_in, sc_conv_w, cf_conv_w, cf_conv_b, cf_ln_g, cf_ln_b,
              out_norm_g_sc, out_norm_g_cf, w_out, norm_ffn_g, peer_w_q, peer_sub_keys, peer_u, peer_v,
              final_norm_g):
    b = x.shape[0]
    meta = jnp.broadcast_to(meta_tokens[None].astype(x.dtype), (b, N_META, x.shape[-1]))
    h = jnp.concatenate([meta, x], axis=1)
    splits = [D_SC, 2 * D_SC, 3 * D_SC, 3 * D_SC + D_CF]
    for l in range(DEPTH):
        hn = rms_norm(h, norm_mix_g[l])
        proj = hn @ w_in[l]
        xv, bg, cg, ga, gg = jnp.split(proj, splits, axis=-1)
        y_sc = short_conv_mixer(xv, bg, cg, sc_conv_w[l])
        y_cf = conformer_conv_mixer(ga, gg, cf_conv_w[l], cf_conv_b[l], cf_ln_g[l], cf_ln_b[l])
        y = jnp.concatenate([rms_norm(y_sc, out_norm_g_sc[l]), rms_norm(y_cf, out_norm_g_cf[l])], axis=-1)
        h = h + y @ w_out[l]
        hn = rms_norm(h, norm_ffn_g[l])
        h = h + peer_layer(hn, peer_w_q[l], peer_sub_keys[l], peer_u[l], peer_v[l])
    h = rms_norm(h, final_norm_g)
    return h[:, N_META:]
```

```python
import numpy as np
from contextlib import ExitStack
import concourse.bass as bass
import concourse.mybir as mybir
from concourse.bass_utils import run_bass_kernel_spmd

F32 = mybir.dt.float32
BF16 = mybir.dt.bfloat16
U32 = mybir.dt.uint32
ALU = mybir.AluOpType
AF = mybir.ActivationFunctionType
AX = mybir.AxisListType

D = 1024
G = 256
HALO = 32
N_CORES = 8
EPS = 1e-6
NS = 9
JB = 4
GM, GF, GO, SCW, CFW, CFB, LNG, LNB, NVEC = 0, 8, 16, 24, 36, 160, 164, 168, 172


class Prog:
    ENGS = ("sync", "scalar", "vector", "gpsimd", "tensor")

    def __init__(self, nc, stack):
        self.nc = nc
        self.stack = stack
        self.q = {e: [] for e in self.ENGS}
        self.cnt = {e: 0 for e in self.ENGS}
        self.sem = {e: stack.enter_context(nc.semaphore("c_" + e)) for e in self.ENGS}
        self.seen = {e: {} for e in self.ENGS}
        self.last_w = {}
        self.readers = {}
        self.dsem = {}
        self.dcnt = {}
        self.alias = {}
        self.defer = None

    def _exp(self, names):
        out = []
        for n in names:
            out.extend(self.alias.get(n, (n,)))
        return out

    def _deps(self, e, reads, writes):
        deps = {}

        def add(ev):
            if ev is None:
                return
            k, s, v = ev
            if k not in deps or deps[k][1] < v:
                deps[k] = (s, v)

        for r in reads:
            add(self.last_w.get(r))
        for w in writes:
            add(self.last_w.get(w))
            for ev in self.readers.get(w, ()):
                add(ev)
        for k, (s, v) in deps.items():
            if e == "tensor" and k == "e_tensor":
                continue
            if self.seen[e].get(k, 0) < v:
                self.seen[e][k] = v
                self.q[e].append(lambda eng, s=s, v=v: eng.wait_ge(s, v))

    def _commit(self, ev, reads, writes):
        for w in writes:
            self.last_w[w] = ev
            self.readers[w] = []
        for r in reads:
            if r not in writes:
                self.readers.setdefault(r, []).append(ev)

    def op(self, e, fn, reads=(), writes=()):
        if self.defer is not None:
            self.defer.append(lambda: self._op(e, fn, reads, writes))
            return
        self._op(e, fn, reads, writes)

    def _op(self, e, fn, reads=(), writes=()):
        reads, writes = self._exp(reads), self._exp(writes)
        self._deps(e, reads, writes)
        self.cnt[e] += 1
        n = self.cnt[e]
        s = self.sem[e]
        self.q[e].append(lambda eng, fn=fn, s=s: fn(eng).then_inc(s, 1))
        self._commit(("e_" + e, s, n), reads, writes)

    def dma(self, e, fn, key, reads=(), writes=()):
        if self.defer is not None:
            self.defer.append(lambda: self._dma(e, fn, key, reads, writes))
            return
        self._dma(e, fn, key, reads, writes)

    def _dma(self, e, fn, key, reads=(), writes=()):
        reads, writes = self._exp(reads), self._exp(writes)
        self._deps(e, reads, writes)
        if key not in self.dsem:
            self.dsem[key] = self.stack.enter_context(self.nc.semaphore("d_" + key))
            self.dcnt[key] = 0
        s = self.dsem[key]
        self.dcnt[key] += 16
        v = self.dcnt[key]
        self.q[e].append(lambda eng, fn=fn, s=s: fn(eng).then_inc(s, 16))
        self._commit(("d_" + key, s, v), reads, writes)

    def wait_all(self, e, resources):
        self._deps(e, self._exp(resources), ())

    def emit(self):
        with self.nc.Block() as block:
            @block.sync
            def _(eng):
                for f in self.q["sync"]:
                    f(eng)

            @block.scalar
            def _(eng):
                for f in self.q["scalar"]:
                    f(eng)

            @block.vector
            def _(eng):
                for f in self.q["vector"]:
                    f(eng)

            @block.gpsimd
            def _(eng):
                for f in self.q["gpsimd"]:
                    f(eng)

            @block.tensor
            def _(eng):
                for f in self.q["tensor"]:
                    f(eng)


def build(n_groups=16, n_peer_j=128):
    nc = bass.Bass("TRN2", target_bir_lowering=False)
    ntok = HALO + n_groups * G
    dr = lambda name, shape, kind="ExternalInput", dt=F32: nc.dram_tensor(name, shape, dt, kind=kind).ap()
    xh = dr("xh", [ntok, D])
    w_in = dr("w_in", [D, 2560])
    w_out = dr("w_out", [D, D])
    w_q = dr("w_q", [D, 2048])
    keysT = dr("keysT", [128, 2048])
    uv_tab = dr("uv_tab", [16384, 2 * D])
    tab_b = dr("tab_b", [16384, 2 * D], kind="Internal", dt=BF16)
    vecs_d = dr("vecs_h", [128, NVEC])
    gB_d = dr("gB_h", [128, 2048])
    out = dr("out", [n_groups * G, D], kind="ExternalOutput")

    with ExitStack() as st:
        P = Prog(nc, st)
        sb = lambda name, shape, dt=F32: st.enter_context(nc.sbuf_tensor(name, shape, dt))
        Wib = sb("Wib", [128, 8, 2560], BF16)
        Wob = sb("Wob", [128, 8, 1024], BF16)
        Wqb = sb("Wqb", [128, 8, 2048], BF16)
        keysb = sb("keysb", [128, 16, 128], BF16)
        vecs = sb("vecs", [128, NVEC])
        gB = sb("gB", [128, D])
        identf = sb("identf", [128, 128])
        identb = sb("identb", [128, 128], BF16)
        onesf = sb("onesf", [128, 128])
        iota16 = sb("iota16", [128, 16])
        xt = [sb("xt%d" % i, [128, D]) for i in range(4)]
        hnb = sb("hnb", [128, D], BF16)
        ss = sb("ss", [128, 8])
        hnT = sb("hnT", [128, 8, G], BF16)
        cgs = sb("cgs", [128, G])
        zb = [sb("z%d" % k, [128, HALO + G]) for k in range(4)]
        c1 = sb("c1", [128, G])
        mixA = sb("mixA", [128, 2048])
        ysc = [mixA[:, k * G:(k + 1) * G] for k in range(4)]
        sq = [sb("sq%d" % k, [128, G]) for k in range(2)]
        ub = [sb("u%d" % k, [128, HALO + G], BF16) for k in range(4)]
        dgs = [sb("dg%d" % k, [128, 128], BF16) for k in range(6)]
        cv = [mixA[:, 1024 + k * G:1024 + (k + 1) * G] for k in range(4)]
        mt = sb("mt", [128, G])
        rt = sb("rt", [128, G])
        qTb = sb("qTb", [128, 1024], BF16)
        Ssb = sb("Ssb", [128, 1024])
        S2 = sb("S2", [128, 256])
        Vt = sb("Vt", [128, 256])
        It = sb("It", [128, 256], U32)
        cand = mixA
        CV = sb("CV", [128, 128])
        CP = sb("CP", [128, 128], U32)
        au = sb("au", [128, 128], U32)
        af = sb("af", [128, 128])
        bf = sb("bf", [128, 128])
        idx2 = [sb("idx%d" % i, [128, 128], U32) for i in range(2)]
        ex = sb("ex", [128, 128])
        Zs = sb("Zs", [128, 8])
        gates2 = [sb("gates%d" % i, [128, 128]) for i in range(2)]
        apre = sb("apre", [128, 128])
        diag = [sb("diag%d" % i, [128, 128], BF16) for i in range(4)]
        GS = [sb("GS%d" % i, [128, 2 * D], BF16) for i in range(NS)]
        yn = [sb("yn%d" % k, [128, G], BF16) for k in range(8)]
        psA = st.enter_context(nc.psum_tensor("psA", [128, 6 * 512], F32))
        psT = st.enter_context(nc.psum_tensor("psT", [128, 1024], BF16))
        pslot = lambda s_, n: psA[:, 1024 + s_ * 256:1024 + s_ * 256 + n]
        bank = lambda i, n=512: psA[:, i * 512:i * 512 + n]
        psT3 = psT[:].rearrange("p (c t) -> p c t", c=8)
        prod = [sb("prod%d" % i, [128, D], BF16) for i in range(2)]
        hbf = [sb("hbf%d" % i, [128, D], BF16) for i in range(2)]
        junk = prod[1][:, :]
        JW = ["prod1"]
        JR = []

        vcol = lambda c: vecs[:, c:c + 1]
        P.alias["cand"] = ["ysc%d" % k for k in range(4)] + ["cv%d" % k for k in range(4)]
        P.alias["mixlo"] = ["ysc%d" % k for k in range(4)]
        P.alias["mixhi"] = ["cv%d" % k for k in range(4)]
        P.alias["b2"] = ["p0", "p1"]
        P.alias["b3"] = ["p2", "p3"]

        P.dma("sync", lambda e: e.dma_start(out=vecs[:], in_=vecs_d), "ldv", writes=["vecs"])
        P.dma("sync", lambda e: e.dma_start(out=gB[:], in_=gB_d[:, D:2 * D]), "ldg", writes=["gB"])
        P.dma("sync", lambda e: e.dma_start(out=xt[0][:, :], in_=gB_d[:, 0:D]), "ldx0", writes=["xt0"])
        P.op("gpsimd", lambda e: e.memset(identf[:], 1.0), writes=["identf"])
        P.op("gpsimd", lambda e: e.affine_select(out=identf[:], in_=identf[:], pattern=[[-1, 128]], compare_op=ALU.is_equal,
                                                  fill=0.0, base=0, channel_multiplier=1), reads=["identf"], writes=["identf"])
        P.op("vector", lambda e: e.tensor_copy(out=identb[:], in_=identf[:]), reads=["identf"], writes=["identb"])
        P.op("gpsimd", lambda e: e.memset(onesf[:], 1.0), writes=["onesf"])
        P.op("gpsimd", lambda e: e.iota(iota16[:], pattern=[[1, 16]], base=0, channel_multiplier=0,
                                        allow_small_or_imprecise_dtypes=True), writes=["iota16"])
        stage = [(mixA[:, 0:1024], "mixlo"), (mixA[:, 1024:2048], "mixhi"), (xt[1][:, :], "xt1"), (xt[2][:, :], "xt2")]
        si = 0
        for (Wd, Wb, ncol, gcol, nm) in ((w_in, Wib, 2560, GM, "Wib"), (w_out, Wob, 1024, GO, "Wob"), (w_q, Wqb, 2048, GF, "Wqb")):
            for c in range(8):
                for c0 in range(0, ncol, 1024):
                    w = min(1024, ncol - c0)
                    sbuf_, rn = stage[si % 4]
                    si += 1
                    P.dma("sync" if si % 2 else "scalar",
                          lambda e, sbuf_=sbuf_, Wd=Wd, c=c, c0=c0, w=w: e.dma_start(out=sbuf_[:, 0:w], in_=Wd[c * 128:(c + 1) * 128, c0:c0 + w]),
                          "ld" + rn, writes=[rn])
                    if si % 2:
                        P.op("vector", lambda e, sbuf_=sbuf_, Wb=Wb, c=c, c0=c0, w=w, gcol=gcol: e.tensor_scalar(
                            out=Wb[:, c, c0:c0 + w], in0=sbuf_[:, 0:w], scalar1=vcol(gcol + c), scalar2=None, op0=ALU.mult),
                            reads=[rn, "vecs"], writes=[nm])
                    else:
                        P.op("scalar", lambda e, sbuf_=sbuf_, Wb=Wb, c=c, c0=c0, w=w, gcol=gcol: e.activation(
                            out=Wb[:, c, c0:c0 + w], in_=sbuf_[:, 0:w], func=AF.Copy, scale=vcol(gcol + c)),
                            reads=[rn, "vecs"], writes=[nm])
        for c0 in range(0, 2048, 1024):
            sbuf_, rn = stage[si % 4]
            si += 1
            P.dma("sync", lambda e, sbuf_=sbuf_, c0=c0: e.dma_start(out=sbuf_[:, :], in_=keysT[:, c0:c0 + 1024]), "ld" + rn, writes=[rn])
            P.op("vector", lambda e, sbuf_=sbuf_, c0=c0: e.tensor_copy(out=keysb[:].rearrange("p g n -> p (g n)")[:, c0:c0 + 1024], in_=sbuf_[:, :]),
                 reads=[rn], writes=["keysb"])
        GSres = ["GS%d" % i for i in range(NS)]
        TBres = ["tab_b%d" % i for i in range(NS)]
        ustage = [(mixA[:, 0:1024], "mixlo"), (xt[1][:, :], "xt1"), (xt[3][:, :], "xt3")]
        vstage = [(mixA[:, 1024:2048], "mixhi"), (xt[2][:, :], "xt2")]
        for it in range(128):
            su_, ru = ustage[it % 3]
            sv_, rv = vstage[it % 2]
            gs_ = it % NS
            P.dma("sync", lambda e, su_=su_, it=it: e.dma_start(out=su_[:, :], in_=uv_tab[it * 128:(it + 1) * 128, 0:D]), "ldu" + ru, writes=[ru])
            P.dma("sync", lambda e, sv_=sv_, it=it: e.dma_start(out=sv_[:, :], in_=uv_tab[it * 128:(it + 1) * 128, D:2 * D]), "ldv" + rv, writes=[rv])
            P.op("vector", lambda e, su_=su_, gs_=gs_: e.tensor_tensor(out=GS[gs_][:, 0:D], in0=su_[:, :], in1=xt[0][:, :], op=ALU.mult),
                 reads=[ru, "xt0"], writes=[GSres[gs_] + "u"])
            P.op("scalar", lambda e, sv_=sv_, gs_=gs_: e.activation(out=GS[gs_][:, D:2 * D], in_=sv_[:, :], func=AF.Copy), reads=[rv], writes=[GSres[gs_] + "v"])
            P.dma("scalar", lambda e, gs_=gs_, it=it: e.dma_start(out=tab_b[it * 128:(it + 1) * 128, :], in_=GS[gs_][:, :]),
                  "st" + GSres[gs_], reads=[GSres[gs_] + "u", GSres[gs_] + "v", GSres[gs_]], writes=[TBres[gs_]])

        def rstd_from_ss(ss_ap, n, res, scale):
            P.op("vector", lambda e: e.tensor_scalar(out=ss_ap, in0=ss_ap, scalar1=scale, scalar2=EPS, op0=ALU.mult, op1=ALU.add),
                 reads=[res], writes=[res])
            P.op("scalar", lambda e: e.activation(out=ss_ap, in_=ss_ap, func=AF.Sqrt), reads=[res], writes=[res])
            P.op("vector", lambda e: e.reciprocal(out=ss_ap, in_=ss_ap), reads=[res], writes=[res])

        def bcast_rstd(src_bank, src_res, dst, dst_res, N, scale):
            P.op("vector", lambda e: e.tensor_scalar(out=dst[:, 0:N], in0=src_bank[:, 0:N], scalar1=scale, scalar2=EPS, op0=ALU.mult, op1=ALU.add),
                 reads=[src_res], writes=[dst_res])
            P.op("scalar", lambda e: e.activation(out=dst[:, 0:N], in_=dst[:, 0:N], func=AF.Sqrt), reads=[dst_res], writes=[dst_res])
            P.op("vector", lambda e: e.reciprocal(out=dst[:, 0:N], in_=dst[:, 0:N]), reads=[dst_res], writes=[dst_res])

        def front(tiles, N, xs):
            off = 0
            for i, (r0, n) in enumerate(tiles):
                xr = "xt%d" % xs[i]
                xi = xt[xs[i]]
                P.dma("sync", lambda e, xi=xi, i=i, r0=r0, n=n: e.dma_start(out=xi[0:n, :], in_=xh[r0:r0 + n, :]), "ldx%d" % xs[i], writes=[xr])
                ssr = "ss%d" % i
                P.op("scalar", lambda e, xi=xi, i=i, n=n: e.activation(out=junk[0:n, :], in_=xi[0:n, :], func=AF.Square, accum_out=ss[0:n, i:i + 1]),
                     reads=[xr], writes=[ssr] + JW)
                rstd_from_ss(ss[0:n, i:i + 1], n, ssr, 1.0 / D)
                P.op("scalar", lambda e, xi=xi, i=i, n=n: e.activation(out=hnb[0:n, :], in_=xi[0:n, :], func=AF.Copy, scale=ss[0:n, i:i + 1]),
                     reads=[xr, ssr], writes=["hnb"])
                for c in range(8):
                    P.op("tensor", lambda e, c=c, n=n: e.transpose(out=psT3[:, c, 0:n], in_=hnb[0:n, c * 128:(c + 1) * 128], identity=identb[0:n, 0:n]),
                         reads=["hnb", "identb"], writes=["bT"])
                P.op("vector", lambda e, n=n, off=off: e.tensor_copy(out=hnT[:, :, off:off + n], in_=psT3[:, :, 0:n]), reads=["bT"], writes=["hnT"])
                off += n

        def proj(col, b, N):
            for k in range(8):
                P.op("tensor", lambda e, k=k, col=col, b=b, N=N: e.matmul(pslot(b, N), lhsT=Wib[:, k, col * 128:(col + 1) * 128], rhs=hnT[:, k, 0:N],
                                                                            start=(k == 0), stop=(k == 7)),
                     reads=["Wib", "hnT"], writes=["p%d" % b])

        def mixer_pre():
            N = HALO
            front([(0, HALO)], N, [0])
            for k in range(4):
                proj(k, 0, N)
                proj(8 + k, 1, N)
                P.op("scalar", lambda e: e.activation(out=cgs[:, 0:N], in_=pslot(1, N), func=AF.Copy), reads=["p1"], writes=["cgs"])
                P.op("vector", lambda e, k=k: e.tensor_tensor(out=zb[k][:, 0:N], in0=pslot(0, N), in1=cgs[:, 0:N], op=ALU.mult),
                     reads=["p0", "cgs"], writes=["z%d" % k])
                proj(12 + k, 0, N)
                proj(16 + k, 1, N)
                P.op("scalar", lambda e: e.activation(out=cgs[:, 0:N], in_=pslot(1, N), func=AF.Sigmoid), reads=["p1"], writes=["cgs"])
                P.op("vector", lambda e, k=k: e.tensor_tensor(out=ub[k][:, 0:N], in0=pslot(0, N), in1=cgs[:, 0:N], op=ALU.mult),
                     reads=["p0", "cgs"], writes=["u%d" % k])

        def mixer_group(g):
            N = G
            base = HALO + g * G
            tiles = [(base, 128), (base + 128, 128)]
            xs = [2 * (g % 2), 2 * (g % 2) + 1]
            front(tiles, N, xs)
            for k in range(4):
                proj(k, 0, N)
                proj(8 + k, 1, N)
                proj(4 + k, 2, N)
                zr = "z%d" % k
                P.op("scalar", lambda e: e.activation(out=cgs[:, 0:N], in_=pslot(1, N), func=AF.Copy), reads=["p1"], writes=["cgs"])
                P.op("vector", lambda e, k=k: e.tensor_tensor(out=zb[k][:, HALO:HALO + N], in0=pslot(0, N), in1=cgs[:, 0:N], op=ALU.mult),
                     reads=["p0", "cgs"], writes=[zr])
                P.op("vector", lambda e, k=k: e.tensor_scalar(out=c1[:, 0:N], in0=zb[k][:, HALO - 2:HALO - 2 + N], scalar1=vcol(SCW + 3 * k), scalar2=None, op0=ALU.mult),
                     reads=[zr, "vecs"], writes=["c1"])
                for j in (1, 2):
                    P.op("vector", lambda e, k=k, j=j: e.scalar_tensor_tensor(out=c1[:, 0:N], in0=zb[k][:, HALO - 2 + j:HALO - 2 + j + N], scalar=vcol(SCW + 3 * k + j),
                                                                              in1=c1[:, 0:N], op0=ALU.mult, op1=ALU.add),
                         reads=[zr, "vecs", "c1"], writes=["c1"])
                yr = "ysc%d" % k
                P.op("vector", lambda e, k=k: e.tensor_tensor(out=ysc[k][:, 0:N], in0=pslot(2, N), in1=c1[:, 0:N], op=ALU.mult),
                     reads=["p2", "c1"], writes=[yr])
                sr = "sq%d" % (k % 2)
                P.op("scalar", lambda e, k=k: e.activation(out=sq[k % 2][:, 0:N], in_=ysc[k][:, 0:N], func=AF.Square), reads=[yr], writes=[sr])
                P.op("tensor", lambda e, k=k: e.matmul(bank(4, N), lhsT=onesf[:], rhs=sq[k % 2][:, 0:N], start=(k == 0), stop=(k == 3)),
                     reads=[sr, "onesf"], writes=["b4"])
                P.op("gpsimd", lambda e, k=k: e.tensor_copy(out=zb[k][:, 0:HALO], in_=zb[k][:, G:G + HALO]), reads=[zr], writes=[zr])
            bcast_rstd(bank(4), "b4", mt, "mt", N, 1.0 / 512)
            for k in range(4):
                P.op("vector", lambda e, k=k: e.tensor_tensor(out=yn[k][:, 0:N], in0=ysc[k][:, 0:N], in1=mt[:, 0:N], op=ALU.mult),
                     reads=["ysc%d" % k, "mt"], writes=["yn%d" % k])
            for k in range(4):
                proj(12 + k, 0, N)
                proj(16 + k, 1, N)
                ur = "u%d" % k
                cr = "cv%d" % k
                P.op("scalar", lambda e: e.activation(out=cgs[:, 0:N], in_=pslot(1, N), func=AF.Sigmoid), reads=["p1"], writes=["cgs"])
                P.op("vector", lambda e, k=k: e.tensor_tensor(out=ub[k][:, HALO:HALO + N], in0=pslot(0, N), in1=cgs[:, 0:N], op=ALU.mult),
                     reads=["p0", "cgs"], writes=[ur])
                for j in range(31):
                    dn = (k * 31 + j) % len(dgs)
                    dr_ = "dg%d" % dn
                    if j % 2:
                        P.op("vector", lambda e, k=k, j=j, dn=dn: e.tensor_tensor(out=dgs[dn][:, :], in0=identb[:, :],
                                                                                 in1=vcol(CFW + 31 * k + j).to_broadcast([128, 128]), op=ALU.mult),
                             reads=["identb", "vecs"], writes=[dr_])
                    else:
                        P.op("scalar", lambda e, k=k, j=j, dn=dn: e.activation(out=dgs[dn][:, :], in_=identb[:, :], func=AF.Copy, scale=vcol(CFW + 31 * k + j)),
                             reads=["identb", "vecs"], writes=[dr_])
                    P.op("tensor", lambda e, k=k, j=j, dn=dn: e.matmul(pslot(3, N), lhsT=dgs[dn][:, :], rhs=ub[k][:, 2 + j:2 + j + N], start=(j == 0), stop=(j == 30)),
                         reads=[dr_, ur], writes=["p3"])
                P.op("vector", lambda e, k=k: e.tensor_scalar(out=cv[k][:, 0:N], in0=pslot(3, N), scalar1=vcol(CFB + k), scalar2=None, op0=ALU.add),
                     reads=["p3", "vecs"], writes=[cr])
                sr = "sq%d" % (k % 2)
                P.op("scalar", lambda e, k=k: e.activation(out=sq[k % 2][:, 0:N], in_=cv[k][:, 0:N], func=AF.Square), reads=[cr], writes=[sr])
                P.op("tensor", lambda e, k=k: e.matmul(bank(4, N), lhsT=onesf[:], rhs=cv[k][:, 0:N], start=(k == 0), stop=(k == 3)),
                     reads=[cr, "onesf"], writes=["b4"])
                P.op("tensor", lambda e, k=k: e.matmul(bank(5, N), lhsT=onesf[:], rhs=sq[k % 2][:, 0:N], start=(k == 0), stop=(k == 3)),
                     reads=[sr, "onesf"], writes=["b5"])
                P.op("gpsimd", lambda e, k=k: e.tensor_copy(out=ub[k][:, 0:HALO], in_=ub[k][:, G:G + HALO]), reads=[ur], writes=[ur])
            P.op("vector", lambda e: e.tensor_scalar(out=mt[:, 0:N], in0=bank(4, N), scalar1=1.0 / 512, scalar2=None, op0=ALU.mult), reads=["b4"], writes=["mt"])
            P.op("vector", lambda e: e.tensor_tensor(out=sq[0][:, 0:N], in0=mt[:, 0:N], in1=mt[:, 0:N], op=ALU.mult), reads=["mt"], writes=["sq0"])
            P.op("vector", lambda e: e.scalar_tensor_tensor(out=rt[:, 0:N], in0=bank(5, N), scalar=1.0 / 512, in1=sq[0][:, 0:N], op0=ALU.mult, op1=ALU.subtract),
                 reads=["b5", "sq0"], writes=["rt"])
            P.op("vector", lambda e: e.tensor_scalar(out=rt[:, 0:N], in0=rt[:, 0:N], scalar1=EPS, scalar2=None, op0=ALU.add), reads=["rt"], writes=["rt"])
            P.op("scalar", lambda e: e.activation(out=rt[:, 0:N], in_=rt[:, 0:N], func=AF.Sqrt), reads=["rt"], writes=["rt"])
            P.op("vector", lambda e: e.reciprocal(out=rt[:, 0:N], in_=rt[:, 0:N]), reads=["rt"], writes=["rt"])
            for k in range(4):
                cr = "cv%d" % k
                P.op("vector", lambda e, k=k: e.tensor_tensor(out=cv[k][:, 0:N], in0=cv[k][:, 0:N], in1=mt[:, 0:N], op=ALU.subtract), reads=[cr, "mt"], writes=[cr])
                P.op("vector", lambda e, k=k: e.tensor_tensor(out=cv[k][:, 0:N], in0=cv[k][:, 0:N], in1=rt[:, 0:N], op=ALU.mult), reads=[cr, "rt"], writes=[cr])
                P.op("scalar", lambda e, k=k: e.activation(out=cv[k][:, 0:N], in_=cv[k][:, 0:N], func=AF.Silu, scale=vcol(LNG + k), bias=vcol(LNB + k)),
                     reads=[cr, "vecs"], writes=[cr])
                sr = "sq%d" % (k % 2)
                P.op("scalar", lambda e, k=k: e.activation(out=sq[k % 2][:, 0:N], in_=cv[k][:, 0:N], func=AF.Square), reads=[cr], writes=[sr])
                P.op("tensor", lambda e, k=k: e.matmul(bank(4, N), lhsT=onesf[:], rhs=sq[k % 2][:, 0:N], start=(k == 0), stop=(k == 3)),
                     reads=[sr, "onesf"], writes=["b4"])
            bcast_rstd(bank(4), "b4", mt, "mt", N, 1.0 / 512)
            for k in range(4):
                P.op("vector", lambda e, k=k: e.tensor_tensor(out=yn[4 + k][:, 0:N], in0=cv[k][:, 0:N], in1=mt[:, 0:N], op=ALU.mult),
                     reads=["cv%d" % k, "mt"], writes=["yn%d" % (4 + k)])
            for i in range(2):
                for half in range(2):
                    for kk in range(8):
                        P.op("tensor", lambda e, i=i, half=half, kk=kk: e.matmul(psA[:, (2 + half) * 512:(3 + half) * 512], lhsT=yn[kk][:, i * 128:(i + 1) * 128],
                                                                                  rhs=Wob[:, kk, half * 512:(half + 1) * 512], start=(kk == 0), stop=(kk == 7)),
                             reads=["yn%d" % kk, "Wob"], writes=["b%d" % (2 + half)])
                P.op("vector", lambda e, i=i: e.tensor_tensor(out=xt[xs[i]][:, :], in0=psA[:, 2 * 512:4 * 512], in1=xt[xs[i]][:, :], op=ALU.add),
                     reads=["b2", "b3", "xt%d" % xs[i]], writes=["xt%d" % xs[i]])

        def route(i, x_):
            xr = "xt%d" % x_
            h2 = xt[x_]
            idx = idx2[i]
            gates = gates2[i]
            ssc = ss[:, 2 + i:3 + i]
            ssr2 = "ss2_%d" % i
            idxr = "idx%d" % i
            gatesr = "gates%d" % i
            P.op("scalar", lambda e: e.activation(out=junk[:, :], in_=h2[:, :], func=AF.Square, accum_out=ssc), reads=[xr], writes=[ssr2] + JW)
            rstd_from_ss(ssc, 128, ssr2, 1.0 / D)
            P.op("scalar", lambda e: e.activation(out=hbf[i][:, :], in_=h2[:, :], func=AF.Copy, scale=ssc), reads=[xr, ssr2], writes=["hbf%d" % i])
            for c in range(8):
                P.op("tensor", lambda e, c=c: e.transpose(out=psT3[:, c, :], in_=hbf[i][:, c * 128:(c + 1) * 128], identity=identb[:, :]),
                     reads=["hbf%d" % i, "identb"], writes=["bT"])
            P.op("vector", lambda e: e.tensor_copy(out=hnT[:, :, 0:128], in_=psT3[:, :, :]), reads=["bT"], writes=["hnT"])
            for hf in range(2):
                for gg in range(8):
                    g = hf * 8 + gg
                    for k in range(8):
                        P.op("tensor", lambda e, g=g, gg=gg, k=k: e.matmul(psA[:, 2048 + gg * 128:2048 + (gg + 1) * 128], lhsT=Wqb[:, k, g * 128:(g + 1) * 128],
                                                                        rhs=hnT[:, k, 0:128], start=(k == 0), stop=(k == 7)),
                             reads=["Wqb", "hnT"], writes=["b%d" % (4 + gg // 4)])
                P.op("scalar", lambda e, hf=hf: e.activation(out=qTb[:, :], in_=psA[:, 2048:3072], func=AF.Copy), reads=["b4", "b5"], writes=["qTb"])
                for gg in range(8):
                    g = hf * 8 + gg
                    P.op("tensor", lambda e, g=g, gg=gg: e.matmul(psA[:, 2048 + gg * 128:2048 + (gg + 1) * 128], lhsT=qTb[:, gg * 128:(gg + 1) * 128], rhs=keysb[:, g, :],
                                                               start=True, stop=True),
                         reads=["qTb", "keysb"], writes=["b%d" % (4 + gg // 4)])
                P.op("scalar", lambda e: e.activation(out=Ssb[:, :], in_=psA[:, 2048:3072], func=AF.Copy), reads=["b4", "b5"], writes=["Ssb"])
                for gg in range(8):
                    g = hf * 8 + gg
                    sg_ = Ssb[:, gg * 128:(gg + 1) * 128]
                    v0 = Vt[:, g * 16:g * 16 + 8]
                    v1 = Vt[:, g * 16 + 8:g * 16 + 16]
                    i0 = It[:, g * 16:g * 16 + 8]
                    i1 = It[:, g * 16 + 8:g * 16 + 16]
                    P.op("vector", lambda e, sg_=sg_, v0=v0: e.max(out=v0, in_=sg_), reads=["Ssb"], writes=["Vt"])
                    P.op("vector", lambda e, sg_=sg_, v0=v0, i0=i0: e.max_index(out=i0, in_max=v0, in_values=sg_), reads=["Ssb", "Vt"], writes=["It"])
                    P.op("vector", lambda e, sg_=sg_, v0=v0: e.match_replace(out=S2[:, 0:128], in_to_replace=v0, in_values=sg_, imm_value=-1e30),
                         reads=["Ssb", "Vt"], writes=["S2"])
                    P.op("vector", lambda e, v1=v1: e.max(out=v1, in_=S2[:, 0:128]), reads=["S2"], writes=["Vt"])
                    P.op("vector", lambda e, v1=v1, i1=i1: e.max_index(out=i1, in_max=v1, in_values=S2[:, 0:128]), reads=["S2", "Vt"], writes=["It"])
            Vt4 = Vt[:].rearrange("p (h s a) -> p h s a", h=8, s=2)
            cand4 = cand[:].rearrange("p (h a b) -> p h a b", h=8, a=16)
            P.op("vector", lambda e: e.tensor_tensor(out=cand4, in0=Vt4[:, :, 0, :].unsqueeze(3).to_broadcast([128, 8, 16, 16]),
                                                      in1=Vt4[:, :, 1, :].unsqueeze(2).to_broadcast([128, 8, 16, 16]), op=ALU.add),
                 reads=["Vt"], writes=["cand"])
            for h in range(8):
                ch = cand[:, h * 256:(h + 1) * 256]
                v0 = CV[:, h * 16:h * 16 + 8]
                v1 = CV[:, h * 16 + 8:h * 16 + 16]
                p0 = CP[:, h * 16:h * 16 + 8]
                p1 = CP[:, h * 16 + 8:h * 16 + 16]
                P.op("vector", lambda e, ch=ch, v0=v0: e.max(out=v0, in_=ch), reads=["cand"], writes=["CV"])
                P.op("vector", lambda e, ch=ch, v0=v0, p0=p0: e.max_index(out=p0, in_max=v0, in_values=ch), reads=["cand", "CV"], writes=["CP"])
                P.op("vector", lambda e, ch=ch, v0=v0: e.match_replace(out=S2[:, :], in_to_replace=v0, in_values=ch, imm_value=-1e30),
                     reads=["cand", "CV"], writes=["S2"])
                P.op("vector", lambda e, v1=v1: e.max(out=v1, in_=S2[:, :]), reads=["S2"], writes=["CV"])
                P.op("vector", lambda e, v1=v1, p1=p1: e.max_index(out=p1, in_max=v1, in_values=S2[:, :]), reads=["S2", "CV"], writes=["CP"])
            CV3 = CV[:].rearrange("p (h k) -> p h k", h=8)
            ex3 = ex[:].rearrange("p (h k) -> p h k", h=8)
            g3 = gates[:].rearrange("p (h k) -> p h k", h=8)
            P.op("vector", lambda e: e.tensor_tensor(out=ex3, in0=CV3, in1=CV3[:, :, 0:1].to_broadcast([128, 8, 16]), op=ALU.subtract), reads=["CV"], writes=["ex"])
            P.op("scalar", lambda e: e.activation(out=ex[:, :], in_=ex[:, :], func=AF.Exp), reads=["ex"], writes=["ex"])
            P.op("vector", lambda e: e.tensor_reduce(out=Zs[:, 0:8], in_=ex3, axis=AX.X, op=ALU.add), reads=["ex"], writes=["Zs"])
            P.op("vector", lambda e: e.reciprocal(out=Zs[:, 0:8], in_=Zs[:, 0:8]), reads=["Zs"], writes=["Zs"])
            P.op("vector", lambda e: e.tensor_tensor(out=g3, in0=ex3, in1=Zs[:, 0:8].unsqueeze(2).to_broadcast([128, 8, 16]), op=ALU.mult),
                 reads=["ex", "Zs"], writes=[gatesr])
            P.op("vector", lambda e: e.tensor_single_scalar(out=au[:, :], in_=CP[:, :], scalar=4, op=ALU.logical_shift_right), reads=["CP"], writes=["au"])
            P.op("vector", lambda e: e.tensor_single_scalar(out=CP[:, :], in_=CP[:, :], scalar=15, op=ALU.bitwise_and), reads=["CP", "au"], writes=["CP"])
            P.op("vector", lambda e: e.tensor_copy(out=af[:, :], in_=au[:, :]), reads=["au"], writes=["af"])
            P.op("vector", lambda e: e.tensor_copy(out=bf[:, :], in_=CP[:, :]), reads=["CP"], writes=["bf"])
            P.op("vector", lambda e: e.tensor_copy(out=S2[:, :], in_=It[:, :]), reads=["It"], writes=["S2"])
            Itf4 = S2[:].rearrange("p (h s a) -> p h s a", h=8, s=2)
            oh4 = Ssb[:].rearrange("p (h k a) -> p h k a", h=4, k=16)
            io4 = iota16[:, :].unsqueeze(1).unsqueeze(1).to_broadcast([128, 4, 16, 16])
            for (srcf, s_, sres) in ((af, 0, "af"), (bf, 1, "bf")):
                s3 = srcf[:].rearrange("p (h k) -> p h k", h=8)
                for hh in range(2):
                    hs = slice(hh * 4, hh * 4 + 4)
                    P.op("vector", lambda e, s3=s3, hs=hs: e.tensor_tensor(out=oh4, in0=s3[:, hs, :].unsqueeze(3).to_broadcast([128, 4, 16, 16]), in1=io4, op=ALU.is_equal),
                         reads=[sres, "iota16"], writes=["Ssb"])
                    P.op("vector", lambda e, s_=s_, hs=hs: e.tensor_tensor(out=oh4, in0=oh4, in1=Itf4[:, hs, s_, :].unsqueeze(2).to_broadcast([128, 4, 16, 16]), op=ALU.mult),
                         reads=["Ssb", "S2"], writes=["Ssb"])
                    P.op("vector", lambda e, s3=s3, hs=hs: e.tensor_reduce(out=s3[:, hs, :], in_=oh4, axis=AX.X, op=ALU.add), reads=["Ssb"], writes=[sres])
            P.op("vector", lambda e: e.scalar_tensor_tensor(out=af[:, :], in0=af[:, :], scalar=128.0, in1=bf[:, :], op0=ALU.mult, op1=ALU.add),
                 reads=["af", "bf"], writes=["af"])
            P.op("vector", lambda e: e.tensor_copy(out=idx[:, :], in_=af[:, :]), reads=["af"], writes=[idxr])

        def experts(i, x_, orow, filler=None):
            xr = "xt%d" % x_
            h2 = xt[x_]
            rate = (len(filler) // n_peer_j + 1) if filler else 0
            idx = idx2[i]
            gates = gates2[i]
            ssc = ss[:, 2 + i:3 + i]
            ssr2 = "ss2_%d" % i
            idxr = "idx%d" % i
            gatesr = "gates%d" % i

            def fill(n):
                for _ in range(n):
                    if filler:
                        filler.pop(0)()
            nj = n_peer_j
            nb = nj // JB

            nj = n_peer_j
            def gather(j):
                s_ = j % NS
                P.dma("gpsimd", lambda e: e.indirect_dma_start(out=GS[s_][:, :], out_offset=None, in_=tab_b,
                                                               in_offset=bass.IndirectOffsetOnAxis(ap=idx[:, j:j + 1], axis=0)),
                      "g" + GSres[s_], reads=[idxr] + TBres, writes=[GSres[s_]])

            def dot(j):
                s_ = j % NS
                p_ = j % 2
                P.op("vector", lambda e: e.tensor_tensor(out=prod[p_][:, :], in0=GS[s_][:, 0:D], in1=hbf[i][:, :], op=ALU.mult),
                     reads=[GSres[s_], "hbf%d" % i], writes=["prod%d" % p_])
                P.op("scalar", lambda e: e.activation(out=prod[p_][:, :], in_=prod[p_][:, :], func=AF.Copy, accum_out=apre[:, j:j + 1]),
                     reads=["prod%d" % p_], writes=["prod%d" % p_, "apre%d" % j])

            def gelu(j):
                P.op("scalar", lambda e: e.activation(out=apre[:, j:j + 1], in_=apre[:, j:j + 1], func=AF.Gelu_apprx_tanh),
                     reads=["apre%d" % j], writes=["apre%d" % j])

            def acc(j):
                s_ = j % NS
                d_ = j % len(diag)
                P.op("vector", lambda e: e.scalar_tensor_tensor(out=diag[d_][:, :], in0=identb[:, :], scalar=apre[:, j:j + 1],
                                                                in1=gates[:, j:j + 1].to_broadcast([128, 128]), op0=ALU.mult, op1=ALU.mult),
                     reads=["identb", "apre%d" % j, gatesr], writes=["diag%d" % d_])
                for half in range(2):
                    P.op("tensor", lambda e, half=half: e.matmul(psA[:, half * 512:(half + 1) * 512], lhsT=diag[d_][:, :],
                                                                 rhs=GS[s_][:, D + half * 512:D + (half + 1) * 512], start=(j == 0), stop=(j == nj - 1)),
                         reads=["diag%d" % d_, GSres[s_]], writes=["b%d" % half])

            for t in range(nj + 4):
                if t < nj:
                    gather(t)
                if 0 <= t - 1 < nj:
                    dot(t - 1)
                if 0 <= t - 2 < nj:
                    gelu(t - 2)
                if 0 <= t - 4 < nj:
                    acc(t - 4)
                fill(rate)
            fill(100000)
            P.op("vector", lambda e: e.tensor_tensor(out=h2[:, :], in0=psA[:, 0:1024], in1=h2[:, :], op=ALU.add), reads=["b0", "b1", xr], writes=[xr])
            P.op("scalar", lambda e: e.activation(out=junk[:, :], in_=h2[:, :], func=AF.Square, accum_out=ss[:, 4 + i:5 + i]), reads=[xr], writes=["ss3_%d" % i] + JW)
            rstd_from_ss(ss[:, 4 + i:5 + i], 128, "ss3_%d" % i, 1.0 / D)
            P.op("vector", lambda e: e.scalar_tensor_tensor(out=h2[:, :], in0=h2[:, :], scalar=ss[:, 4 + i:5 + i], in1=gB[:, :], op0=ALU.mult, op1=ALU.mult),
                 reads=[xr, "ss3_%d" % i, "gB"], writes=[xr])
            P.dma("sync", lambda e: e.dma_start(out=out[orow:orow + 128, :], in_=h2[:, :]), "st%d" % x_, reads=[xr], writes=["out%d" % x_])

        mixer_pre()
        mixer_group(0)
        route(0, 0)
        for g in range(n_groups):
            x0 = 2 * (g % 2)
            P.defer = []
            route(1, x0 + 1)
            pending, P.defer = P.defer, None
            experts(0, x0, g * G, pending)
            pending = None
            if g + 1 < n_groups:
                P.defer = []
                mixer_group(g + 1)
                route(0, 2 * ((g + 1) % 2))
                pending, P.defer = P.defer, None
            experts(1, x0 + 1, g * G + 128, pending)
        P.wait_all("sync", ["out0", "out1", "out2", "out3"])
        P.emit()
    return nc


def _prep_shared(inp):
    f = lambda a: np.ascontiguousarray(np.asarray(a, dtype=np.float32))
    col = lambda v, n: np.asarray(v, np.float32).reshape(n, 128).T
    vecs = np.zeros((128, NVEC), np.float32)
    vecs[:, GM:GM + 8] = col(inp["norm_mix_g"][0], 8)
    vecs[:, GF:GF + 8] = col(inp["norm_ffn_g"][0], 8)
    vecs[:, GO:GO + 4] = col(inp["out_norm_g_sc"][0], 4)
    vecs[:, GO + 4:GO + 8] = col(inp["out_norm_g_cf"][0], 4)
    scw = np.asarray(inp["sc_conv_w"][0], np.float32)
    cfw = np.asarray(inp["cf_conv_w"][0], np.float32)
    for k in range(4):
        vecs[:, SCW + 3 * k:SCW + 3 * k + 3] = scw[:, k * 128:(k + 1) * 128].T
        vecs[:, CFW + 31 * k:CFW + 31 * k + 31] = cfw[:, k * 128:(k + 1) * 128].T
    vecs[:, CFB:CFB + 4] = col(inp["cf_conv_b"][0], 4)
    vecs[:, LNG:LNG + 4] = col(inp["cf_ln_g"][0], 4)
    vecs[:, LNB:LNB + 4] = col(inp["cf_ln_b"][0], 4)
    gB = np.concatenate([np.broadcast_to(np.asarray(inp["norm_ffn_g"][0], np.float32)[None, :], (128, D)),
                         np.broadcast_to(np.asarray(inp["final_norm_g"], np.float32)[None, :], (128, D))], axis=1)
    sk = np.asarray(inp["peer_sub_keys"][0], np.float32)
    keysT = np.ascontiguousarray(sk.reshape(16, 128, 128).transpose(2, 0, 1).reshape(128, 2048))
    return {
        "w_in": f(inp["w_in"][0]), "w_out": f(inp["w_out"][0]), "w_q": f(inp["peer_w_q"][0]),
        "keysT": keysT, "uv_tab": np.ascontiguousarray(np.concatenate([f(inp["peer_u"][0]), f(inp["peer_v"][0])], axis=1)),
        "vecs_h": vecs, "gB_h": np.ascontiguousarray(gB),
    }


def _core_x(inp, core, n_groups=16):
    x = np.asarray(inp["x"], np.float32)
    meta = np.asarray(inp["meta_tokens"], np.float32)
    b, half = core // 2, core % 2
    ntok = n_groups * G
    if half == 0:
        halo = np.concatenate([np.zeros((HALO - meta.shape[0], D), np.float32), meta], axis=0)
        body = x[b, 0:ntok]
    else:
        halo = x[b, 4096 - HALO:4096]
        body = x[b, 4096:4096 + ntok]
    return np.ascontiguousarray(np.concatenate([halo, body], axis=0))


def kernel(**inputs):
    shared = _prep_shared(inputs)
    nc = build(16)
    in_maps = []
    for c in range(N_CORES):
        m = dict(shared)
        m["xh"] = _core_x(inputs, c)
        in_maps.append(m)
    res = run_bass_kernel_spmd(nc, in_maps, core_ids=list(range(N_CORES)))
    outp = np.empty((4, 8192, D), np.float32)
    for c in range(N_CORES):
        b, half = c // 2, c % 2
        outp[b, half * 4096:(half + 1) * 4096] = res.results[c]["out"]
    return outp
```

```python
import numpy as np
from contextlib import ExitStack
import concourse.bass as bass
import concourse.mybir as mybir
from concourse.bass_utils import run_bass_kernel_spmd

F32 = mybir.dt.float32
BF16 = mybir.dt.bfloat16
U32 = mybir.dt.uint32
ALU = mybir.AluOpType
AF = mybir.ActivationFunctionType
AX = mybir.AxisListType

D = 1024
G = 256
HALO = 32
N_CORES = 8
EPS = 1e-6
NS = 10
JB = 4
GM, GF, GO, SCW, CFW, CFB, LNG, LNB, NVEC = 0, 8, 16, 24, 36, 160, 164, 168, 172


class Prog:
    ENGS = ("sync", "scalar", "vector", "gpsimd", "tensor")

    def __init__(self, nc, stack):
        self.nc = nc
        self.stack = stack
        self.q = {e: [] for e in self.ENGS}
        self.cnt = {e: 0 for e in self.ENGS}
        self.sem = {e: stack.enter_context(nc.semaphore("c_" + e)) for e in self.ENGS}
        self.seen = {e: {} for e in self.ENGS}
        self.last_w = {}
        self.readers = {}
        self.dsem = {}
        self.dcnt = {}
        self.alias = {}
        self.defer = None

    def _exp(self, names):
        out = []
        for n in names:
            out.extend(self.alias.get(n, (n,)))
        return out

    def _deps(self, e, reads, writes):
        deps = {}

        def add(ev):
            if ev is None:
                return
            k, s, v = ev
            if k not in deps or deps[k][1] < v:
                deps[k] = (s, v)

        for r in reads:
            add(self.last_w.get(r))
        for w in writes:
            rd = self.readers.get(w, ())
            if not rd:
                add(self.last_w.get(w))
            for ev in rd:
                add(ev)
        for k, (s, v) in deps.items():
            if e == "tensor" and k == "e_tensor":
                continue
            if self.seen[e].get(k, 0) < v:
                self.seen[e][k] = v
                self.q[e].append(lambda eng, s=s, v=v: eng.wait_ge(s, v))

    def _commit(self, ev, reads, writes):
        for w in writes:
            self.last_w[w] = ev
            self.readers[w] = []
        for r in reads:
            if r not in writes:
                self.readers.setdefault(r, []).append(ev)

    def op(self, e, fn, reads=(), writes=(), weak=()):
        if self.defer is not None:
            self.defer.append(lambda: self._op(e, fn, reads, writes, weak))
            return
        self._op(e, fn, reads, writes, weak)

    def _op(self, e, fn, reads=(), writes=(), weak=()):
        reads, writes, weak = self._exp(reads), self._exp(writes), self._exp(weak)
        self._deps(e, list(reads) + list(weak), writes)
        self.cnt[e] += 1
        n = self.cnt[e]
        s = self.sem[e]
        self.q[e].append(lambda eng, fn=fn, s=s: fn(eng).then_inc(s, 1))
        self._commit(("e_" + e, s, n), reads, writes)

    def dma(self, e, fn, key, reads=(), writes=()):
        if self.defer is not None:
            self.defer.append(lambda: self._dma(e, fn, key, reads, writes))
            return
        self._dma(e, fn, key, reads, writes)

    def _dma(self, e, fn, key, reads=(), writes=()):
        reads, writes = self._exp(reads), self._exp(writes)
        self._deps(e, reads, writes)
        if key not in self.dsem:
            self.dsem[key] = self.stack.enter_context(self.nc.semaphore("d_" + key))
            self.dcnt[key] = 0
        s = self.dsem[key]
        self.dcnt[key] += 16
        v = self.dcnt[key]
        self.q[e].append(lambda eng, fn=fn, s=s: fn(eng).then_inc(s, 16))
        self._commit(("d_" + key, s, v), reads, writes)

    def wait_all(self, e, resources):
        self._deps(e, self._exp(resources), ())

    def emit(self):
        with self.nc.Block() as block:
            @block.sync
            def _(eng):
                for f in self.q["sync"]:
                    f(eng)

            @block.scalar
            def _(eng):
                for f in self.q["scalar"]:
                    f(eng)

            @block.vector
            def _(eng):
                for f in self.q["vector"]:
                    f(eng)

            @block.gpsimd
            def _(eng):
                for f in self.q["gpsimd"]:
                    f(eng)

            @block.tensor
            def _(eng):
                for f in self.q["tensor"]:
                    f(eng)


def build(n_groups=16, n_peer_j=128):
    nc = bass.Bass("TRN2", target_bir_lowering=False)
    ntok = HALO + n_groups * G
    dr = lambda name, shape, kind="ExternalInput", dt=F32: nc.dram_tensor(name, shape, dt, kind=kind).ap()
    xh = dr("xh", [ntok, D])
    w_in = dr("w_in", [D, 2560])
    w_out = dr("w_out", [D, D])
    w_q = dr("w_q", [D, 2048])
    keysT = dr("keysT", [128, 2048])
    uv_tab = dr("uv_tab", [16384, 2 * D])
    tab_b = dr("tab_b", [16384, 2 * D], kind="Internal", dt=BF16)
    vecs_d = dr("vecs_h", [128, NVEC])
    gB_d = dr("gB_h", [128, 2048])
    out = dr("out", [n_groups * G, D], kind="ExternalOutput")

    with ExitStack() as st:
        P = Prog(nc, st)
        sb = lambda name, shape, dt=F32: st.enter_context(nc.sbuf_tensor(name, shape, dt))
        Wib = sb("Wib", [128, 8, 2560], BF16)
        Wob = sb("Wob", [128, 8, 1024], BF16)
        Wqb = sb("Wqb", [128, 8, 2048], BF16)
        keysb = sb("keysb", [128, 16, 128], BF16)
        vecs = sb("vecs", [128, NVEC])
        gB = sb("gB", [128, D])
        identb = sb("identb", [128, 128], BF16)
        onesf = sb("onesf", [128, 128])
        iota16 = sb("iota16", [128, 16])
        xt = [sb("xt%d" % i, [128, D]) for i in range(4)]
        hnb = sb("hnb", [128, D], BF16)
        ss = sb("ss", [128, 8])
        hnT = sb("hnT", [128, 8, G], BF16)
        cgs = sb("cgs", [128, G])
        zb = [sb("z%d" % k, [128, HALO + G]) for k in range(4)]
        mixA = sb("mixA", [128, 2048])
        ysc = [mixA[:, k * G:(k + 1) * G] for k in range(4)]
        sq = [sb("sq%d" % k, [128, G]) for k in range(2)]
        ub = [sb("u%d" % k, [128, HALO + G], BF16) for k in range(4)]
        dgs = [sb("dg%d" % k, [128, 128], BF16) for k in range(4)]
        cv = [mixA[:, 1024 + k * G:1024 + (k + 1) * G] for k in range(4)]
        mt = sb("mt", [128, G])
        rt = sb("rt", [128, G])
        qTb = sb("qTb", [128, 1024], BF16)
        Ssb = sb("Ssb", [128, 1024])
        S2 = sb("S2", [128, 256])
        Vt = sb("Vt", [128, 256])
        It = sb("It", [128, 256], U32)
        cand = mixA
        CV = sb("CV", [128, 128])
        CP = sb("CP", [128, 128], U32)
        au = sb("au", [128, 128], U32)
        af = sb("af", [128, 128])
        bf = sb("bf", [128, 128])
        idx2 = [sb("idx%d" % i, [128, 128], U32) for i in range(2)]
        ex = sb("ex", [128, 128])
        Zs = sb("Zs", [128, 8])
        gates2 = [sb("gates%d" % i, [128, 128]) for i in range(2)]
        apre = sb("apre", [128, 128])
        diag = [sb("diag%d" % i, [128, 128], BF16) for i in range(4)]
        GS = [sb("GS%d" % i, [128, 2 * D], BF16) for i in range(NS)]
        yn = [sb("yn%d" % k, [128, G], BF16) for k in range(8)]
        psA = st.enter_context(nc.psum_tensor("psA", [128, 6 * 512], F32))
        psT = st.enter_context(nc.psum_tensor("psT", [128, 1024], BF16))
        pslot = lambda s_, n: psA[:, 1024 + s_ * 256:1024 + s_ * 256 + n]
        bank = lambda i, n=512: psA[:, i * 512:i * 512 + n]
        psT3 = psT[:].rearrange("p (c t) -> p c t", c=8)
        prod = [sb("prod%d" % i, [128, D], BF16) for i in range(2)]
        hbf = [sb("hbf%d" % i, [128, D], BF16) for i in range(2)]
        junk = prod[1][:, :]
        JW = ["prod1"]
        JR = []

        vcol = lambda c: vecs[:, c:c + 1]
        P.alias["cand"] = ["ysc%d" % k for k in range(4)] + ["cv%d" % k for k in range(4)]
        P.alias["mixlo"] = ["ysc%d" % k for k in range(4)]
        P.alias["mixhi"] = ["cv%d" % k for k in range(4)]
        P.alias["b2"] = ["p0", "p1"]
        P.alias["b3"] = ["p2", "p3"]

        P.dma("sync", lambda e: e.dma_start(out=vecs[:], in_=vecs_d), "ldv", writes=["vecs"])
        P.dma("sync", lambda e: e.dma_start(out=gB[:], in_=gB_d[:, D:2 * D]), "ldg", writes=["gB"])
        P.dma("sync", lambda e: e.dma_start(out=xt[0][:, :], in_=gB_d[:, 0:D]), "ldx0", writes=["xt0"])
        P.op("gpsimd", lambda e: e.memset(apre[:], 1.0), writes=["apre_setup"])
        P.op("gpsimd", lambda e: e.affine_select(out=apre[:], in_=apre[:], pattern=[[-1, 128]], compare_op=ALU.is_equal,
                                                  fill=0.0, base=0, channel_multiplier=1), reads=["apre_setup"], writes=["apre_setup"])
        P.op("vector", lambda e: e.tensor_copy(out=identb[:], in_=apre[:]), reads=["apre_setup"], writes=["identb"])
        P.op("gpsimd", lambda e: e.memset(onesf[:], 1.0), writes=["onesf"])
        P.op("gpsimd", lambda e: e.iota(iota16[:], pattern=[[1, 16]], base=0, channel_multiplier=0,
                                        allow_small_or_imprecise_dtypes=True), writes=["iota16"])
        stage = [(mixA[:, 0:1024], "mixlo"), (mixA[:, 1024:2048], "mixhi"), (xt[1][:, :], "xt1"), (xt[2][:, :], "xt2")]
        si = 0
        for (Wd, Wb, ncol, gcol, nm) in ((w_in, Wib, 2560, GM, "Wib"), (w_out, Wob, 1024, GO, "Wob"), (w_q, Wqb, 2048, GF, "Wqb")):
            for c in range(8):
                for c0 in range(0, ncol, 1024):
                    w = min(1024, ncol - c0)
                    sbuf_, rn = stage[si % 4]
                    si += 1
                    P.dma("sync" if si % 2 else "scalar",
                          lambda e, sbuf_=sbuf_, Wd=Wd, c=c, c0=c0, w=w: e.dma_start(out=sbuf_[:, 0:w], in_=Wd[c * 128:(c + 1) * 128, c0:c0 + w]),
                          "ld" + rn, writes=[rn])
                    if si % 2:
                        P.op("vector", lambda e, sbuf_=sbuf_, Wb=Wb, c=c, c0=c0, w=w, gcol=gcol: e.tensor_scalar(
                            out=Wb[:, c, c0:c0 + w], in0=sbuf_[:, 0:w], scalar1=vcol(gcol + c), scalar2=None, op0=ALU.mult),
                            reads=[rn, "vecs"], writes=[nm])
                    else:
                        P.op("scalar", lambda e, sbuf_=sbuf_, Wb=Wb, c=c, c0=c0, w=w, gcol=gcol: e.activation(
                            out=Wb[:, c, c0:c0 + w], in_=sbuf_[:, 0:w], func=AF.Copy, scale=vcol(gcol + c)),
                            reads=[rn, "vecs"], writes=[nm])
        for c0 in range(0, 2048, 1024):
            sbuf_, rn = stage[si % 4]
            si += 1
            P.dma("sync", lambda e, sbuf_=sbuf_, c0=c0: e.dma_start(out=sbuf_[:, :], in_=keysT[:, c0:c0 + 1024]), "ld" + rn, writes=[rn])
            P.op("vector", lambda e, sbuf_=sbuf_, c0=c0: e.tensor_copy(out=keysb[:].rearrange("p g n -> p (g n)")[:, c0:c0 + 1024], in_=sbuf_[:, :]),
                 reads=[rn], writes=["keysb"])
        GSres = ["GS%d" % i for i in range(NS)]
        TBres = ["tab_b%d" % i for i in range(NS)]
        ustage = [(mixA[:, 0:1024], "mixlo"), (xt[1][:, :], "xt1"), (xt[3][:, :], "xt3")]
        vstage = [(mixA[:, 1024:2048], "mixhi"), (xt[2][:, :], "xt2")]
        for it in range(128):
            su_, ru = ustage[it % 3]
            sv_, rv = vstage[it % 2]
            gs_ = it % NS
            P.dma("sync", lambda e, su_=su_, it=it: e.dma_start(out=su_[:, :], in_=uv_tab[it * 128:(it + 1) * 128, 0:D]), "ldu" + ru, writes=[ru])
            P.dma("sync", lambda e, sv_=sv_, it=it: e.dma_start(out=sv_[:, :], in_=uv_tab[it * 128:(it + 1) * 128, D:2 * D]), "ldv" + rv, writes=[rv])
            P.op("vector", lambda e, su_=su_, gs_=gs_: e.tensor_tensor(out=GS[gs_][:, 0:D], in0=su_[:, :], in1=xt[0][:, :], op=ALU.mult),
                 reads=[ru, "xt0"], writes=[GSres[gs_] + "u"])
            P.op("scalar", lambda e, sv_=sv_, gs_=gs_: e.activation(out=GS[gs_][:, D:2 * D], in_=sv_[:, :], func=AF.Copy), reads=[rv], writes=[GSres[gs_] + "v"])
            P.dma("scalar", lambda e, gs_=gs_, it=it: e.dma_start(out=tab_b[it * 128:(it + 1) * 128, :], in_=GS[gs_][:, :]),
                  "st" + GSres[gs_], reads=[GSres[gs_] + "u", GSres[gs_] + "v", GSres[gs_]], writes=[TBres[gs_]])

        def rstd_from_ss(ss_ap, n, res, scale):
            P.op("vector", lambda e: e.tensor_scalar(out=ss_ap, in0=ss_ap, scalar1=scale, scalar2=EPS, op0=ALU.mult, op1=ALU.add),
                 reads=[res], writes=[res])
            P.op("scalar", lambda e: e.activation(out=ss_ap, in_=ss_ap, func=AF.Sqrt), reads=[res], writes=[res])
            P.op("vector", lambda e: e.reciprocal(out=ss_ap, in_=ss_ap), reads=[res], writes=[res])

        def bcast_rstd(src_bank, src_res, dst, dst_res, N, scale):
            P.op("vector", lambda e: e.tensor_scalar(out=dst[:, 0:N], in0=src_bank[:, 0:N], scalar1=scale, scalar2=EPS, op0=ALU.mult, op1=ALU.add),
                 reads=[src_res], writes=[dst_res])
            P.op("scalar", lambda e: e.activation(out=dst[:, 0:N], in_=dst[:, 0:N], func=AF.Sqrt), reads=[dst_res], writes=[dst_res])
            P.op("vector", lambda e: e.reciprocal(out=dst[:, 0:N], in_=dst[:, 0:N]), reads=[dst_res], writes=[dst_res])

        def front(tiles, N, xs):
            off = 0
            for i, (r0, n) in enumerate(tiles):
                xr = "xt%d" % xs[i]
                xi = xt[xs[i]]
                P.dma("sync", lambda e, xi=xi, i=i, r0=r0, n=n: e.dma_start(out=xi[0:n, :], in_=xh[r0:r0 + n, :]), "ldx%d" % xs[i], writes=[xr])
                ssr = "ss%d" % i
                P.op("scalar", lambda e, xi=xi, i=i, n=n: e.activation(out=junk[0:n, :], in_=xi[0:n, :], func=AF.Square, accum_out=ss[0:n, i:i + 1]),
                     reads=[xr], writes=[ssr] + JW)
                rstd_from_ss(ss[0:n, i:i + 1], n, ssr, 1.0 / D)
                P.op("scalar", lambda e, xi=xi, i=i, n=n: e.activation(out=hnb[0:n, :], in_=xi[0:n, :], func=AF.Copy, scale=ss[0:n, i:i + 1]),
                     reads=[xr, ssr], writes=["hnb"])
                for c in range(8):
                    P.op("tensor", lambda e, c=c, n=n: e.transpose(out=psT3[:, c, 0:n], in_=hnb[0:n, c * 128:(c + 1) * 128], identity=identb[0:n, 0:n]),
                         reads=["hnb", "identb"], writes=["bT"])
                P.op("vector", lambda e, n=n, off=off: e.tensor_copy(out=hnT[:, :, off:off + n], in_=psT3[:, :, 0:n]), reads=["bT"], writes=["hnT"])
                off += n

        def proj(col, b, N):
            for k in range(8):
                P.op("tensor", lambda e, k=k, col=col, b=b, N=N: e.matmul(pslot(b, N), lhsT=Wib[:, k, col * 128:(col + 1) * 128], rhs=hnT[:, k, 0:N],
                                                                            start=(k == 0), stop=(k == 7)),
                     reads=["Wib", "hnT"], writes=["p%d" % b])

        def mixer_pre():
            N = HALO
            front([(0, HALO)], N, [0])
            for k in range(4):
                proj(k, 0, N)
                proj(8 + k, 1, N)
                P.op("scalar", lambda e: e.activation(out=cgs[:, 0:N], in_=pslot(1, N), func=AF.Copy), reads=["p1"], writes=["cgs"])
                P.op("vector", lambda e, k=k: e.tensor_tensor(out=zb[k][:, 0:N], in0=pslot(0, N), in1=cgs[:, 0:N], op=ALU.mult),
                     reads=["p0", "cgs"], writes=["z%d" % k])
                proj(12 + k, 0, N)
                proj(16 + k, 1, N)
                P.op("scalar", lambda e: e.activation(out=cgs[:, 0:N], in_=pslot(1, N), func=AF.Sigmoid), reads=["p1"], writes=["cgs"])
                P.op("vector", lambda e, k=k: e.tensor_tensor(out=ub[k][:, 0:N], in0=pslot(0, N), in1=cgs[:, 0:N], op=ALU.mult),
                     reads=["p0", "cgs"], writes=["u%d" % k])

        def mixer_group(g):
            N = G
            base = HALO + g * G
            tiles = [(base, 128), (base + 128, 128)]
            xs = [2 * (g % 2), 2 * (g % 2) + 1]
            front(tiles, N, xs)
            for k in range(4):
                proj(k, 0, N)
                proj(8 + k, 1, N)
                proj(4 + k, 2, N)
                zr = "z%d" % k
                P.op("scalar", lambda e: e.activation(out=cgs[:, 0:N], in_=pslot(1, N), func=AF.Copy), reads=["p1"], writes=["cgs"])
                P.op("vector", lambda e, k=k: e.tensor_tensor(out=zb[k][:, HALO:HALO + N], in0=pslot(0, N), in1=cgs[:, 0:N], op=ALU.mult),
                     reads=["p0", "cgs"], writes=[zr])
                P.op("vector", lambda e, k=k: e.tensor_scalar(out=rt[:, 0:N], in0=zb[k][:, HALO - 2:HALO - 2 + N], scalar1=vcol(SCW + 3 * k), scalar2=None, op0=ALU.mult),
                     reads=[zr, "vecs"], writes=["rt"])
                for j in (1, 2):
                    P.op("vector", lambda e, k=k, j=j: e.scalar_tensor_tensor(out=rt[:, 0:N], in0=zb[k][:, HALO - 2 + j:HALO - 2 + j + N], scalar=vcol(SCW + 3 * k + j),
                                                                              in1=rt[:, 0:N], op0=ALU.mult, op1=ALU.add),
                         reads=[zr, "vecs", "rt"], writes=["rt"])
                yr = "ysc%d" % k
                P.op("vector", lambda e, k=k: e.tensor_tensor(out=ysc[k][:, 0:N], in0=pslot(2, N), in1=rt[:, 0:N], op=ALU.mult),
                     reads=["p2", "rt"], writes=[yr])
                sr = "sq%d" % (k % 2)
                P.op("scalar", lambda e, k=k: e.activation(out=sq[k % 2][:, 0:N], in_=ysc[k][:, 0:N], func=AF.Square), reads=[yr], writes=[sr])
                P.op("tensor", lambda e, k=k: e.matmul(bank(4, N), lhsT=onesf[:], rhs=sq[k % 2][:, 0:N], start=(k == 0), stop=(k == 3)),
                     reads=[sr, "onesf"], writes=["b4"])
                P.op("gpsimd", lambda e, k=k: e.tensor_copy(out=zb[k][:, 0:HALO], in_=zb[k][:, G:G + HALO]), reads=[zr], writes=[zr])
            bcast_rstd(bank(4), "b4", mt, "mt", N, 1.0 / 512)
            for k in range(4):
                P.op("vector", lambda e, k=k: e.tensor_tensor(out=yn[k][:, 0:N], in0=ysc[k][:, 0:N], in1=mt[:, 0:N], op=ALU.mult),
                     reads=["ysc%d" % k, "mt"], writes=["yn%d" % k])
            if P.defer is not None:
                P.mark = len(P.defer)
            for k in range(4):
                proj(12 + k, 0, N)
                proj(16 + k, 1, N)
                ur = "u%d" % k
                cr = "cv%d" % k
                P.op("scalar", lambda e: e.activation(out=cgs[:, 0:N], in_=pslot(1, N), func=AF.Sigmoid), reads=["p1"], writes=["cgs"])
                P.op("vector", lambda e, k=k: e.tensor_tensor(out=ub[k][:, HALO:HALO + N], in0=pslot(0, N), in1=cgs[:, 0:N], op=ALU.mult),
                     reads=["p0", "cgs"], writes=[ur])
                for j in range(31):
                    dn = (k * 31 + j) % len(dgs)
                    dr_ = "dg%d" % dn
                    if j % 2:
                        P.op("vector", lambda e, k=k, j=j, dn=dn: e.tensor_tensor(out=dgs[dn][:, :], in0=identb[:, :],
                                                                                 in1=vcol(CFW + 31 * k + j).to_broadcast([128, 128]), op=ALU.mult),
                             reads=["identb", "vecs"], writes=[dr_])
                    else:
                        P.op("scalar", lambda e, k=k, j=j, dn=dn: e.activation(out=dgs[dn][:, :], in_=identb[:, :], func=AF.Copy, scale=vcol(CFW + 31 * k + j)),
                             reads=["identb", "vecs"], writes=[dr_])
                    P.op("tensor", lambda e, k=k, j=j, dn=dn: e.matmul(pslot(3, N), lhsT=dgs[dn][:, :], rhs=ub[k][:, 2 + j:2 + j + N], start=(j == 0), stop=(j == 30)),
                         reads=[dr_, ur], writes=["p3"])
                P.op("vector", lambda e, k=k: e.tensor_scalar(out=cv[k][:, 0:N], in0=pslot(3, N), scalar1=vcol(CFB + k), scalar2=None, op0=ALU.add),
                     reads=["p3", "vecs"], writes=[cr])
                sr = "sq%d" % (k % 2)
                P.op("scalar", lambda e, k=k: e.activation(out=sq[k % 2][:, 0:N], in_=cv[k][:, 0:N], func=AF.Square), reads=[cr], writes=[sr])
                P.op("tensor", lambda e, k=k: e.matmul(bank(4, N), lhsT=onesf[:], rhs=cv[k][:, 0:N], start=(k == 0), stop=(k == 3)),
                     reads=[cr, "onesf"], writes=["b4"])
                P.op("tensor", lambda e, k=k: e.matmul(bank(5, N), lhsT=onesf[:], rhs=sq[k % 2][:, 0:N], start=(k == 0), stop=(k == 3)),
                     reads=[sr, "onesf"], writes=["b5"])
                P.op("gpsimd", lambda e, k=k: e.tensor_copy(out=ub[k][:, 0:HALO], in_=ub[k][:, G:G + HALO]), reads=[ur], writes=[ur])
            P.op("vector", lambda e: e.tensor_scalar(out=mt[:, 0:N], in0=bank(4, N), scalar1=1.0 / 512, scalar2=None, op0=ALU.mult), reads=["b4"], writes=["mt"])
            P.op("vector", lambda e: e.tensor_tensor(out=sq[0][:, 0:N], in0=mt[:, 0:N], in1=mt[:, 0:N], op=ALU.mult), reads=["mt"], writes=["sq0"])
            P.op("vector", lambda e: e.scalar_tensor_tensor(out=rt[:, 0:N], in0=bank(5, N), scalar=1.0 / 512, in1=sq[0][:, 0:N], op0=ALU.mult, op1=ALU.subtract),
                 reads=["b5", "sq0"], writes=["rt"])
            P.op("vector", lambda e: e.tensor_scalar(out=rt[:, 0:N], in0=rt[:, 0:N], scalar1=EPS, scalar2=None, op0=ALU.add), reads=["rt"], writes=["rt"])
            P.op("scalar", lambda e: e.activation(out=rt[:, 0:N], in_=rt[:, 0:N], func=AF.Sqrt), reads=["rt"], writes=["rt"])
            P.op("vector", lambda e: e.reciprocal(out=rt[:, 0:N], in_=rt[:, 0:N]), reads=["rt"], writes=["rt"])
            for k in range(4):
                cr = "cv%d" % k
                P.op("vector", lambda e, k=k: e.tensor_tensor(out=cv[k][:, 0:N], in0=cv[k][:, 0:N], in1=mt[:, 0:N], op=ALU.subtract), reads=[cr, "mt"], writes=[cr])
                P.op("vector", lambda e, k=k: e.tensor_tensor(out=cv[k][:, 0:N], in0=cv[k][:, 0:N], in1=rt[:, 0:N], op=ALU.mult), reads=[cr, "rt"], writes=[cr])
                P.op("scalar", lambda e, k=k: e.activation(out=cv[k][:, 0:N], in_=cv[k][:, 0:N], func=AF.Silu, scale=vcol(LNG + k), bias=vcol(LNB + k)),
                     reads=[cr, "vecs"], writes=[cr])
                sr = "sq%d" % (k % 2)
                P.op("scalar", lambda e, k=k: e.activation(out=sq[k % 2][:, 0:N], in_=cv[k][:, 0:N], func=AF.Square), reads=[cr], writes=[sr])
                P.op("tensor", lambda e, k=k: e.matmul(bank(4, N), lhsT=onesf[:], rhs=sq[k % 2][:, 0:N], start=(k == 0), stop=(k == 3)),
                     reads=[sr, "onesf"], writes=["b4"])
            bcast_rstd(bank(4), "b4", mt, "mt", N, 1.0 / 512)
            for k in range(4):
                P.op("vector", lambda e, k=k: e.tensor_tensor(out=yn[4 + k][:, 0:N], in0=cv[k][:, 0:N], in1=mt[:, 0:N], op=ALU.mult),
                     reads=["cv%d" % k, "mt"], writes=["yn%d" % (4 + k)])
            for i in range(2):
                for half in range(2):
                    for kk in range(8):
                        P.op("tensor", lambda e, i=i, half=half, kk=kk: e.matmul(psA[:, (2 + half) * 512:(3 + half) * 512], lhsT=yn[kk][:, i * 128:(i + 1) * 128],
                                                                                  rhs=Wob[:, kk, half * 512:(half + 1) * 512], start=(kk == 0), stop=(kk == 7)),
                             reads=["yn%d" % kk, "Wob"], writes=["b%d" % (2 + half)])
                P.op("vector", lambda e, i=i: e.tensor_tensor(out=xt[xs[i]][:, :], in0=psA[:, 2 * 512:4 * 512], in1=xt[xs[i]][:, :], op=ALU.add),
                     reads=["b2", "b3", "xt%d" % xs[i]], writes=["xt%d" % xs[i]])

        def route(i, x_):
            xr = "xt%d" % x_
            h2 = xt[x_]
            idx = idx2[i]
            gates = gates2[i]
            ssc = ss[:, 2 + i:3 + i]
            ssr2 = "ss2_%d" % i
            idxr = "idx%d" % i
            gatesr = "gates%d" % i
            P.op("scalar", lambda e: e.activation(out=junk[:, :], in_=h2[:, :], func=AF.Square, accum_out=ssc), reads=[xr], writes=[ssr2] + JW)
            rstd_from_ss(ssc, 128, ssr2, 1.0 / D)
            P.op("scalar", lambda e: e.activation(out=hbf[i][:, :], in_=h2[:, :], func=AF.Copy, scale=ssc), reads=[xr, ssr2], writes=["hbf%d" % i])
            for c in range(8):
                P.op("tensor", lambda e, c=c: e.transpose(out=psT3[:, c, :], in_=hbf[i][:, c * 128:(c + 1) * 128], identity=identb[:, :]),
                     reads=["hbf%d" % i, "identb"], writes=["bT"])
            P.op("vector", lambda e: e.tensor_copy(out=hnT[:, :, 0:128], in_=psT3[:, :, :]), reads=["bT"], writes=["hnT"])
            for hf in range(2):
                for gg in range(8):
                    g = hf * 8 + gg
                    for k in range(8):
                        P.op("tensor", lambda e, g=g, gg=gg, k=k: e.matmul(psA[:, 2048 + gg * 128:2048 + (gg + 1) * 128], lhsT=Wqb[:, k, g * 128:(g + 1) * 128],
                                                                        rhs=hnT[:, k, 0:128], start=(k == 0), stop=(k == 7)),
                             reads=["Wqb", "hnT"], writes=["b%d" % (4 + gg // 4)])
                P.op("scalar", lambda e, hf=hf: e.activation(out=qTb[:, :], in_=psA[:, 2048:3072], func=AF.Copy), reads=["b4", "b5"], writes=["qTb"])
                for gg in range(8):
                    g = hf * 8 + gg
                    P.op("tensor", lambda e, g=g, gg=gg: e.matmul(psA[:, 2048 + gg * 128:2048 + (gg + 1) * 128], lhsT=qTb[:, gg * 128:(gg + 1) * 128], rhs=keysb[:, g, :],
                                                               start=True, stop=True),
                         reads=["qTb", "keysb"], writes=["b%d" % (4 + gg // 4)])
                P.op("scalar", lambda e: e.activation(out=Ssb[:, :], in_=psA[:, 2048:3072], func=AF.Copy), reads=["b4", "b5"], writes=["Ssb"])
                for gg in range(8):
                    g = hf * 8 + gg
                    sg_ = Ssb[:, gg * 128:(gg + 1) * 128]
                    v0 = Vt[:, g * 16:g * 16 + 8]
                    v1 = Vt[:, g * 16 + 8:g * 16 + 16]
                    i0 = It[:, g * 16:g * 16 + 8]
                    i1 = It[:, g * 16 + 8:g * 16 + 16]
                    P.op("vector", lambda e, sg_=sg_, v0=v0: e.max(out=v0, in_=sg_), reads=["Ssb"], writes=["Vt"])
                    P.op("vector", lambda e, sg_=sg_, v0=v0, i0=i0: e.max_index(out=i0, in_max=v0, in_values=sg_), reads=["Ssb", "Vt"], writes=["It"])
                    P.op("vector", lambda e, sg_=sg_, v0=v0: e.match_replace(out=S2[:, 0:128], in_to_replace=v0, in_values=sg_, imm_value=-1e30),
                         reads=["Ssb", "Vt"], writes=["S2"])
                    P.op("vector", lambda e, v1=v1: e.max(out=v1, in_=S2[:, 0:128]), reads=["S2"], writes=["Vt"])
                    P.op("vector", lambda e, v1=v1, i1=i1: e.max_index(out=i1, in_max=v1, in_values=S2[:, 0:128]), reads=["S2", "Vt"], writes=["It"])
            Vt4 = Vt[:].rearrange("p (h s a) -> p h s a", h=8, s=2)
            cand4 = cand[:].rearrange("p (h a b) -> p h a b", h=8, a=16)
            P.op("vector", lambda e: e.tensor_tensor(out=cand4, in0=Vt4[:, :, 0, :].unsqueeze(3).to_broadcast([128, 8, 16, 16]),
                                                      in1=Vt4[:, :, 1, :].unsqueeze(2).to_broadcast([128, 8, 16, 16]), op=ALU.add),
                 reads=["Vt"], writes=["cand"])
            for h in range(8):
                ch = cand[:, h * 256:(h + 1) * 256]
                v0 = CV[:, h * 16:h * 16 + 8]
                v1 = CV[:, h * 16 + 8:h * 16 + 16]
                p0 = CP[:, h * 16:h * 16 + 8]
                p1 = CP[:, h * 16 + 8:h * 16 + 16]
                P.op("vector", lambda e, ch=ch, v0=v0: e.max(out=v0, in_=ch), reads=["cand"], writes=["CV"])
                P.op("vector", lambda e, ch=ch, v0=v0, p0=p0: e.max_index(out=p0, in_max=v0, in_values=ch), reads=["cand", "CV"], writes=["CP"])
                P.op("vector", lambda e, ch=ch, v0=v0: e.match_replace(out=S2[:, :], in_to_replace=v0, in_values=ch, imm_value=-1e30),
                     reads=["cand", "CV"], writes=["S2"])
                P.op("vector", lambda e, v1=v1: e.max(out=v1, in_=S2[:, :]), reads=["S2"], writes=["CV"])
                P.op("vector", lambda e, v1=v1, p1=p1: e.max_index(out=p1, in_max=v1, in_values=S2[:, :]), reads=["S2", "CV"], writes=["CP"])
            CV3 = CV[:].rearrange("p (h k) -> p h k", h=8)
            ex3 = ex[:].rearrange("p (h k) -> p h k", h=8)
            g3 = gates[:].rearrange("p (h k) -> p h k", h=8)
            P.op("vector", lambda e: e.tensor_tensor(out=ex3, in0=CV3, in1=CV3[:, :, 0:1].to_broadcast([128, 8, 16]), op=ALU.subtract), reads=["CV"], writes=["ex"])
            P.op("scalar", lambda e: e.activation(out=ex[:, :], in_=ex[:, :], func=AF.Exp), reads=["ex"], writes=["ex"])
            P.op("vector", lambda e: e.tensor_reduce(out=Zs[:, 0:8], in_=ex3, axis=AX.X, op=ALU.add), reads=["ex"], writes=["Zs"])
            P.op("vector", lambda e: e.reciprocal(out=Zs[:, 0:8], in_=Zs[:, 0:8]), reads=["Zs"], writes=["Zs"])
            P.op("vector", lambda e: e.tensor_tensor(out=g3, in0=ex3, in1=Zs[:, 0:8].unsqueeze(2).to_broadcast([128, 8, 16]), op=ALU.mult),
                 reads=["ex", "Zs"], writes=[gatesr])
            P.op("vector", lambda e: e.tensor_single_scalar(out=au[:, :], in_=CP[:, :], scalar=4, op=ALU.logical_shift_right), reads=["CP"], writes=["au"])
            P.op("vector", lambda e: e.tensor_single_scalar(out=CP[:, :], in_=CP[:, :], scalar=15, op=ALU.bitwise_and), reads=["CP", "au"], writes=["CP"])
            P.op("vector", lambda e: e.tensor_copy(out=af[:, :], in_=au[:, :]), reads=["au"], writes=["af"])
            P.op("vector", lambda e: e.tensor_copy(out=bf[:, :], in_=CP[:, :]), reads=["CP"], writes=["bf"])
            P.op("vector", lambda e: e.tensor_copy(out=S2[:, :], in_=It[:, :]), reads=["It"], writes=["S2"])
            Itf4 = S2[:].rearrange("p (h s a) -> p h s a", h=8, s=2)
            oh4 = Ssb[:].rearrange("p (h k a) -> p h k a", h=4, k=16)
            io4 = iota16[:, :].unsqueeze(1).unsqueeze(1).to_broadcast([128, 4, 16, 16])
            for (srcf, s_, sres) in ((af, 0, "af"), (bf, 1, "bf")):
                s3 = srcf[:].rearrange("p (h k) -> p h k", h=8)
                for hh in range(2):
                    hs = slice(hh * 4, hh * 4 + 4)
                    P.op("vector", lambda e, s3=s3, hs=hs: e.tensor_tensor(out=oh4, in0=s3[:, hs, :].unsqueeze(3).to_broadcast([128, 4, 16, 16]), in1=io4, op=ALU.is_equal),
                         reads=[sres, "iota16"], writes=["Ssb"])
                    P.op("vector", lambda e, s_=s_, hs=hs: e.tensor_tensor(out=oh4, in0=oh4, in1=Itf4[:, hs, s_, :].unsqueeze(2).to_broadcast([128, 4, 16, 16]), op=ALU.mult),
                         reads=["Ssb", "S2"], writes=["Ssb"])
                    P.op("vector", lambda e, s3=s3, hs=hs: e.tensor_reduce(out=s3[:, hs, :], in_=oh4, axis=AX.X, op=ALU.add), reads=["Ssb"], writes=[sres])
            P.op("vector", lambda e: e.scalar_tensor_tensor(out=af[:, :], in0=af[:, :], scalar=128.0, in1=bf[:, :], op0=ALU.mult, op1=ALU.add),
                 reads=["af", "bf"], writes=["af"])
            P.op("vector", lambda e: e.tensor_copy(out=idx[:, :], in_=af[:, :]), reads=["af"], writes=[idxr])

        def experts(i, x_, orow, filler=None):
            xr = "xt%d" % x_
            h2 = xt[x_]
            rate = (len(filler) // n_peer_j + 1) if filler else 0
            idx = idx2[i]
            gates = gates2[i]
            ssc = ss[:, 2 + i:3 + i]
            ssr2 = "ss2_%d" % i
            idxr = "idx%d" % i
            gatesr = "gates%d" % i

            def fill(n):
                for _ in range(n):
                    if filler:
                        filler.pop(0)()
            nj = n_peer_j
            nb = nj // JB

            nj = n_peer_j
            def gather(j):
                s_ = j % NS
                P.dma("gpsimd", lambda e: e.indirect_dma_start(out=GS[s_][:, :], out_offset=None, in_=tab_b,
                                                               in_offset=bass.IndirectOffsetOnAxis(ap=idx[:, j:j + 1], axis=0)),
                      "g" + GSres[s_], reads=[idxr] + TBres, writes=[GSres[s_]])

            def dot(j):
                s_ = j % NS
                p_ = j % 2
                P.op("vector", lambda e: e.tensor_tensor(out=prod[p_][:, :], in0=GS[s_][:, 0:D], in1=hbf[i][:, :], op=ALU.mult),
                     reads=["hbf%d" % i], writes=["prod%d" % p_], weak=[GSres[s_]])
                P.op("scalar", lambda e: e.activation(out=prod[p_][:, :], in_=prod[p_][:, :], func=AF.Copy, accum_out=apre[:, j:j + 1]),
                     reads=["prod%d" % p_], writes=["prod%d" % p_, "apre%d" % j])

            def gelu(j):
                P.op("scalar", lambda e: e.activation(out=apre[:, j:j + 1], in_=apre[:, j:j + 1], func=AF.Gelu_apprx_tanh),
                     reads=["apre%d" % j], writes=["apre%d" % j])

            def acc(j):
                s_ = j % NS
                d_ = j % len(diag)
                P.op("vector", lambda e: e.scalar_tensor_tensor(out=diag[d_][:, :], in0=identb[:, :], scalar=apre[:, j:j + 1],
                                                                in1=gates[:, j:j + 1].to_broadcast([128, 128]), op0=ALU.mult, op1=ALU.mult),
                     reads=["identb", "apre%d" % j, gatesr], writes=["diag%d" % d_])
                for half in range(2):
                    P.op("tensor", lambda e, half=half: e.matmul(psA[:, half * 512:(half + 1) * 512], lhsT=diag[d_][:, :],
                                                                 rhs=GS[s_][:, D + half * 512:D + (half + 1) * 512], start=(j == 0), stop=(j == nj - 1)),
                         reads=["diag%d" % d_, GSres[s_]], writes=["b%d" % half])

            for t in range(nj + 4):
                if t < nj:
                    gather(t)
                if 0 <= t - 1 < nj:
                    dot(t - 1)
                if 0 <= t - 2 < nj:
                    gelu(t - 2)
                if 0 <= t - 4 < nj:
                    acc(t - 4)
                fill(rate)
            fill(100000)
            P.op("vector", lambda e: e.tensor_tensor(out=h2[:, :], in0=psA[:, 0:1024], in1=h2[:, :], op=ALU.add), reads=["b0", "b1", xr], writes=[xr])
            P.op("scalar", lambda e: e.activation(out=junk[:, :], in_=h2[:, :], func=AF.Square, accum_out=ss[:, 4 + i:5 + i]), reads=[xr], writes=["ss3_%d" % i] + JW)
            rstd_from_ss(ss[:, 4 + i:5 + i], 128, "ss3_%d" % i, 1.0 / D)
            P.op("vector", lambda e: e.scalar_tensor_tensor(out=h2[:, :], in0=h2[:, :], scalar=ss[:, 4 + i:5 + i], in1=gB[:, :], op0=ALU.mult, op1=ALU.mult),
                 reads=[xr, "ss3_%d" % i, "gB"], writes=[xr])
            P.dma("sync", lambda e: e.dma_start(out=out[orow:orow + 128, :], in_=h2[:, :]), "st%d" % x_, reads=[xr], writes=["out%d" % x_])

        mixer_pre()
        mixer_group(0)
        route(0, 0)
        for g in range(n_groups):
            x0 = 2 * (g % 2)
            P.defer = []
            route(1, x0 + 1)
            R1 = P.defer
            M, R0, ms = [], [], 0
            if g + 1 < n_groups:
                P.defer = []
                P.mark = 0
                mixer_group(g + 1)
                M, ms = P.defer, P.mark
                P.defer = []
                route(0, 2 * ((g + 1) % 2))
                R0 = P.defer
            P.defer = None
            experts(0, x0, g * G, R1)
            experts(1, x0 + 1, g * G + 128, M + R0)
        P.wait_all("sync", ["out0", "out1", "out2", "out3"])
        P.emit()
    return nc


def _prep_shared(inp):
    f = lambda a: np.ascontiguousarray(np.asarray(a, dtype=np.float32))
    col = lambda v, n: np.asarray(v, np.float32).reshape(n, 128).T
    vecs = np.zeros((128, NVEC), np.float32)
    vecs[:, GM:GM + 8] = col(inp["norm_mix_g"][0], 8)
    vecs[:, GF:GF + 8] = col(inp["norm_ffn_g"][0], 8)
    vecs[:, GO:GO + 4] = col(inp["out_norm_g_sc"][0], 4)
    vecs[:, GO + 4:GO + 8] = col(inp["out_norm_g_cf"][0], 4)
    scw = np.asarray(inp["sc_conv_w"][0], np.float32)
    cfw = np.asarray(inp["cf_conv_w"][0], np.float32)
    for k in range(4):
        vecs[:, SCW + 3 * k:SCW + 3 * k + 3] = scw[:, k * 128:(k + 1) * 128].T
        vecs[:, CFW + 31 * k:CFW + 31 * k + 31] = cfw[:, k * 128:(k + 1) * 128].T
    vecs[:, CFB:CFB + 4] = col(inp["cf_conv_b"][0], 4)
    vecs[:, LNG:LNG + 4] = col(inp["cf_ln_g"][0], 4)
    vecs[:, LNB:LNB + 4] = col(inp["cf_ln_b"][0], 4)
    gB = np.concatenate([np.broadcast_to(np.asarray(inp["norm_ffn_g"][0], np.float32)[None, :], (128, D)),
                         np.broadcast_to(np.asarray(inp["final_norm_g"], np.float32)[None, :], (128, D))], axis=1)
    sk = np.asarray(inp["peer_sub_keys"][0], np.float32)
    keysT = np.ascontiguousarray(sk.reshape(16, 128, 128).transpose(2, 0, 1).reshape(128, 2048))
    return {
        "w_in": f(inp["w_in"][0]), "w_out": f(inp["w_out"][0]), "w_q": f(inp["peer_w_q"][0]),
        "keysT": keysT, "uv_tab": np.ascontiguousarray(np.concatenate([f(inp["peer_u"][0]), f(inp["peer_v"][0])], axis=1)),
        "vecs_h": vecs, "gB_h": np.ascontiguousarray(gB),
    }


def _core_x(inp, core, n_groups=16):
    x = np.asarray(inp["x"], np.float32)
    meta = np.asarray(inp["meta_tokens"], np.float32)
    b, half = core // 2, core % 2
    ntok = n_groups * G
    if half == 0:
        halo = np.concatenate([np.zeros((HALO - meta.shape[0], D), np.float32), meta], axis=0)
        body = x[b, 0:ntok]
    else:
        halo = x[b, 4096 - HALO:4096]
        body = x[b, 4096:4096 + ntok]
    return np.ascontiguousarray(np.concatenate([halo, body], axis=0))


def kernel(**inputs):
    shared = _prep_shared(inputs)
    nc = build(16)
    in_maps = []
    for c in range(N_CORES):
        m = dict(shared)
        m["xh"] = _core_x(inputs, c)
        in_maps.append(m)
    res = run_bass_kernel_spmd(nc, in_maps, core_ids=list(range(N_CORES)))
    outp = np.empty((4, 8192, D), np.float32)
    for c in range(N_CORES):
        b, half = c // 2, c % 2
        outp[b, half * 4096:(half + 1) * 4096] = res.results[c]["out"]
    return outp
```

```python
import numpy as np
from contextlib import ExitStack
import concourse.bass as bass
import concourse.mybir as mybir
from concourse.bass_utils import run_bass_kernel_spmd

F32 = mybir.dt.float32
BF16 = mybir.dt.bfloat16
U32 = mybir.dt.uint32
ALU = mybir.AluOpType
AF = mybir.ActivationFunctionType
AX = mybir.AxisListType

D = 1024
G = 256
HALO = 32
N_CORES = 8
EPS = 1e-6
NS = 10
JB = 4
GM, GF, GO, SCW, CFW, CFB, LNG, LNB, NVEC = 0, 8, 16, 24, 36, 160, 164, 168, 172


class Prog:
    ENGS = ("sync", "scalar", "vector", "gpsimd", "tensor")

    def __init__(self, nc, stack):
        self.nc = nc
        self.stack = stack
        self.q = {e: [] for e in self.ENGS}
        self.cnt = {e: 0 for e in self.ENGS}
        self.sem = {e: stack.enter_context(nc.semaphore("c_" + e)) for e in self.ENGS}
        self.seen = {e: {} for e in self.ENGS}
        self.last_w = {}
        self.readers = {}
        self.dsem = {}
        self.dcnt = {}
        self.alias = {}
        self.defer = None

    def _exp(self, names):
        out = []
        for n in names:
            out.extend(self.alias.get(n, (n,)))
        return out

    def _deps(self, e, reads, writes):
        deps = {}

        def add(ev):
            if ev is None:
                return
            k, s, v = ev
            if k not in deps or deps[k][1] < v:
                deps[k] = (s, v)

        for r in reads:
            add(self.last_w.get(r))
        for w in writes:
            rd = self.readers.get(w, ())
            if not rd:
                add(self.last_w.get(w))
            for ev in rd:
                add(ev)
        for k, (s, v) in deps.items():
            if e == "tensor" and k == "e_tensor":
                continue
            if self.seen[e].get(k, 0) < v:
                self.seen[e][k] = v
                self.q[e].append(lambda eng, s=s, v=v: eng.wait_ge(s, v))

    def _commit(self, ev, reads, writes):
        for w in writes:
            self.last_w[w] = ev
            self.readers[w] = []
        for r in reads:
            if r not in writes:
                self.readers.setdefault(r, []).append(ev)

    def op(self, e, fn, reads=(), writes=(), weak=()):
        if self.defer is not None:
            self.defer.append(lambda: self._op(e, fn, reads, writes, weak))
            return
        self._op(e, fn, reads, writes, weak)

    def _op(self, e, fn, reads=(), writes=(), weak=()):
        reads, writes, weak = self._exp(reads), self._exp(writes), self._exp(weak)
        self._deps(e, list(reads) + list(weak), writes)
        self.cnt[e] += 1
        n = self.cnt[e]
        s = self.sem[e]
        self.q[e].append(lambda eng, fn=fn, s=s: fn(eng).then_inc(s, 1))
        self._commit(("e_" + e, s, n), reads, writes)

    def dma(self, e, fn, key, reads=(), writes=()):
        if self.defer is not None:
            self.defer.append(lambda: self._dma(e, fn, key, reads, writes))
            return
        self._dma(e, fn, key, reads, writes)

    def _dma(self, e, fn, key, reads=(), writes=()):
        reads, writes = self._exp(reads), self._exp(writes)
        self._deps(e, reads, writes)
        if key not in self.dsem:
            self.dsem[key] = self.stack.enter_context(self.nc.semaphore("d_" + key))
            self.dcnt[key] = 0
        s = self.dsem[key]
        self.dcnt[key] += 16
        v = self.dcnt[key]
        self.q[e].append(lambda eng, fn=fn, s=s: fn(eng).then_inc(s, 16))
        self._commit(("d_" + key, s, v), reads, writes)

    def wait_all(self, e, resources):
        self._deps(e, self._exp(resources), ())

    def emit(self):
        with self.nc.Block() as block:
            @block.sync
            def _(eng):
                for f in self.q["sync"]:
                    f(eng)

            @block.scalar
            def _(eng):
                for f in self.q["scalar"]:
                    f(eng)

            @block.vector
            def _(eng):
                for f in self.q["vector"]:
                    f(eng)

            @block.gpsimd
            def _(eng):
                for f in self.q["gpsimd"]:
                    f(eng)

            @block.tensor
            def _(eng):
                for f in self.q["tensor"]:
                    f(eng)


def build(n_groups=16, n_peer_j=128):
    nc = bass.Bass("TRN2", target_bir_lowering=False)
    ntok = HALO + n_groups * G
    dr = lambda name, shape, kind="ExternalInput", dt=F32: nc.dram_tensor(name, shape, dt, kind=kind).ap()
    xh = dr("xh", [ntok, D])
    w_in = dr("w_in", [D, 2560])
    w_out = dr("w_out", [D, D])
    w_q = dr("w_q", [D, 2048])
    keysT = dr("keysT", [128, 2048])
    uv_tab = dr("uv_tab", [16384, 2 * D])
    tab_b = dr("tab_b", [16384, 2 * D], kind="Internal", dt=BF16)
    vecs_d = dr("vecs_h", [128, NVEC])
    gB_d = dr("gB_h", [128, 2048])
    out = dr("out", [n_groups * G, D], kind="ExternalOutput")

    with ExitStack() as st:
        P = Prog(nc, st)
        sb = lambda name, shape, dt=F32: st.enter_context(nc.sbuf_tensor(name, shape, dt))
        Wib = sb("Wib", [128, 8, 2560], BF16)
        Wob = sb("Wob", [128, 8, 1024], BF16)
        Wqb = sb("Wqb", [128, 8, 2048], BF16)
        keysb = sb("keysb", [128, 16, 128], BF16)
        vecs = sb("vecs", [128, NVEC])
        gB = sb("gB", [128, D])
        identb = sb("identb", [128, 128], BF16)
        onesf = sb("onesf", [128, 128])
        iota16 = sb("iota16", [128, 16])
        xt = [sb("xt%d" % i, [128, D]) for i in range(4)]
        hnb = sb("hnb", [128, D], BF16)
        ss = sb("ss", [128, 8])
        hnT = sb("hnT", [128, 8, G], BF16)
        cgs = sb("cgs", [128, G])
        zb = [sb("z%d" % k, [128, HALO + G]) for k in range(4)]
        mixA = sb("mixA", [128, 2048])
        ysc = [mixA[:, k * G:(k + 1) * G] for k in range(4)]
        sq = [sb("sq%d" % k, [128, G]) for k in range(2)]
        ub = [sb("u%d" % k, [128, HALO + G], BF16) for k in range(4)]
        dgs = [sb("dg%d" % k, [128, 128], BF16) for k in range(4)]
        cv = [mixA[:, 1024 + k * G:1024 + (k + 1) * G] for k in range(4)]
        mt = sb("mt", [128, G])
        rt = sb("rt", [128, G])
        qTb = sb("qTb", [128, 1024], BF16)
        Ssb = sb("Ssb", [128, 1024])
        S2 = sb("S2", [128, 256])
        Vt = sb("Vt", [128, 256])
        It = sb("It", [128, 256], U32)
        cand = mixA
        CV = sb("CV", [128, 128])
        CP = sb("CP", [128, 128], U32)
        au = sb("au", [128, 128], U32)
        af = sb("af", [128, 128])
        bf = sb("bf", [128, 128])
        idx2 = [sb("idx%d" % i, [128, 128], U32) for i in range(2)]
        ex = sb("ex", [128, 128])
        Zs = sb("Zs", [128, 8])
        gates2 = [sb("gates%d" % i, [128, 128]) for i in range(2)]
        apre = sb("apre", [128, 128])
        diag = [sb("diag%d" % i, [128, 128], BF16) for i in range(4)]
        GS = [sb("GS%d" % i, [128, 2 * D], BF16) for i in range(NS)]
        yn = [sb("yn%d" % k, [128, G], BF16) for k in range(8)]
        psA = st.enter_context(nc.psum_tensor("psA", [128, 6 * 512], F32))
        psT = st.enter_context(nc.psum_tensor("psT", [128, 1024], BF16))
        pslot = lambda s_, n: psA[:, 1024 + s_ * 256:1024 + s_ * 256 + n]
        bank = lambda i, n=512: psA[:, i * 512:i * 512 + n]
        psT3 = psT[:].rearrange("p (c t) -> p c t", c=8)
        prod = [sb("prod%d" % i, [128, D], BF16) for i in range(2)]
        hbf = [sb("hbf%d" % i, [128, D], BF16) for i in range(2)]
        junk = prod[1][:, :]
        JW = ["prod1"]
        JR = []

        vcol = lambda c: vecs[:, c:c + 1]
        epsc = ss[:, 7:8]
        P.op("vector", lambda e: e.memset(epsc, EPS), writes=["epsc"])
        P.alias["cand"] = ["ysc%d" % k for k in range(4)] + ["cv%d" % k for k in range(4)]
        P.alias["mixlo"] = ["ysc%d" % k for k in range(4)]
        P.alias["mixhi"] = ["cv%d" % k for k in range(4)]
        P.alias["b2"] = ["p0", "p1"]
        P.alias["b3"] = ["p2", "p3"]

        P.dma("sync", lambda e: e.dma_start(out=vecs[:], in_=vecs_d), "ldv", writes=["vecs"])
        P.dma("sync", lambda e: e.dma_start(out=gB[:], in_=gB_d[:, D:2 * D]), "ldg", writes=["gB"])
        P.dma("sync", lambda e: e.dma_start(out=xt[0][:, :], in_=gB_d[:, 0:D]), "ldx0", writes=["xt0"])
        P.op("gpsimd", lambda e: e.memset(apre[:], 1.0), writes=["apre_setup"])
        P.op("gpsimd", lambda e: e.affine_select(out=apre[:], in_=apre[:], pattern=[[-1, 128]], compare_op=ALU.is_equal,
                                                  fill=0.0, base=0, channel_multiplier=1), reads=["apre_setup"], writes=["apre_setup"])
        P.op("vector", lambda e: e.tensor_copy(out=identb[:], in_=apre[:]), reads=["apre_setup"], writes=["identb"])
        P.op("gpsimd", lambda e: e.memset(onesf[:], 1.0), writes=["onesf"])
        P.op("gpsimd", lambda e: e.iota(iota16[:], pattern=[[1, 16]], base=0, channel_multiplier=0,
                                        allow_small_or_imprecise_dtypes=True), writes=["iota16"])
        stage = [(mixA[:, 0:1024], "mixlo"), (mixA[:, 1024:2048], "mixhi"), (xt[1][:, :], "xt1"), (xt[2][:, :], "xt2")]
        si = 0
        for (Wd, Wb, ncol, gcol, nm) in ((w_in, Wib, 2560, GM, "Wib"), (w_out, Wob, 1024, GO, "Wob"), (w_q, Wqb, 2048, GF, "Wqb")):
            for c in range(8):
                for c0 in range(0, ncol, 1024):
                    w = min(1024, ncol - c0)
                    sbuf_, rn = stage[si % 4]
                    si += 1
                    P.dma("sync" if si % 2 else "scalar",
                          lambda e, sbuf_=sbuf_, Wd=Wd, c=c, c0=c0, w=w: e.dma_start(out=sbuf_[:, 0:w], in_=Wd[c * 128:(c + 1) * 128, c0:c0 + w]),
                          "ld" + rn, writes=[rn])
                    if si % 2:
                        P.op("vector", lambda e, sbuf_=sbuf_, Wb=Wb, c=c, c0=c0, w=w, gcol=gcol: e.tensor_scalar(
                            out=Wb[:, c, c0:c0 + w], in0=sbuf_[:, 0:w], scalar1=vcol(gcol + c), scalar2=None, op0=ALU.mult),
                            reads=[rn, "vecs"], writes=[nm])
                    else:
                        P.op("scalar", lambda e, sbuf_=sbuf_, Wb=Wb, c=c, c0=c0, w=w, gcol=gcol: e.activation(
                            out=Wb[:, c, c0:c0 + w], in_=sbuf_[:, 0:w], func=AF.Copy, scale=vcol(gcol + c)),
                            reads=[rn, "vecs"], writes=[nm])
        for c0 in range(0, 2048, 1024):
            sbuf_, rn = stage[si % 4]
            si += 1
            P.dma("sync", lambda e, sbuf_=sbuf_, c0=c0: e.dma_start(out=sbuf_[:, :], in_=keysT[:, c0:c0 + 1024]), "ld" + rn, writes=[rn])
            P.op("vector", lambda e, sbuf_=sbuf_, c0=c0: e.tensor_copy(out=keysb[:].rearrange("p g n -> p (g n)")[:, c0:c0 + 1024], in_=sbuf_[:, :]),
                 reads=[rn], writes=["keysb"])
        GSres = ["GS%d" % i for i in range(NS)]
        TBres = ["tab_b%d" % i for i in range(NS)]
        ustage = [(mixA[:, 0:1024], "mixlo"), (xt[1][:, :], "xt1"), (xt[3][:, :], "xt3")]
        vstage = [(mixA[:, 1024:2048], "mixhi"), (xt[2][:, :], "xt2")]
        for it in range(128):
            su_, ru = ustage[it % 3]
            sv_, rv = vstage[it % 2]
            gs_ = it % NS
            P.dma("sync", lambda e, su_=su_, it=it: e.dma_start(out=su_[:, :], in_=uv_tab[it * 128:(it + 1) * 128, 0:D]), "ldu" + ru, writes=[ru])
            P.dma("sync", lambda e, sv_=sv_, it=it: e.dma_start(out=sv_[:, :], in_=uv_tab[it * 128:(it + 1) * 128, D:2 * D]), "ldv" + rv, writes=[rv])
            P.op("vector", lambda e, su_=su_, gs_=gs_: e.tensor_tensor(out=GS[gs_][:, 0:D], in0=su_[:, :], in1=xt[0][:, :], op=ALU.mult),
                 reads=[ru, "xt0"], writes=[GSres[gs_] + "u"])
            P.op("scalar", lambda e, sv_=sv_, gs_=gs_: e.activation(out=GS[gs_][:, D:2 * D], in_=sv_[:, :], func=AF.Copy), reads=[rv], writes=[GSres[gs_] + "v"])
            P.dma("scalar", lambda e, gs_=gs_, it=it: e.dma_start(out=tab_b[it * 128:(it + 1) * 128, :], in_=GS[gs_][:, :]),
                  "st" + GSres[gs_], reads=[GSres[gs_] + "u", GSres[gs_] + "v", GSres[gs_]], writes=[TBres[gs_]])

        def rstd_from_ss(ss_ap, n, res, scale):
            P.op("scalar", lambda e: e.activation(out=ss_ap, in_=ss_ap, func=AF.Sqrt, scale=scale, bias=epsc[0:n, 0:1]), reads=[res, "epsc"], writes=[res])
            P.op("vector", lambda e: e.reciprocal(out=ss_ap, in_=ss_ap), reads=[res], writes=[res])

        def bcast_rstd(src_bank, src_res, dst, dst_res, N, scale):
            P.op("scalar", lambda e: e.activation(out=dst[:, 0:N], in_=src_bank[:, 0:N], func=AF.Sqrt, scale=scale, bias=epsc[:, 0:1]),
                 reads=[src_res, "epsc"], writes=[dst_res])
            P.op("vector", lambda e: e.reciprocal(out=dst[:, 0:N], in_=dst[:, 0:N]), reads=[dst_res], writes=[dst_res])

        def front(tiles, N, xs, preloaded=False):
            off = 0
            for i, (r0, n) in enumerate(tiles):
                xr = "xt%d" % xs[i]
                xi = xt[xs[i]]
                if not preloaded:
                    P.dma("sync", lambda e, xi=xi, i=i, r0=r0, n=n: e.dma_start(out=xi[0:n, :], in_=xh[r0:r0 + n, :]), "ldx%d" % xs[i], writes=[xr])
                ssr = "ss%d" % i
                P.op("scalar", lambda e, xi=xi, i=i, n=n: e.activation(out=junk[0:n, :], in_=xi[0:n, :], func=AF.Square, accum_out=ss[0:n, i:i + 1]),
                     reads=[xr], writes=[ssr] + JW)
                rstd_from_ss(ss[0:n, i:i + 1], n, ssr, 1.0 / D)
                P.op("scalar", lambda e, xi=xi, i=i, n=n: e.activation(out=hnb[0:n, :], in_=xi[0:n, :], func=AF.Copy, scale=ss[0:n, i:i + 1]),
                     reads=[xr, ssr], writes=["hnb"])
                for c in range(8):
                    P.op("tensor", lambda e, c=c, n=n: e.transpose(out=psT3[:, c, 0:n], in_=hnb[0:n, c * 128:(c + 1) * 128], identity=identb[0:n, 0:n]),
                         reads=["hnb", "identb"], writes=["bT"])
                P.op("vector", lambda e, n=n, off=off: e.tensor_copy(out=hnT[:, :, off:off + n], in_=psT3[:, :, 0:n]), reads=["bT"], writes=["hnT"])
                off += n

        def proj(col, b, N):
            for k in range(8):
                P.op("tensor", lambda e, k=k, col=col, b=b, N=N: e.matmul(pslot(b, N), lhsT=Wib[:, k, col * 128:(col + 1) * 128], rhs=hnT[:, k, 0:N],
                                                                            start=(k == 0), stop=(k == 7)),
                     reads=["Wib", "hnT"], writes=["p%d" % b])

        def mixer_pre():
            N = HALO
            front([(0, HALO)], N, [0])
            for k in range(4):
                proj(k, 0, N)
                proj(8 + k, 1, N)
                P.op("scalar", lambda e: e.activation(out=cgs[:, 0:N], in_=pslot(1, N), func=AF.Copy), reads=["p1"], writes=["cgs"])
                P.op("vector", lambda e, k=k: e.tensor_tensor(out=zb[k][:, 0:N], in0=pslot(0, N), in1=cgs[:, 0:N], op=ALU.mult),
                     reads=["p0", "cgs"], writes=["z%d" % k])
                proj(12 + k, 0, N)
                proj(16 + k, 1, N)
                P.op("scalar", lambda e: e.activation(out=cgs[:, 0:N], in_=pslot(1, N), func=AF.Sigmoid), reads=["p1"], writes=["cgs"])
                P.op("vector", lambda e, k=k: e.tensor_tensor(out=ub[k][:, 0:N], in0=pslot(0, N), in1=cgs[:, 0:N], op=ALU.mult),
                     reads=["p0", "cgs"], writes=["u%d" % k])

        def mixer_group(g):
            N = G
            base = HALO + g * G
            tiles = [(base, 128), (base + 128, 128)]
            xs = [2 * (g % 2), 2 * (g % 2) + 1]
            front(tiles, N, xs, preloaded=(g > 0))
            for k in range(4):
                proj(k, 0, N)
                proj(8 + k, 1, N)
                proj(4 + k, 2, N)
                zr = "z%d" % k
                P.op("scalar", lambda e: e.activation(out=cgs[:, 0:N], in_=pslot(1, N), func=AF.Copy), reads=["p1"], writes=["cgs"])
                if g > 0:
                    P.op("vector", lambda e, k=k: e.tensor_copy(out=zb[k][:, 0:HALO], in_=zb[k][:, G:G + HALO]), reads=[zr], writes=[zr])
                P.op("vector", lambda e, k=k: e.tensor_tensor(out=zb[k][:, HALO:HALO + N], in0=pslot(0, N), in1=cgs[:, 0:N], op=ALU.mult),
                     reads=["p0", "cgs"], writes=[zr])
                P.op("vector", lambda e, k=k: e.tensor_scalar(out=rt[:, 0:N], in0=zb[k][:, HALO - 2:HALO - 2 + N], scalar1=vcol(SCW + 3 * k), scalar2=None, op0=ALU.mult),
                     reads=[zr, "vecs"], writes=["rt"])
                for j in (1, 2):
                    P.op("vector", lambda e, k=k, j=j: e.scalar_tensor_tensor(out=rt[:, 0:N], in0=zb[k][:, HALO - 2 + j:HALO - 2 + j + N], scalar=vcol(SCW + 3 * k + j),
                                                                              in1=rt[:, 0:N], op0=ALU.mult, op1=ALU.add),
                         reads=[zr, "vecs", "rt"], writes=["rt"])
                yr = "ysc%d" % k
                P.op("vector", lambda e, k=k: e.tensor_tensor(out=ysc[k][:, 0:N], in0=pslot(2, N), in1=rt[:, 0:N], op=ALU.mult),
                     reads=["p2", "rt"], writes=[yr])
                sr = "sq%d" % (k % 2)
                P.op("scalar", lambda e, k=k: e.activation(out=sq[k % 2][:, 0:N], in_=ysc[k][:, 0:N], func=AF.Square), reads=[yr], writes=[sr])
                P.op("tensor", lambda e, k=k: e.matmul(bank(4, N), lhsT=onesf[:], rhs=sq[k % 2][:, 0:N], start=(k == 0), stop=(k == 3)),
                     reads=[sr, "onesf"], writes=["b4"])
            bcast_rstd(bank(4), "b4", mt, "mt", N, 1.0 / 512)
            for k in range(4):
                P.op("vector", lambda e, k=k: e.tensor_tensor(out=yn[k][:, 0:N], in0=ysc[k][:, 0:N], in1=mt[:, 0:N], op=ALU.mult),
                     reads=["ysc%d" % k, "mt"], writes=["yn%d" % k])
            if P.defer is not None:
                P.mark = len(P.defer)
            for k in range(4):
                proj(12 + k, 0, N)
                proj(16 + k, 1, N)
                ur = "u%d" % k
                cr = "cv%d" % k
                P.op("scalar", lambda e: e.activation(out=cgs[:, 0:N], in_=pslot(1, N), func=AF.Sigmoid), reads=["p1"], writes=["cgs"])
                if g > 0:
                    P.op("vector", lambda e, k=k: e.tensor_copy(out=ub[k][:, 0:HALO], in_=ub[k][:, G:G + HALO]), reads=[ur], writes=[ur])
                P.op("vector", lambda e, k=k: e.tensor_tensor(out=ub[k][:, HALO:HALO + N], in0=pslot(0, N), in1=cgs[:, 0:N], op=ALU.mult),
                     reads=["p0", "cgs"], writes=[ur])
                for j in range(31):
                    dn = (k * 31 + j) % len(dgs)
                    dr_ = "dg%d" % dn
                    if j % 2:
                        P.op("vector", lambda e, k=k, j=j, dn=dn: e.tensor_tensor(out=dgs[dn][:, :], in0=identb[:, :],
                                                                                 in1=vcol(CFW + 31 * k + j).to_broadcast([128, 128]), op=ALU.mult),
                             reads=["identb", "vecs"], writes=[dr_])
                    else:
                        P.op("scalar", lambda e, k=k, j=j, dn=dn: e.activation(out=dgs[dn][:, :], in_=identb[:, :], func=AF.Copy, scale=vcol(CFW + 31 * k + j)),
                             reads=["identb", "vecs"], writes=[dr_])
                    P.op("tensor", lambda e, k=k, j=j, dn=dn: e.matmul(pslot(3, N), lhsT=dgs[dn][:, :], rhs=ub[k][:, 2 + j:2 + j + N], start=(j == 0), stop=(j == 30)),
                         reads=[dr_, ur], writes=["p3"])
                P.op("vector", lambda e, k=k: e.tensor_scalar(out=cv[k][:, 0:N], in0=pslot(3, N), scalar1=vcol(CFB + k), scalar2=None, op0=ALU.add),
                     reads=["p3", "vecs"], writes=[cr])
                sr = "sq%d" % (k % 2)
                P.op("scalar", lambda e, k=k: e.activation(out=sq[k % 2][:, 0:N], in_=cv[k][:, 0:N], func=AF.Square), reads=[cr], writes=[sr])
                P.op("tensor", lambda e, k=k: e.matmul(bank(4, N), lhsT=onesf[:], rhs=cv[k][:, 0:N], start=(k == 0), stop=(k == 3)),
                     reads=[cr, "onesf"], writes=["b4"])
                P.op("tensor", lambda e, k=k: e.matmul(bank(5, N), lhsT=onesf[:], rhs=sq[k % 2][:, 0:N], start=(k == 0), stop=(k == 3)),
                     reads=[sr, "onesf"], writes=["b5"])
            P.op("vector", lambda e: e.tensor_scalar(out=mt[:, 0:N], in0=bank(4, N), scalar1=1.0 / 512, scalar2=None, op0=ALU.mult), reads=["b4"], writes=["mt"])
            P.op("vector", lambda e: e.tensor_tensor(out=sq[0][:, 0:N], in0=mt[:, 0:N], in1=mt[:, 0:N], op=ALU.mult), reads=["mt"], writes=["sq0"])
            P.op("vector", lambda e: e.scalar_tensor_tensor(out=rt[:, 0:N], in0=bank(5, N), scalar=1.0 / 512, in1=sq[0][:, 0:N], op0=ALU.mult, op1=ALU.subtract),
                 reads=["b5", "sq0"], writes=["rt"])
            P.op("vector", lambda e: e.tensor_scalar(out=rt[:, 0:N], in0=rt[:, 0:N], scalar1=EPS, scalar2=None, op0=ALU.add), reads=["rt"], writes=["rt"])
            P.op("scalar", lambda e: e.activation(out=rt[:, 0:N], in_=rt[:, 0:N], func=AF.Sqrt), reads=["rt"], writes=["rt"])
            P.op("vector", lambda e: e.reciprocal(out=rt[:, 0:N], in_=rt[:, 0:N]), reads=["rt"], writes=["rt"])
            for k in range(4):
                cr = "cv%d" % k
                P.op("vector", lambda e, k=k: e.tensor_tensor(out=cv[k][:, 0:N], in0=cv[k][:, 0:N], in1=mt[:, 0:N], op=ALU.subtract), reads=[cr, "mt"], writes=[cr])
                P.op("vector", lambda e, k=k: e.tensor_tensor(out=cv[k][:, 0:N], in0=cv[k][:, 0:N], in1=rt[:, 0:N], op=ALU.mult), reads=[cr, "rt"], writes=[cr])
                P.op("scalar", lambda e, k=k: e.activation(out=cv[k][:, 0:N], in_=cv[k][:, 0:N], func=AF.Silu, scale=vcol(LNG + k), bias=vcol(LNB + k)),
                     reads=[cr, "vecs"], writes=[cr])
                sr = "sq%d" % (k % 2)
                P.op("scalar", lambda e, k=k: e.activation(out=sq[k % 2][:, 0:N], in_=cv[k][:, 0:N], func=AF.Square), reads=[cr], writes=[sr])
                P.op("tensor", lambda e, k=k: e.matmul(bank(4, N), lhsT=onesf[:], rhs=sq[k % 2][:, 0:N], start=(k == 0), stop=(k == 3)),
                     reads=[sr, "onesf"], writes=["b4"])
            bcast_rstd(bank(4), "b4", mt, "mt", N, 1.0 / 512)
            for k in range(4):
                P.op("vector", lambda e, k=k: e.tensor_tensor(out=yn[4 + k][:, 0:N], in0=cv[k][:, 0:N], in1=mt[:, 0:N], op=ALU.mult),
                     reads=["cv%d" % k, "mt"], writes=["yn%d" % (4 + k)])
            for i in range(2):
                for half in range(2):
                    for kk in range(8):
                        P.op("tensor", lambda e, i=i, half=half, kk=kk: e.matmul(psA[:, (2 + half) * 512:(3 + half) * 512], lhsT=yn[kk][:, i * 128:(i + 1) * 128],
                                                                                  rhs=Wob[:, kk, half * 512:(half + 1) * 512], start=(kk == 0), stop=(kk == 7)),
                             reads=["yn%d" % kk, "Wob"], writes=["b%d" % (2 + half)])
                P.op("vector", lambda e, i=i: e.tensor_tensor(out=xt[xs[i]][:, :], in0=psA[:, 2 * 512:4 * 512], in1=xt[xs[i]][:, :], op=ALU.add),
                     reads=["b2", "b3", "xt%d" % xs[i]], writes=["xt%d" % xs[i]])

        def route(i, x_):
            xr = "xt%d" % x_
            h2 = xt[x_]
            idx = idx2[i]
            gates = gates2[i]
            ssc = ss[:, 2 + i:3 + i]
            ssr2 = "ss2_%d" % i
            idxr = "idx%d" % i
            gatesr = "gates%d" % i
            P.op("scalar", lambda e: e.activation(out=junk[:, :], in_=h2[:, :], func=AF.Square, accum_out=ssc), reads=[xr], writes=[ssr2] + JW)
            rstd_from_ss(ssc, 128, ssr2, 1.0 / D)
            P.op("scalar", lambda e: e.activation(out=hbf[i][:, :], in_=h2[:, :], func=AF.Copy, scale=ssc), reads=[xr, ssr2], writes=["hbf%d" % i])
            for c in range(8):
                P.op("tensor", lambda e, c=c: e.transpose(out=psT3[:, c, :], in_=hbf[i][:, c * 128:(c + 1) * 128], identity=identb[:, :]),
                     reads=["hbf%d" % i, "identb"], writes=["bT"])
            P.op("vector", lambda e: e.tensor_copy(out=hnT[:, :, 0:128], in_=psT3[:, :, :]), reads=["bT"], writes=["hnT"])
            for hf in range(2):
                for gg in range(8):
                    g = hf * 8 + gg
                    for k in range(8):
                        P.op("tensor", lambda e, g=g, gg=gg, k=k: e.matmul(psA[:, 2048 + gg * 128:2048 + (gg + 1) * 128], lhsT=Wqb[:, k, g * 128:(g + 1) * 128],
                                                                        rhs=hnT[:, k, 0:128], start=(k == 0), stop=(k == 7)),
                             reads=["Wqb", "hnT"], writes=["b%d" % (4 + gg // 4)])
                P.op("scalar", lambda e, hf=hf: e.activation(out=qTb[:, :], in_=psA[:, 2048:3072], func=AF.Copy), reads=["b4", "b5"], writes=["qTb"])
                for gg in range(8):
                    g = hf * 8 + gg
                    P.op("tensor", lambda e, g=g, gg=gg: e.matmul(psA[:, 2048 + gg * 128:2048 + (gg + 1) * 128], lhsT=qTb[:, gg * 128:(gg + 1) * 128], rhs=keysb[:, g, :],
                                                               start=True, stop=True),
                         reads=["qTb", "keysb"], writes=["b%d" % (4 + gg // 4)])
                P.op("scalar", lambda e: e.activation(out=Ssb[:, :], in_=psA[:, 2048:3072], func=AF.Copy), reads=["b4", "b5"], writes=["Ssb"])
                for gg in range(8):
                    g = hf * 8 + gg
                    sg_ = Ssb[:, gg * 128:(gg + 1) * 128]
                    v0 = Vt[:, g * 16:g * 16 + 8]
                    v1 = Vt[:, g * 16 + 8:g * 16 + 16]
                    i0 = It[:, g * 16:g * 16 + 8]
                    i1 = It[:, g * 16 + 8:g * 16 + 16]
                    P.op("vector", lambda e, sg_=sg_, v0=v0: e.max(out=v0, in_=sg_), reads=["Ssb"], writes=["Vt"])
                    P.op("vector", lambda e, sg_=sg_, v0=v0, i0=i0: e.max_index(out=i0, in_max=v0, in_values=sg_), reads=["Ssb", "Vt"], writes=["It"])
                    P.op("vector", lambda e, sg_=sg_, v0=v0: e.match_replace(out=S2[:, 0:128], in_to_replace=v0, in_values=sg_, imm_value=-1e30),
                         reads=["Ssb", "Vt"], writes=["S2"])
                    P.op("vector", lambda e, v1=v1: e.max(out=v1, in_=S2[:, 0:128]), reads=["S2"], writes=["Vt"])
                    P.op("vector", lambda e, v1=v1, i1=i1: e.max_index(out=i1, in_max=v1, in_values=S2[:, 0:128]), reads=["S2", "Vt"], writes=["It"])
            Vt4 = Vt[:].rearrange("p (h s a) -> p h s a", h=8, s=2)
            cand4 = cand[:].rearrange("p (h a b) -> p h a b", h=8, a=16)
            P.op("vector", lambda e: e.tensor_tensor(out=cand4, in0=Vt4[:, :, 0, :].unsqueeze(3).to_broadcast([128, 8, 16, 16]),
                                                      in1=Vt4[:, :, 1, :].unsqueeze(2).to_broadcast([128, 8, 16, 16]), op=ALU.add),
                 reads=["Vt"], writes=["cand"])
            for h in range(8):
                ch = cand[:, h * 256:(h + 1) * 256]
                v0 = CV[:, h * 16:h * 16 + 8]
                v1 = CV[:, h * 16 + 8:h * 16 + 16]
                p0 = CP[:, h * 16:h * 16 + 8]
                p1 = CP[:, h * 16 + 8:h * 16 + 16]
                P.op("vector", lambda e, ch=ch, v0=v0: e.max(out=v0, in_=ch), reads=["cand"], writes=["CV"])
                P.op("vector", lambda e, ch=ch, v0=v0, p0=p0: e.max_index(out=p0, in_max=v0, in_values=ch), reads=["cand", "CV"], writes=["CP"])
                P.op("vector", lambda e, ch=ch, v0=v0: e.match_replace(out=S2[:, :], in_to_replace=v0, in_values=ch, imm_value=-1e30),
                     reads=["cand", "CV"], writes=["S2"])
                P.op("vector", lambda e, v1=v1: e.max(out=v1, in_=S2[:, :]), reads=["S2"], writes=["CV"])
                P.op("vector", lambda e, v1=v1, p1=p1: e.max_index(out=p1, in_max=v1, in_values=S2[:, :]), reads=["S2", "CV"], writes=["CP"])
            CV3 = CV[:].rearrange("p (h k) -> p h k", h=8)
            ex3 = ex[:].rearrange("p (h k) -> p h k", h=8)
            g3 = gates[:].rearrange("p (h k) -> p h k", h=8)
            P.op("vector", lambda e: e.tensor_tensor(out=ex3, in0=CV3, in1=CV3[:, :, 0:1].to_broadcast([128, 8, 16]), op=ALU.subtract), reads=["CV"], writes=["ex"])
            P.op("scalar", lambda e: e.activation(out=ex[:, :], in_=ex[:, :], func=AF.Exp), reads=["ex"], writes=["ex"])
            P.op("vector", lambda e: e.tensor_reduce(out=Zs[:, 0:8], in_=ex3, axis=AX.X, op=ALU.add), reads=["ex"], writes=["Zs"])
            P.op("vector", lambda e: e.reciprocal(out=Zs[:, 0:8], in_=Zs[:, 0:8]), reads=["Zs"], writes=["Zs"])
            P.op("vector", lambda e: e.tensor_tensor(out=g3, in0=ex3, in1=Zs[:, 0:8].unsqueeze(2).to_broadcast([128, 8, 16]), op=ALU.mult),
                 reads=["ex", "Zs"], writes=[gatesr])
            P.op("vector", lambda e: e.tensor_single_scalar(out=au[:, :], in_=CP[:, :], scalar=4, op=ALU.logical_shift_right), reads=["CP"], writes=["au"])
            P.op("vector", lambda e: e.tensor_single_scalar(out=CP[:, :], in_=CP[:, :], scalar=15, op=ALU.bitwise_and), reads=["CP", "au"], writes=["CP"])
            P.op("vector", lambda e: e.tensor_copy(out=af[:, :], in_=au[:, :]), reads=["au"], writes=["af"])
            P.op("vector", lambda e: e.tensor_copy(out=bf[:, :], in_=CP[:, :]), reads=["CP"], writes=["bf"])
            P.op("vector", lambda e: e.tensor_copy(out=S2[:, :], in_=It[:, :]), reads=["It"], writes=["S2"])
            Itf4 = S2[:].rearrange("p (h s a) -> p h s a", h=8, s=2)
            oh4 = Ssb[:].rearrange("p (h k a) -> p h k a", h=4, k=16)
            io4 = iota16[:, :].unsqueeze(1).unsqueeze(1).to_broadcast([128, 4, 16, 16])
            for (srcf, s_, sres) in ((af, 0, "af"), (bf, 1, "bf")):
                s3 = srcf[:].rearrange("p (h k) -> p h k", h=8)
                for hh in range(2):
                    hs = slice(hh * 4, hh * 4 + 4)
                    P.op("vector", lambda e, s3=s3, hs=hs: e.tensor_tensor(out=oh4, in0=s3[:, hs, :].unsqueeze(3).to_broadcast([128, 4, 16, 16]), in1=io4, op=ALU.is_equal),
                         reads=[sres, "iota16"], writes=["Ssb"])
                    P.op("vector", lambda e, s_=s_, hs=hs: e.tensor_tensor(out=oh4, in0=oh4, in1=Itf4[:, hs, s_, :].unsqueeze(2).to_broadcast([128, 4, 16, 16]), op=ALU.mult),
                         reads=["Ssb", "S2"], writes=["Ssb"])
                    P.op("vector", lambda e, s3=s3, hs=hs: e.tensor_reduce(out=s3[:, hs, :], in_=oh4, axis=AX.X, op=ALU.add), reads=["Ssb"], writes=[sres])
            P.op("vector", lambda e: e.scalar_tensor_tensor(out=af[:, :], in0=af[:, :], scalar=128.0, in1=bf[:, :], op0=ALU.mult, op1=ALU.add),
                 reads=["af", "bf"], writes=["af"])
            P.op("vector", lambda e: e.tensor_copy(out=idx[:, :], in_=af[:, :]), reads=["af"], writes=[idxr])

        def experts(i, x_, orow, filler=None):
            xr = "xt%d" % x_
            h2 = xt[x_]
            rate = (len(filler) // n_peer_j + 1) if filler else 0
            idx = idx2[i]
            gates = gates2[i]
            ssc = ss[:, 2 + i:3 + i]
            ssr2 = "ss2_%d" % i
            idxr = "idx%d" % i
            gatesr = "gates%d" % i

            def fill(n):
                for _ in range(n):
                    if filler:
                        filler.pop(0)()
            nj = n_peer_j
            nb = nj // JB

            nj = n_peer_j
            def gather(j):
                s_ = j % NS
                P.dma("gpsimd", lambda e: e.indirect_dma_start(out=GS[s_][:, :], out_offset=None, in_=tab_b,
                                                               in_offset=bass.IndirectOffsetOnAxis(ap=idx[:, j:j + 1], axis=0)),
                      "g" + GSres[s_], reads=[idxr] + TBres, writes=[GSres[s_]])

            def dot(j):
                s_ = j % NS
                p_ = j % 2
                P.op("vector", lambda e: e.tensor_tensor(out=prod[p_][:, :], in0=GS[s_][:, 0:D], in1=hbf[i][:, :], op=ALU.mult),
                     reads=["hbf%d" % i], writes=["prod%d" % p_], weak=[GSres[s_]])
                P.op("scalar", lambda e: e.activation(out=prod[p_][:, :], in_=prod[p_][:, :], func=AF.Copy, accum_out=apre[:, j:j + 1]),
                     reads=["prod%d" % p_], writes=["prod%d" % p_, "apre%d" % j])

            def gelu(j):
                P.op("scalar", lambda e: e.activation(out=apre[:, j:j + 1], in_=apre[:, j:j + 1], func=AF.Gelu_apprx_tanh),
                     reads=["apre%d" % j], writes=["apre%d" % j])

            def acc(j):
                s_ = j % NS
                d_ = j % len(diag)
                P.op("vector", lambda e: e.scalar_tensor_tensor(out=diag[d_][:, :], in0=identb[:, :], scalar=apre[:, j:j + 1],
                                                                in1=gates[:, j:j + 1].to_broadcast([128, 128]), op0=ALU.mult, op1=ALU.mult),
                     reads=["identb", "apre%d" % j, gatesr], writes=["diag%d" % d_])
                for half in range(2):
                    P.op("tensor", lambda e, half=half: e.matmul(psA[:, half * 512:(half + 1) * 512], lhsT=diag[d_][:, :],
                                                                 rhs=GS[s_][:, D + half * 512:D + (half + 1) * 512], start=(j == 0), stop=(j == nj - 1)),
                         reads=["diag%d" % d_, GSres[s_]], writes=["b%d" % half])

            for t in range(nj + 4):
                if t < nj:
                    gather(t)
                if 0 <= t - 1 < nj:
                    dot(t - 1)
                if 0 <= t - 2 < nj:
                    gelu(t - 2)
                if 0 <= t - 4 < nj:
                    acc(t - 4)
                fill(rate)
            fill(100000)
            P.op("vector", lambda e: e.tensor_tensor(out=h2[:, :], in0=psA[:, 0:1024], in1=h2[:, :], op=ALU.add), reads=["b0", "b1", xr], writes=[xr])
            P.op("scalar", lambda e: e.activation(out=junk[:, :], in_=h2[:, :], func=AF.Square, accum_out=ss[:, 4 + i:5 + i]), reads=[xr], writes=["ss3_%d" % i] + JW)
            rstd_from_ss(ss[:, 4 + i:5 + i], 128, "ss3_%d" % i, 1.0 / D)
            P.op("vector", lambda e: e.scalar_tensor_tensor(out=h2[:, :], in0=h2[:, :], scalar=ss[:, 4 + i:5 + i], in1=gB[:, :], op0=ALU.mult, op1=ALU.mult),
                 reads=[xr, "ss3_%d" % i, "gB"], writes=[xr])
            P.dma("sync", lambda e: e.dma_start(out=out[orow:orow + 128, :], in_=h2[:, :]), "st%d" % x_, reads=[xr], writes=["out%d" % x_])

        mixer_pre()
        mixer_group(0)
        route(0, 0)
        for g in range(n_groups):
            x0 = 2 * (g % 2)
            P.defer = []
            route(1, x0 + 1)
            R1 = P.defer
            M, R0, ms = [], [], 0
            if g + 1 < n_groups:
                for i_ in range(2):
                    xs_ = 2 * ((g + 1) % 2) + i_
                    r0_ = HALO + (g + 1) * G + i_ * 128
                    P._dma("sync", lambda e, xs_=xs_, r0_=r0_: e.dma_start(out=xt[xs_][:, :], in_=xh[r0_:r0_ + 128, :]), "ldx%d" % xs_, writes=["xt%d" % xs_])
                P.defer = []
                P.mark = 0
                mixer_group(g + 1)
                M, ms = P.defer, P.mark
                P.defer = []
                route(0, 2 * ((g + 1) % 2))
                R0 = P.defer
            P.defer = None
            experts(0, x0, g * G, R1)
            experts(1, x0 + 1, g * G + 128, M + R0)
        P.wait_all("sync", ["out0", "out1", "out2", "out3"])
        P.emit()
    return nc


def _prep_shared(inp):
    f = lambda a: np.ascontiguousarray(np.asarray(a, dtype=np.float32))
    col = lambda v, n: np.asarray(v, np.float32).reshape(n, 128).T
    vecs = np.zeros((128, NVEC), np.float32)
    vecs[:, GM:GM + 8] = col(inp["norm_mix_g"][0], 8)
    vecs[:, GF:GF + 8] = col(inp["norm_ffn_g"][0], 8)
    vecs[:, GO:GO + 4] = col(inp["out_norm_g_sc"][0], 4)
    vecs[:, GO + 4:GO + 8] = col(inp["out_norm_g_cf"][0], 4)
    scw = np.asarray(inp["sc_conv_w"][0], np.float32)
    cfw = np.asarray(inp["cf_conv_w"][0], np.float32)
    for k in range(4):
        vecs[:, SCW + 3 * k:SCW + 3 * k + 3] = scw[:, k * 128:(k + 1) * 128].T
        vecs[:, CFW + 31 * k:CFW + 31 * k + 31] = cfw[:, k * 128:(k + 1) * 128].T
    vecs[:, CFB:CFB + 4] = col(inp["cf_conv_b"][0], 4)
    vecs[:, LNG:LNG + 4] = col(inp["cf_ln_g"][0], 4)
    vecs[:, LNB:LNB + 4] = col(inp["cf_ln_b"][0], 4)
    gB = np.concatenate([np.broadcast_to(np.asarray(inp["norm_ffn_g"][0], np.float32)[None, :], (128, D)),
                         np.broadcast_to(np.asarray(inp["final_norm_g"], np.float32)[None, :], (128, D))], axis=1)
    sk = np.asarray(inp["peer_sub_keys"][0], np.float32)
    keysT = np.ascontiguousarray(sk.reshape(16, 128, 128).transpose(2, 0, 1).reshape(128, 2048))
    return {
        "w_in": f(inp["w_in"][0]), "w_out": f(inp["w_out"][0]), "w_q": f(inp["peer_w_q"][0]),
        "keysT": keysT, "uv_tab": np.ascontiguousarray(np.concatenate([f(inp["peer_u"][0]), f(inp["peer_v"][0])], axis=1)),
        "vecs_h": vecs, "gB_h": np.ascontiguousarray(gB),
    }


def _core_x(inp, core, n_groups=16):
    x = np.asarray(inp["x"], np.float32)
    meta = np.asarray(inp["meta_tokens"], np.float32)
    b, half = core // 2, core % 2
    ntok = n_groups * G
    if half == 0:
        halo = np.concatenate([np.zeros((HALO - meta.shape[0], D), np.float32), meta], axis=0)
        body = x[b, 0:ntok]
    else:
        halo = x[b, 4096 - HALO:4096]
        body = x[b, 4096:4096 + ntok]
    return np.ascontiguousarray(np.concatenate([halo, body], axis=0))


def kernel(**inputs):
    shared = _prep_shared(inputs)
    nc = build(16)
    in_maps = []
    for c in range(N_CORES):
        m = dict(shared)
        m["xh"] = _core_x(inputs, c)
        in_maps.append(m)
    res = run_bass_kernel_spmd(nc, in_maps, core_ids=list(range(N_CORES)))
    outp = np.empty((4, 8192, D), np.float32)
    for c in range(N_CORES):
        b, half = c // 2, c % 2
        outp[b, half * 4096:(half + 1) * 4096] = res.results[c]["out"]
    return outp
```

```python
import numpy as np
from contextlib import ExitStack
import concourse.bass as bass
import concourse.mybir as mybir
from concourse.bass_utils import run_bass_kernel_spmd

F32 = mybir.dt.float32
BF16 = mybir.dt.bfloat16
U32 = mybir.dt.uint32
ALU = mybir.AluOpType
AF = mybir.ActivationFunctionType
AX = mybir.AxisListType

D = 1024
G = 256
HALO = 32
N_CORES = 8
EPS = 1e-6
NS = 10
JB = 4
GM, GF, GO, SCW, CFW, CFB, LNG, LNB, NVEC = 0, 8, 16, 24, 36, 160, 164, 168, 172


class Prog:
    ENGS = ("sync", "scalar", "vector", "gpsimd", "tensor")

    def __init__(self, nc, stack):
        self.nc = nc
        self.stack = stack
        self.q = {e: [] for e in self.ENGS}
        self.cnt = {e: 0 for e in self.ENGS}
        self.sem = {e: stack.enter_context(nc.semaphore("c_" + e)) for e in self.ENGS}
        self.seen = {e: {} for e in self.ENGS}
        self.last_w = {}
        self.readers = {}
        self.dsem = {}
        self.dcnt = {}
        self.alias = {}
        self.defer = None

    def _exp(self, names):
        out = []
        for n in names:
            out.extend(self.alias.get(n, (n,)))
        return out

    def _deps(self, e, reads, writes):
        deps = {}

        def add(ev):
            if ev is None:
                return
            k, s, v = ev
            if k not in deps or deps[k][1] < v:
                deps[k] = (s, v)

        for r in reads:
            add(self.last_w.get(r))
        for w in writes:
            rd = self.readers.get(w, ())
            if not rd:
                add(self.last_w.get(w))
            for ev in rd:
                add(ev)
        for k, (s, v) in deps.items():
            if e == "tensor" and k == "e_tensor":
                continue
            if self.seen[e].get(k, 0) < v:
                self.seen[e][k] = v
                self.q[e].append(lambda eng, s=s, v=v: eng.wait_ge(s, v))

    def _commit(self, ev, reads, writes):
        for w in writes:
            self.last_w[w] = ev
            self.readers[w] = []
        for r in reads:
            if r not in writes:
                self.readers.setdefault(r, []).append(ev)

    def op(self, e, fn, reads=(), writes=(), weak=()):
        if self.defer is not None:
            self.defer.append((e, lambda: self._op(e, fn, reads, writes, weak),
                               set(self._exp(reads)) | set(self._exp(weak)), set(self._exp(writes))))
            return
        self._op(e, fn, reads, writes, weak)

    def _op(self, e, fn, reads=(), writes=(), weak=()):
        reads, writes, weak = self._exp(reads), self._exp(writes), self._exp(weak)
        self._deps(e, list(reads) + list(weak), writes)
        self.cnt[e] += 1
        n = self.cnt[e]
        s = self.sem[e]
        self.q[e].append(lambda eng, fn=fn, s=s: fn(eng).then_inc(s, 1))
        self._commit(("e_" + e, s, n), reads, writes)

    def dma(self, e, fn, key, reads=(), writes=()):
        if self.defer is not None:
            self.defer.append((e, lambda: self._dma(e, fn, key, reads, writes), set(self._exp(reads)), set(self._exp(writes))))
            return
        self._dma(e, fn, key, reads, writes)

    def _dma(self, e, fn, key, reads=(), writes=()):
        reads, writes = self._exp(reads), self._exp(writes)
        self._deps(e, reads, writes)
        if key not in self.dsem:
            self.dsem[key] = self.stack.enter_context(self.nc.semaphore("d_" + key))
            self.dcnt[key] = 0
        s = self.dsem[key]
        self.dcnt[key] += 16
        v = self.dcnt[key]
        self.q[e].append(lambda eng, fn=fn, s=s: fn(eng).then_inc(s, 16))
        self._commit(("d_" + key, s, v), reads, writes)

    def wait_all(self, e, resources):
        self._deps(e, self._exp(resources), ())

    def emit(self):
        with self.nc.Block() as block:
            @block.sync
            def _(eng):
                for f in self.q["sync"]:
                    f(eng)

            @block.scalar
            def _(eng):
                for f in self.q["scalar"]:
                    f(eng)

            @block.vector
            def _(eng):
                for f in self.q["vector"]:
                    f(eng)

            @block.gpsimd
            def _(eng):
                for f in self.q["gpsimd"]:
                    f(eng)

            @block.tensor
            def _(eng):
                for f in self.q["tensor"]:
                    f(eng)


PE_BANK = {"p0": "B2", "p1": "B2", "p2": "B3", "p3": "B3", "b0": "B0", "b1": "B1", "b4": "B4", "b5": "B5", "bT": "BT"}


def reorder(entries, dmin=6):
    units = []
    for (e, th, rd, wr) in entries:
        rd, wr = set(rd), set(wr)
        if e == "tensor":
            wr |= {"pe_" + PE_BANK[w] for w in wr if w in PE_BANK}
            if units and units[-1]["pe"]:
                u = units[-1]
                u["th"].append((e, th)); u["rd"] |= rd; u["wr"] |= wr
                continue
        units.append({"pe": e == "tensor", "th": [(e, th)], "rd": rd, "wr": wr})
    n = len(units)
    preds = [set() for _ in range(n)]
    succs = [set() for _ in range(n)]
    last_w, readers = {}, {}
    for i, u in enumerate(units):
        for r in u["rd"]:
            if r in last_w:
                preds[i].add(last_w[r])
        for w in u["wr"]:
            if w in last_w:
                preds[i].add(last_w[w])
            preds[i].update(readers.get(w, ()))
        for w in u["wr"]:
            last_w[w] = i
            readers[w] = []
        for r in u["rd"]:
            if r not in u["wr"]:
                readers.setdefault(r, []).append(i)
        preds[i].discard(i)
        for p in preds[i]:
            succs[p].add(i)
    cp = [1] * n
    for i in range(n - 1, -1, -1):
        for q in succs[i]:
            cp[i] = max(cp[i], 1 + cp[q])
    npred = [len(p) for p in preds]
    ready = [i for i in range(n) if npred[i] == 0]
    pos = {}
    out = []
    while ready:
        step = len(pos)

        def key(i):
            last = max((pos[p] for p in preds[i]), default=-10 ** 6)
            far = (step - last) >= dmin
            return (0 if far else 1, -cp[i] if far else last, i)

        b = min(ready, key=key)
        ready.remove(b)
        pos[b] = step
        out.extend(units[b]["th"])
        for q in succs[b]:
            npred[q] -= 1
            if npred[q] == 0:
                ready.append(q)
    assert len(pos) == n
    return out


def build(n_groups=16, n_peer_j=128):
    nc = bass.Bass("TRN2", target_bir_lowering=False)
    ntok = HALO + n_groups * G
    dr = lambda name, shape, kind="ExternalInput", dt=F32: nc.dram_tensor(name, shape, dt, kind=kind).ap()
    xh = dr("xh", [ntok, D])
    w_in = dr("w_in", [D, 2560])
    w_out = dr("w_out", [D, D])
    w_q = dr("w_q", [D, 2048])
    keysT = dr("keysT", [128, 2048])
    uv_tab = dr("uv_tab", [16384, 2 * D])
    tab_b = dr("tab_b", [16384, 2 * D], kind="Internal", dt=BF16)
    vecs_d = dr("vecs_h", [128, NVEC])
    gB_d = dr("gB_h", [128, 2048])
    out = dr("out", [n_groups * G, D], kind="ExternalOutput")

    with ExitStack() as st:
        P = Prog(nc, st)
        sb = lambda name, shape, dt=F32: st.enter_context(nc.sbuf_tensor(name, shape, dt))
        Wib = sb("Wib", [128, 8, 2560], BF16)
        Wob = sb("Wob", [128, 8, 1024], BF16)
        Wqb = sb("Wqb", [128, 8, 2048], BF16)
        keysb = sb("keysb", [128, 16, 128], BF16)
        vecs = sb("vecs", [128, NVEC])
        gB = sb("gB", [128, D])
        identb = sb("identb", [128, 128], BF16)
        onesf = sb("onesf", [128, 128])
        iota16 = sb("iota16", [128, 16])
        xt = [sb("xt%d" % i, [128, D]) for i in range(4)]
        hnb = sb("hnb", [128, D], BF16)
        ss = sb("ss", [128, 8])
        hnT = sb("hnT", [128, 8, G], BF16)
        cgs = sb("cgs", [128, G])
        zb = [sb("z%d" % k, [128, HALO + G]) for k in range(4)]
        mixA = sb("mixA", [128, 2048])
        ysc = [mixA[:, k * G:(k + 1) * G] for k in range(4)]
        sq = [sb("sq%d" % k, [128, G]) for k in range(2)]
        ub = [sb("u%d" % k, [128, HALO + G], BF16) for k in range(4)]
        dgs = [sb("dg%d" % k, [128, 128], BF16) for k in range(4)]
        cv = [mixA[:, 1024 + k * G:1024 + (k + 1) * G] for k in range(4)]
        mt = sb("mt", [128, G])
        rt = sb("rt", [128, G])
        qTb = sb("qTb", [128, 1024], BF16)
        Ssb = sb("Ssb", [128, 1024])
        S2 = sb("S2", [128, 256])
        Vt = sb("Vt", [128, 256])
        It = sb("It", [128, 256], U32)
        cand = mixA
        CV = sb("CV", [128, 128])
        CP = sb("CP", [128, 128], U32)
        au = sb("au", [128, 128], U32)
        af = sb("af", [128, 128])
        bf = sb("bf", [128, 128])
        idx2 = [sb("idx%d" % i, [128, 128], U32) for i in range(2)]
        ex = sb("ex", [128, 128])
        Zs = sb("Zs", [128, 8])
        gates2 = [sb("gates%d" % i, [128, 128]) for i in range(2)]
        apre = sb("apre", [128, 128])
        diag = [sb("diag%d" % i, [128, 128], BF16) for i in range(4)]
        GS = [sb("GS%d" % i, [128, 2 * D], BF16) for i in range(NS)]
        yn = [sb("yn%d" % k, [128, G], BF16) for k in range(8)]
        psA = st.enter_context(nc.psum_tensor("psA", [128, 6 * 512], F32))
        psT = st.enter_context(nc.psum_tensor("psT", [128, 1024], BF16))
        pslot = lambda s_, n: psA[:, 1024 + s_ * 256:1024 + s_ * 256 + n]
        bank = lambda i, n=512: psA[:, i * 512:i * 512 + n]
        psT3 = psT[:].rearrange("p (c t) -> p c t", c=8)
        prod = [sb("prod%d" % i, [128, D], BF16) for i in range(2)]
        hbf = [sb("hbf%d" % i, [128, D], BF16) for i in range(2)]
        junk = prod[1][:, :]
        JW = ["prod1"]
        JR = []

        vcol = lambda c: vecs[:, c:c + 1]
        epsc = ss[:, 7:8]
        P.op("vector", lambda e: e.memset(epsc, EPS), writes=["epsc"])
        P.alias["cand"] = ["ysc%d" % k for k in range(4)] + ["cv%d" % k for k in range(4)]
        P.alias["mixlo"] = ["ysc%d" % k for k in range(4)]
        P.alias["mixhi"] = ["cv%d" % k for k in range(4)]
        P.alias["b2"] = ["p0", "p1"]
        P.alias["b3"] = ["p2", "p3"]

        P.dma("sync", lambda e: e.dma_start(out=vecs[:], in_=vecs_d), "ldv", writes=["vecs"])
        P.dma("sync", lambda e: e.dma_start(out=gB[:], in_=gB_d[:, D:2 * D]), "ldg", writes=["gB"])
        P.dma("sync", lambda e: e.dma_start(out=xt[0][:, :], in_=gB_d[:, 0:D]), "ldx0", writes=["xt0"])
        P.op("gpsimd", lambda e: e.memset(apre[:], 1.0), writes=["apre_setup"])
        P.op("gpsimd", lambda e: e.affine_select(out=apre[:], in_=apre[:], pattern=[[-1, 128]], compare_op=ALU.is_equal,
                                                  fill=0.0, base=0, channel_multiplier=1), reads=["apre_setup"], writes=["apre_setup"])
        P.op("vector", lambda e: e.tensor_copy(out=identb[:], in_=apre[:]), reads=["apre_setup"], writes=["identb"])
        P.op("gpsimd", lambda e: e.memset(onesf[:], 1.0), writes=["onesf"])
        P.op("gpsimd", lambda e: e.iota(iota16[:], pattern=[[1, 16]], base=0, channel_multiplier=0,
                                        allow_small_or_imprecise_dtypes=True), writes=["iota16"])
        stage = [(mixA[:, 0:1024], "mixlo"), (mixA[:, 1024:2048], "mixhi"), (xt[1][:, :], "xt1"), (xt[2][:, :], "xt2")]
        si = 0
        for (Wd, Wb, ncol, gcol, nm) in ((w_in, Wib, 2560, GM, "Wib"), (w_out, Wob, 1024, GO, "Wob"), (w_q, Wqb, 2048, GF, "Wqb")):
            for c in range(8):
                for c0 in range(0, ncol, 1024):
                    w = min(1024, ncol - c0)
                    sbuf_, rn = stage[si % 4]
                    si += 1
                    P.dma("sync" if si % 2 else "scalar",
                          lambda e, sbuf_=sbuf_, Wd=Wd, c=c, c0=c0, w=w: e.dma_start(out=sbuf_[:, 0:w], in_=Wd[c * 128:(c + 1) * 128, c0:c0 + w]),
                          "ld" + rn, writes=[rn])
                    if si % 2:
                        P.op("vector", lambda e, sbuf_=sbuf_, Wb=Wb, c=c, c0=c0, w=w, gcol=gcol: e.tensor_scalar(
                            out=Wb[:, c, c0:c0 + w], in0=sbuf_[:, 0:w], scalar1=vcol(gcol + c), scalar2=None, op0=ALU.mult),
                            reads=[rn, "vecs"], writes=[nm])
                    else:
                        P.op("scalar", lambda e, sbuf_=sbuf_, Wb=Wb, c=c, c0=c0, w=w, gcol=gcol: e.activation(
                            out=Wb[:, c, c0:c0 + w], in_=sbuf_[:, 0:w], func=AF.Copy, scale=vcol(gcol + c)),
                            reads=[rn, "vecs"], writes=[nm])
        for c0 in range(0, 2048, 1024):
            sbuf_, rn = stage[si % 4]
            si += 1
            P.dma("sync", lambda e, sbuf_=sbuf_, c0=c0: e.dma_start(out=sbuf_[:, :], in_=keysT[:, c0:c0 + 1024]), "ld" + rn, writes=[rn])
            P.op("vector", lambda e, sbuf_=sbuf_, c0=c0: e.tensor_copy(out=keysb[:].rearrange("p g n -> p (g n)")[:, c0:c0 + 1024], in_=sbuf_[:, :]),
                 reads=[rn], writes=["keysb"])
        GSres = ["GS%d" % i for i in range(NS)]
        TBres = ["tab_b%d" % i for i in range(NS)]
        ustage = [(mixA[:, 0:1024], "mixlo"), (xt[1][:, :], "xt1"), (xt[3][:, :], "xt3")]
        vstage = [(mixA[:, 1024:2048], "mixhi"), (xt[2][:, :], "xt2")]
        for it in range(128):
            su_, ru = ustage[it % 3]
            sv_, rv = vstage[it % 2]
            gs_ = it % NS
            P.dma("sync", lambda e, su_=su_, it=it: e.dma_start(out=su_[:, :], in_=uv_tab[it * 128:(it + 1) * 128, 0:D]), "ldu" + ru, writes=[ru])
            P.dma("sync", lambda e, sv_=sv_, it=it: e.dma_start(out=sv_[:, :], in_=uv_tab[it * 128:(it + 1) * 128, D:2 * D]), "ldv" + rv, writes=[rv])
            P.op("vector", lambda e, su_=su_, gs_=gs_: e.tensor_tensor(out=GS[gs_][:, 0:D], in0=su_[:, :], in1=xt[0][:, :], op=ALU.mult),
                 reads=[ru, "xt0"], writes=[GSres[gs_] + "u"])
            P.op("scalar", lambda e, sv_=sv_, gs_=gs_: e.activation(out=GS[gs_][:, D:2 * D], in_=sv_[:, :], func=AF.Copy), reads=[rv], writes=[GSres[gs_] + "v"])
            P.dma("scalar", lambda e, gs_=gs_, it=it: e.dma_start(out=tab_b[it * 128:(it + 1) * 128, :], in_=GS[gs_][:, :]),
                  "st" + GSres[gs_], reads=[GSres[gs_] + "u", GSres[gs_] + "v", GSres[gs_]], writes=[TBres[gs_]])

        def rstd_from_ss(ss_ap, n, res, scale):
            P.op("scalar", lambda e: e.activation(out=ss_ap, in_=ss_ap, func=AF.Sqrt, scale=scale, bias=epsc[0:n, 0:1]), reads=[res, "epsc"], writes=[res])
            P.op("vector", lambda e: e.reciprocal(out=ss_ap, in_=ss_ap), reads=[res], writes=[res])

        def bcast_rstd(src_bank, src_res, dst, dst_res, N, scale):
            P.op("scalar", lambda e: e.activation(out=dst[:, 0:N], in_=src_bank[:, 0:N], func=AF.Sqrt, scale=scale, bias=epsc[:, 0:1]),
                 reads=[src_res, "epsc"], writes=[dst_res])
            P.op("vector", lambda e: e.reciprocal(out=dst[:, 0:N], in_=dst[:, 0:N]), reads=[dst_res], writes=[dst_res])

        def front(tiles, N, xs, preloaded=False):
            off = 0
            for i, (r0, n) in enumerate(tiles):
                xr = "xt%d" % xs[i]
                xi = xt[xs[i]]
                if not preloaded:
                    P.dma("sync", lambda e, xi=xi, i=i, r0=r0, n=n: e.dma_start(out=xi[0:n, :], in_=xh[r0:r0 + n, :]), "ldx%d" % xs[i], writes=[xr])
                ssr = "ss%d" % i
                P.op("scalar", lambda e, xi=xi, i=i, n=n: e.activation(out=junk[0:n, :], in_=xi[0:n, :], func=AF.Square, accum_out=ss[0:n, i:i + 1]),
                     reads=[xr], writes=[ssr] + JW)
                rstd_from_ss(ss[0:n, i:i + 1], n, ssr, 1.0 / D)
                P.op("scalar", lambda e, xi=xi, i=i, n=n: e.activation(out=hnb[0:n, :], in_=xi[0:n, :], func=AF.Copy, scale=ss[0:n, i:i + 1]),
                     reads=[xr, ssr], writes=["hnb"])
                for c in range(8):
                    P.op("tensor", lambda e, c=c, n=n: e.transpose(out=psT3[:, c, 0:n], in_=hnb[0:n, c * 128:(c + 1) * 128], identity=identb[0:n, 0:n]),
                         reads=["hnb", "identb"], writes=["bT"])
                P.op("vector", lambda e, n=n, off=off: e.tensor_copy(out=hnT[:, :, off:off + n], in_=psT3[:, :, 0:n]), reads=["bT"], writes=["hnT"])
                off += n

        def proj(col, b, N):
            for k in range(8):
                P.op("tensor", lambda e, k=k, col=col, b=b, N=N: e.matmul(pslot(b, N), lhsT=Wib[:, k, col * 128:(col + 1) * 128], rhs=hnT[:, k, 0:N],
                                                                            start=(k == 0), stop=(k == 7)),
                     reads=["Wib", "hnT"], writes=["p%d" % b])

        def mixer_pre():
            N = HALO
            front([(0, HALO)], N, [0])
            for k in range(4):
                proj(k, 0, N)
                proj(8 + k, 1, N)
                P.op("scalar", lambda e: e.activation(out=cgs[:, 0:N], in_=pslot(1, N), func=AF.Copy), reads=["p1"], writes=["cgs"])
                P.op("vector", lambda e, k=k: e.tensor_tensor(out=zb[k][:, 0:N], in0=pslot(0, N), in1=cgs[:, 0:N], op=ALU.mult),
                     reads=["p0", "cgs"], writes=["z%d" % k])
                proj(12 + k, 0, N)
                proj(16 + k, 1, N)
                P.op("scalar", lambda e: e.activation(out=cgs[:, 0:N], in_=pslot(1, N), func=AF.Sigmoid), reads=["p1"], writes=["cgs"])
                P.op("vector", lambda e, k=k: e.tensor_tensor(out=ub[k][:, 0:N], in0=pslot(0, N), in1=cgs[:, 0:N], op=ALU.mult),
                     reads=["p0", "cgs"], writes=["u%d" % k])

        def mixer_group(g):
            N = G
            base = HALO + g * G
            tiles = [(base, 128), (base + 128, 128)]
            xs = [2 * (g % 2), 2 * (g % 2) + 1]
            front(tiles, N, xs, preloaded=(g > 0))
            for k in range(4):
                proj(k, 0, N)
                proj(8 + k, 1, N)
                proj(4 + k, 2, N)
                zr = "z%d" % k
                P.op("scalar", lambda e: e.activation(out=cgs[:, 0:N], in_=pslot(1, N), func=AF.Copy), reads=["p1"], writes=["cgs"])
                if g > 0:
                    P.op("vector", lambda e, k=k: e.tensor_copy(out=zb[k][:, 0:HALO], in_=zb[k][:, G:G + HALO]), reads=[zr], writes=[zr])
                P.op("vector", lambda e, k=k: e.tensor_tensor(out=zb[k][:, HALO:HALO + N], in0=pslot(0, N), in1=cgs[:, 0:N], op=ALU.mult),
                     reads=["p0", "cgs"], writes=[zr])
                P.op("vector", lambda e, k=k: e.tensor_scalar(out=rt[:, 0:N], in0=zb[k][:, HALO - 2:HALO - 2 + N], scalar1=vcol(SCW + 3 * k), scalar2=None, op0=ALU.mult),
                     reads=[zr, "vecs"], writes=["rt"])
                for j in (1, 2):
                    P.op("vector", lambda e, k=k, j=j: e.scalar_tensor_tensor(out=rt[:, 0:N], in0=zb[k][:, HALO - 2 + j:HALO - 2 + j + N], scalar=vcol(SCW + 3 * k + j),
                                                                              in1=rt[:, 0:N], op0=ALU.mult, op1=ALU.add),
                         reads=[zr, "vecs", "rt"], writes=["rt"])
                yr = "ysc%d" % k
                P.op("vector", lambda e, k=k: e.tensor_tensor(out=ysc[k][:, 0:N], in0=pslot(2, N), in1=rt[:, 0:N], op=ALU.mult),
                     reads=["p2", "rt"], writes=[yr])
                sr = "sq%d" % (k % 2)
                P.op("scalar", lambda e, k=k: e.activation(out=sq[k % 2][:, 0:N], in_=ysc[k][:, 0:N], func=AF.Square), reads=[yr], writes=[sr])
                P.op("tensor", lambda e, k=k: e.matmul(bank(4, N), lhsT=onesf[:], rhs=sq[k % 2][:, 0:N], start=(k == 0), stop=(k == 3)),
                     reads=[sr, "onesf"], writes=["b4"])
            bcast_rstd(bank(4), "b4", mt, "mt", N, 1.0 / 512)
            for k in range(4):
                P.op("vector", lambda e, k=k: e.tensor_tensor(out=yn[k][:, 0:N], in0=ysc[k][:, 0:N], in1=mt[:, 0:N], op=ALU.mult),
                     reads=["ysc%d" % k, "mt"], writes=["yn%d" % k])
            if P.defer is not None:
                P.mark = len(P.defer)
            for k in range(4):
                proj(12 + k, 0, N)
                proj(16 + k, 1, N)
                ur = "u%d" % k
                cr = "cv%d" % k
                P.op("scalar", lambda e: e.activation(out=cgs[:, 0:N], in_=pslot(1, N), func=AF.Sigmoid), reads=["p1"], writes=["cgs"])
                if g > 0:
                    P.op("vector", lambda e, k=k: e.tensor_copy(out=ub[k][:, 0:HALO], in_=ub[k][:, G:G + HALO]), reads=[ur], writes=[ur])
                P.op("vector", lambda e, k=k: e.tensor_tensor(out=ub[k][:, HALO:HALO + N], in0=pslot(0, N), in1=cgs[:, 0:N], op=ALU.mult),
                     reads=["p0", "cgs"], writes=[ur])
                for j in range(31):
                    dn = (k * 31 + j) % len(dgs)
                    dr_ = "dg%d" % dn
                    if j % 2:
                        P.op("vector", lambda e, k=k, j=j, dn=dn: e.tensor_tensor(out=dgs[dn][:, :], in0=identb[:, :],
                                                                                 in1=vcol(CFW + 31 * k + j).to_broadcast([128, 128]), op=ALU.mult),
                             reads=["identb", "vecs"], writes=[dr_])
                    else:
                        P.op("scalar", lambda e, k=k, j=j, dn=dn: e.activation(out=dgs[dn][:, :], in_=identb[:, :], func=AF.Copy, scale=vcol(CFW + 31 * k + j)),
                             reads=["identb", "vecs"], writes=[dr_])
                    P.op("tensor", lambda e, k=k, j=j, dn=dn: e.matmul(pslot(3, N), lhsT=dgs[dn][:, :], rhs=ub[k][:, 2 + j:2 + j + N], start=(j == 0), stop=(j == 30)),
                         reads=[dr_, ur], writes=["p3"])
                P.op("vector", lambda e, k=k: e.tensor_scalar(out=cv[k][:, 0:N], in0=pslot(3, N), scalar1=vcol(CFB + k), scalar2=None, op0=ALU.add),
                     reads=["p3", "vecs"], writes=[cr])
                sr = "sq%d" % (k % 2)
                P.op("scalar", lambda e, k=k: e.activation(out=sq[k % 2][:, 0:N], in_=cv[k][:, 0:N], func=AF.Square), reads=[cr], writes=[sr])
                P.op("tensor", lambda e, k=k: e.matmul(bank(4, N), lhsT=onesf[:], rhs=cv[k][:, 0:N], start=(k == 0), stop=(k == 3)),
                     reads=[cr, "onesf"], writes=["b4"])
                P.op("tensor", lambda e, k=k: e.matmul(bank(5, N), lhsT=onesf[:], rhs=sq[k % 2][:, 0:N], start=(k == 0), stop=(k == 3)),
                     reads=[sr, "onesf"], writes=["b5"])
            P.op("vector", lambda e: e.tensor_scalar(out=mt[:, 0:N], in0=bank(4, N), scalar1=1.0 / 512, scalar2=None, op0=ALU.mult), reads=["b4"], writes=["mt"])
            P.op("vector", lambda e: e.tensor_tensor(out=sq[0][:, 0:N], in0=mt[:, 0:N], in1=mt[:, 0:N], op=ALU.mult), reads=["mt"], writes=["sq0"])
            P.op("vector", lambda e: e.scalar_tensor_tensor(out=rt[:, 0:N], in0=bank(5, N), scalar=1.0 / 512, in1=sq[0][:, 0:N], op0=ALU.mult, op1=ALU.subtract),
                 reads=["b5", "sq0"], writes=["rt"])
            P.op("vector", lambda e: e.tensor_scalar(out=rt[:, 0:N], in0=rt[:, 0:N], scalar1=EPS, scalar2=None, op0=ALU.add), reads=["rt"], writes=["rt"])
            P.op("scalar", lambda e: e.activation(out=rt[:, 0:N], in_=rt[:, 0:N], func=AF.Sqrt), reads=["rt"], writes=["rt"])
            P.op("vector", lambda e: e.reciprocal(out=rt[:, 0:N], in_=rt[:, 0:N]), reads=["rt"], writes=["rt"])
            for k in range(4):
                cr = "cv%d" % k
                P.op("vector", lambda e, k=k: e.tensor_tensor(out=cv[k][:, 0:N], in0=cv[k][:, 0:N], in1=mt[:, 0:N], op=ALU.subtract), reads=[cr, "mt"], writes=[cr])
                P.op("vector", lambda e, k=k: e.tensor_tensor(out=cv[k][:, 0:N], in0=cv[k][:, 0:N], in1=rt[:, 0:N], op=ALU.mult), reads=[cr, "rt"], writes=[cr])
                P.op("scalar", lambda e, k=k: e.activation(out=cv[k][:, 0:N], in_=cv[k][:, 0:N], func=AF.Silu, scale=vcol(LNG + k), bias=vcol(LNB + k)),
                     reads=[cr, "vecs"], writes=[cr])
                sr = "sq%d" % (k % 2)
                P.op("scalar", lambda e, k=k: e.activation(out=sq[k % 2][:, 0:N], in_=cv[k][:, 0:N], func=AF.Square), reads=[cr], writes=[sr])
                P.op("tensor", lambda e, k=k: e.matmul(bank(4, N), lhsT=onesf[:], rhs=sq[k % 2][:, 0:N], start=(k == 0), stop=(k == 3)),
                     reads=[sr, "onesf"], writes=["b4"])
            bcast_rstd(bank(4), "b4", mt, "mt", N, 1.0 / 512)
            for k in range(4):
                P.op("vector", lambda e, k=k: e.tensor_tensor(out=yn[4 + k][:, 0:N], in0=cv[k][:, 0:N], in1=mt[:, 0:N], op=ALU.mult),
                     reads=["cv%d" % k, "mt"], writes=["yn%d" % (4 + k)])
            for i in range(2):
                for half in range(2):
                    for kk in range(8):
                        P.op("tensor", lambda e, i=i, half=half, kk=kk: e.matmul(psA[:, (2 + half) * 512:(3 + half) * 512], lhsT=yn[kk][:, i * 128:(i + 1) * 128],
                                                                                  rhs=Wob[:, kk, half * 512:(half + 1) * 512], start=(kk == 0), stop=(kk == 7)),
                             reads=["yn%d" % kk, "Wob"], writes=["b%d" % (2 + half)])
                P.op("vector", lambda e, i=i: e.tensor_tensor(out=xt[xs[i]][:, :], in0=psA[:, 2 * 512:4 * 512], in1=xt[xs[i]][:, :], op=ALU.add),
                     reads=["b2", "b3", "xt%d" % xs[i]], writes=["xt%d" % xs[i]])

        def route(i, x_):
            xr = "xt%d" % x_
            h2 = xt[x_]
            idx = idx2[i]
            gates = gates2[i]
            ssc = ss[:, 2 + i:3 + i]
            ssr2 = "ss2_%d" % i
            idxr = "idx%d" % i
            gatesr = "gates%d" % i
            P.op("scalar", lambda e: e.activation(out=junk[:, :], in_=h2[:, :], func=AF.Square, accum_out=ssc), reads=[xr], writes=[ssr2] + JW)
            rstd_from_ss(ssc, 128, ssr2, 1.0 / D)
            P.op("scalar", lambda e: e.activation(out=hbf[i][:, :], in_=h2[:, :], func=AF.Copy, scale=ssc), reads=[xr, ssr2], writes=["hbf%d" % i])
            for c in range(8):
                P.op("tensor", lambda e, c=c: e.transpose(out=psT3[:, c, :], in_=hbf[i][:, c * 128:(c + 1) * 128], identity=identb[:, :]),
                     reads=["hbf%d" % i, "identb"], writes=["bT"])
            P.op("vector", lambda e: e.tensor_copy(out=hnT[:, :, 0:128], in_=psT3[:, :, :]), reads=["bT"], writes=["hnT"])
            for hf in range(2):
                for gg in range(8):
                    g = hf * 8 + gg
                    for k in range(8):
                        P.op("tensor", lambda e, g=g, gg=gg, k=k: e.matmul(psA[:, 2048 + gg * 128:2048 + (gg + 1) * 128], lhsT=Wqb[:, k, g * 128:(g + 1) * 128],
                                                                        rhs=hnT[:, k, 0:128], start=(k == 0), stop=(k == 7)),
                             reads=["Wqb", "hnT"], writes=["b%d" % (4 + gg // 4)])
                P.op("scalar", lambda e, hf=hf: e.activation(out=qTb[:, :], in_=psA[:, 2048:3072], func=AF.Copy), reads=["b4", "b5"], writes=["qTb"])
                for gg in range(8):
                    g = hf * 8 + gg
                    P.op("tensor", lambda e, g=g, gg=gg: e.matmul(psA[:, 2048 + gg * 128:2048 + (gg + 1) * 128], lhsT=qTb[:, gg * 128:(gg + 1) * 128], rhs=keysb[:, g, :],
                                                               start=True, stop=True),
                         reads=["qTb", "keysb"], writes=["b%d" % (4 + gg // 4)])
                P.op("scalar", lambda e: e.activation(out=Ssb[:, :], in_=psA[:, 2048:3072], func=AF.Copy), reads=["b4", "b5"], writes=["Ssb"])
                for gg in range(8):
                    g = hf * 8 + gg
                    sg_ = Ssb[:, gg * 128:(gg + 1) * 128]
                    v0 = Vt[:, g * 16:g * 16 + 8]
                    v1 = Vt[:, g * 16 + 8:g * 16 + 16]
                    i0 = It[:, g * 16:g * 16 + 8]
                    i1 = It[:, g * 16 + 8:g * 16 + 16]
                    P.op("vector", lambda e, sg_=sg_, v0=v0: e.max(out=v0, in_=sg_), reads=["Ssb"], writes=["Vt"])
                    P.op("vector", lambda e, sg_=sg_, v0=v0, i0=i0: e.max_index(out=i0, in_max=v0, in_values=sg_), reads=["Ssb", "Vt"], writes=["It"])
                    P.op("vector", lambda e, sg_=sg_, v0=v0: e.match_replace(out=S2[:, 0:128], in_to_replace=v0, in_values=sg_, imm_value=-1e30),
                         reads=["Ssb", "Vt"], writes=["S2"])
                    P.op("vector", lambda e, v1=v1: e.max(out=v1, in_=S2[:, 0:128]), reads=["S2"], writes=["Vt"])
                    P.op("vector", lambda e, v1=v1, i1=i1: e.max_index(out=i1, in_max=v1, in_values=S2[:, 0:128]), reads=["S2", "Vt"], writes=["It"])
            Vt4 = Vt[:].rearrange("p (h s a) -> p h s a", h=8, s=2)
            cand4 = cand[:].rearrange("p (h a b) -> p h a b", h=8, a=16)
            P.op("vector", lambda e: e.tensor_tensor(out=cand4, in0=Vt4[:, :, 0, :].unsqueeze(3).to_broadcast([128, 8, 16, 16]),
                                                      in1=Vt4[:, :, 1, :].unsqueeze(2).to_broadcast([128, 8, 16, 16]), op=ALU.add),
                 reads=["Vt"], writes=["cand"])
            for h in range(8):
                ch = cand[:, h * 256:(h + 1) * 256]
                v0 = CV[:, h * 16:h * 16 + 8]
                v1 = CV[:, h * 16 + 8:h * 16 + 16]
                p0 = CP[:, h * 16:h * 16 + 8]
                p1 = CP[:, h * 16 + 8:h * 16 + 16]
                P.op("vector", lambda e, ch=ch, v0=v0: e.max(out=v0, in_=ch), reads=["cand"], writes=["CV"])
                P.op("vector", lambda e, ch=ch, v0=v0, p0=p0: e.max_index(out=p0, in_max=v0, in_values=ch), reads=["cand", "CV"], writes=["CP"])
                P.op("vector", lambda e, ch=ch, v0=v0: e.match_replace(out=S2[:, :], in_to_replace=v0, in_values=ch, imm_value=-1e30),
                     reads=["cand", "CV"], writes=["S2"])
                P.op("vector", lambda e, v1=v1: e.max(out=v1, in_=S2[:, :]), reads=["S2"], writes=["CV"])
                P.op("vector", lambda e, v1=v1, p1=p1: e.max_index(out=p1, in_max=v1, in_values=S2[:, :]), reads=["S2", "CV"], writes=["CP"])
            CV3 = CV[:].rearrange("p (h k) -> p h k", h=8)
            ex3 = ex[:].rearrange("p (h k) -> p h k", h=8)
            g3 = gates[:].rearrange("p (h k) -> p h k", h=8)
            P.op("vector", lambda e: e.tensor_tensor(out=ex3, in0=CV3, in1=CV3[:, :, 0:1].to_broadcast([128, 8, 16]), op=ALU.subtract), reads=["CV"], writes=["ex"])
            P.op("scalar", lambda e: e.activation(out=ex[:, :], in_=ex[:, :], func=AF.Exp), reads=["ex"], writes=["ex"])
            P.op("vector", lambda e: e.tensor_reduce(out=Zs[:, 0:8], in_=ex3, axis=AX.X, op=ALU.add), reads=["ex"], writes=["Zs"])
            P.op("vector", lambda e: e.reciprocal(out=Zs[:, 0:8], in_=Zs[:, 0:8]), reads=["Zs"], writes=["Zs"])
            P.op("vector", lambda e: e.tensor_tensor(out=g3, in0=ex3, in1=Zs[:, 0:8].unsqueeze(2).to_broadcast([128, 8, 16]), op=ALU.mult),
                 reads=["ex", "Zs"], writes=[gatesr])
            P.op("vector", lambda e: e.tensor_single_scalar(out=au[:, :], in_=CP[:, :], scalar=4, op=ALU.logical_shift_right), reads=["CP"], writes=["au"])
            P.op("vector", lambda e: e.tensor_single_scalar(out=CP[:, :], in_=CP[:, :], scalar=15, op=ALU.bitwise_and), reads=["CP", "au"], writes=["CP"])
            P.op("vector", lambda e: e.tensor_copy(out=af[:, :], in_=au[:, :]), reads=["au"], writes=["af"])
            P.op("vector", lambda e: e.tensor_copy(out=bf[:, :], in_=CP[:, :]), reads=["CP"], writes=["bf"])
            P.op("vector", lambda e: e.tensor_copy(out=S2[:, :], in_=It[:, :]), reads=["It"], writes=["S2"])
            Itf4 = S2[:].rearrange("p (h s a) -> p h s a", h=8, s=2)
            oh4 = Ssb[:].rearrange("p (h k a) -> p h k a", h=4, k=16)
            io4 = iota16[:, :].unsqueeze(1).unsqueeze(1).to_broadcast([128, 4, 16, 16])
            for (srcf, s_, sres) in ((af, 0, "af"), (bf, 1, "bf")):
                s3 = srcf[:].rearrange("p (h k) -> p h k", h=8)
                for hh in range(2):
                    hs = slice(hh * 4, hh * 4 + 4)
                    P.op("vector", lambda e, s3=s3, hs=hs: e.tensor_tensor(out=oh4, in0=s3[:, hs, :].unsqueeze(3).to_broadcast([128, 4, 16, 16]), in1=io4, op=ALU.is_equal),
                         reads=[sres, "iota16"], writes=["Ssb"])
                    P.op("vector", lambda e, s_=s_, hs=hs: e.tensor_tensor(out=oh4, in0=oh4, in1=Itf4[:, hs, s_, :].unsqueeze(2).to_broadcast([128, 4, 16, 16]), op=ALU.mult),
                         reads=["Ssb", "S2"], writes=["Ssb"])
                    P.op("vector", lambda e, s3=s3, hs=hs: e.tensor_reduce(out=s3[:, hs, :], in_=oh4, axis=AX.X, op=ALU.add), reads=["Ssb"], writes=[sres])
            P.op("vector", lambda e: e.scalar_tensor_tensor(out=af[:, :], in0=af[:, :], scalar=128.0, in1=bf[:, :], op0=ALU.mult, op1=ALU.add),
                 reads=["af", "bf"], writes=["af"])
            P.op("vector", lambda e: e.tensor_copy(out=idx[:, :], in_=af[:, :]), reads=["af"], writes=[idxr])

        def experts(i, x_, orow, filler=None):
            xr = "xt%d" % x_
            h2 = xt[x_]
            rate = (sum(1 for e_, _ in filler if e_ != "tensor") // n_peer_j + 1) if filler else 0
            idx = idx2[i]
            gates = gates2[i]
            ssc = ss[:, 2 + i:3 + i]
            ssr2 = "ss2_%d" % i
            idxr = "idx%d" % i
            gatesr = "gates%d" % i

            def fill(n):
                while n > 0 and filler:
                    e_, th = filler.pop(0)
                    th()
                    if e_ != "tensor":
                        n -= 1
            nj = n_peer_j
            nb = nj // JB

            nj = n_peer_j
            def gather(j):
                s_ = j % NS
                P.dma("gpsimd", lambda e: e.indirect_dma_start(out=GS[s_][:, :], out_offset=None, in_=tab_b,
                                                               in_offset=bass.IndirectOffsetOnAxis(ap=idx[:, j:j + 1], axis=0)),
                      "g" + GSres[s_], reads=[idxr] + TBres, writes=[GSres[s_]])

            def dot(j):
                s_ = j % NS
                p_ = j % 2
                P.op("vector", lambda e: e.tensor_tensor(out=prod[p_][:, :], in0=GS[s_][:, 0:D], in1=hbf[i][:, :], op=ALU.mult),
                     reads=["hbf%d" % i], writes=["prod%d" % p_], weak=[GSres[s_]])
                P.op("scalar", lambda e: e.activation(out=prod[p_][:, :], in_=prod[p_][:, :], func=AF.Copy, accum_out=apre[:, j:j + 1]),
                     reads=["prod%d" % p_], writes=["prod%d" % p_, "apre%d" % j])

            def gelu(j):
                P.op("scalar", lambda e: e.activation(out=apre[:, j:j + 1], in_=apre[:, j:j + 1], func=AF.Gelu_apprx_tanh),
                     reads=["apre%d" % j], writes=["apre%d" % j])

            def acc(j):
                s_ = j % NS
                d_ = j % len(diag)
                P.op("vector", lambda e: e.scalar_tensor_tensor(out=diag[d_][:, :], in0=identb[:, :], scalar=apre[:, j:j + 1],
                                                                in1=gates[:, j:j + 1].to_broadcast([128, 128]), op0=ALU.mult, op1=ALU.mult),
                     reads=["identb", "apre%d" % j, gatesr], writes=["diag%d" % d_])
                for half in range(2):
                    P.op("tensor", lambda e, half=half: e.matmul(psA[:, half * 512:(half + 1) * 512], lhsT=diag[d_][:, :],
                                                                 rhs=GS[s_][:, D + half * 512:D + (half + 1) * 512], start=(j == 0), stop=(j == nj - 1)),
                         reads=["diag%d" % d_, GSres[s_]], writes=["b%d" % half])

            r1 = rate // 3
            r2 = (rate - r1) // 2
            r3 = rate - r1 - r2
            for t in range(nj + 4):
                if t < nj:
                    gather(t)
                fill(r3)
                if 0 <= t - 1 < nj:
                    dot(t - 1)
                fill(r2)
                if 0 <= t - 2 < nj:
                    gelu(t - 2)
                if 0 <= t - 4 < nj:
                    acc(t - 4)
                fill(r1)
            fill(100000)
            P.op("vector", lambda e: e.tensor_tensor(out=h2[:, :], in0=psA[:, 0:1024], in1=h2[:, :], op=ALU.add), reads=["b0", "b1", xr], writes=[xr])
            P.op("scalar", lambda e: e.activation(out=junk[:, :], in_=h2[:, :], func=AF.Square, accum_out=ss[:, 4 + i:5 + i]), reads=[xr], writes=["ss3_%d" % i] + JW)
            rstd_from_ss(ss[:, 4 + i:5 + i], 128, "ss3_%d" % i, 1.0 / D)
            P.op("vector", lambda e: e.scalar_tensor_tensor(out=h2[:, :], in0=h2[:, :], scalar=ss[:, 4 + i:5 + i], in1=gB[:, :], op0=ALU.mult, op1=ALU.mult),
                 reads=[xr, "ss3_%d" % i, "gB"], writes=[xr])
            P.dma("sync", lambda e: e.dma_start(out=out[orow:orow + 128, :], in_=h2[:, :]), "st%d" % x_, reads=[xr], writes=["out%d" % x_])

        mixer_pre()
        mixer_group(0)
        route(0, 0)
        for g in range(n_groups):
            x0 = 2 * (g % 2)
            P.defer = []
            route(1, x0 + 1)
            R1 = P.defer
            M, R0, ms = [], [], 0
            if g + 1 < n_groups:
                for i_ in range(2):
                    xs_ = 2 * ((g + 1) % 2) + i_
                    r0_ = HALO + (g + 1) * G + i_ * 128
                    P._dma("sync", lambda e, xs_=xs_, r0_=r0_: e.dma_start(out=xt[xs_][:, :], in_=xh[r0_:r0_ + 128, :]), "ldx%d" % xs_, writes=["xt%d" % xs_])
                P.defer = []
                P.mark = 0
                mixer_group(g + 1)
                M, ms = P.defer, P.mark
                P.defer = []
                route(0, 2 * ((g + 1) % 2))
                R0 = P.defer
            P.defer = None
            experts(0, x0, g * G, reorder(R1 + M[:ms]))
            experts(1, x0 + 1, g * G + 128, [(e_, th_) for (e_, th_, _r, _w) in M[ms:] + R0])
        P.wait_all("sync", ["out0", "out1", "out2", "out3"])
        P.emit()
    return nc


def _prep_shared(inp):
    f = lambda a: np.ascontiguousarray(np.asarray(a, dtype=np.float32))
    col = lambda v, n: np.asarray(v, np.float32).reshape(n, 128).T
    vecs = np.zeros((128, NVEC), np.float32)
    vecs[:, GM:GM + 8] = col(inp["norm_mix_g"][0], 8)
    vecs[:, GF:GF + 8] = col(inp["norm_ffn_g"][0], 8)
    vecs[:, GO:GO + 4] = col(inp["out_norm_g_sc"][0], 4)
    vecs[:, GO + 4:GO + 8] = col(inp["out_norm_g_cf"][0], 4)
    scw = np.asarray(inp["sc_conv_w"][0], np.float32)
    cfw = np.asarray(inp["cf_conv_w"][0], np.float32)
    for k in range(4):
        vecs[:, SCW + 3 * k:SCW + 3 * k + 3] = scw[:, k * 128:(k + 1) * 128].T
        vecs[:, CFW + 31 * k:CFW + 31 * k + 31] = cfw[:, k * 128:(k + 1) * 128].T
    vecs[:, CFB:CFB + 4] = col(inp["cf_conv_b"][0], 4)
    vecs[:, LNG:LNG + 4] = col(inp["cf_ln_g"][0], 4)
    vecs[:, LNB:LNB + 4] = col(inp["cf_ln_b"][0], 4)
    gB = np.concatenate([np.broadcast_to(np.asarray(inp["norm_ffn_g"][0], np.float32)[None, :], (128, D)),
                         np.broadcast_to(np.asarray(inp["final_norm_g"], np.float32)[None, :], (128, D))], axis=1)
    sk = np.asarray(inp["peer_sub_keys"][0], np.float32)
    keysT = np.ascontiguousarray(sk.reshape(16, 128, 128).transpose(2, 0, 1).reshape(128, 2048))
    return {
        "w_in": f(inp["w_in"][0]), "w_out": f(inp["w_out"][0]), "w_q": f(inp["peer_w_q"][0]),
        "keysT": keysT, "uv_tab": np.ascontiguousarray(np.concatenate([f(inp["peer_u"][0]), f(inp["peer_v"][0])], axis=1)),
        "vecs_h": vecs, "gB_h": np.ascontiguousarray(gB),
    }


def _core_x(inp, core, n_groups=16):
    x = np.asarray(inp["x"], np.float32)
    meta = np.asarray(inp["meta_tokens"], np.float32)
    b, half = core // 2, core % 2
    ntok = n_groups * G
    if half == 0:
        halo = np.concatenate([np.zeros((HALO - meta.shape[0], D), np.float32), meta], axis=0)
        body = x[b, 0:ntok]
    else:
        halo = x[b, 4096 - HALO:4096]
        body = x[b, 4096:4096 + ntok]
    return np.ascontiguousarray(np.concatenate([halo, body], axis=0))


def kernel(**inputs):
    shared = _prep_shared(inputs)
    nc = build(16)
    in_maps = []
    for c in range(N_CORES):
        m = dict(shared)
        m["xh"] = _core_x(inputs, c)
        in_maps.append(m)
    res = run_bass_kernel_spmd(nc, in_maps, core_ids=list(range(N_CORES)))
    outp = np.empty((4, 8192, D), np.float32)
    for c in range(N_CORES):
        b, half = c // 2, c % 2
        outp[b, half * 4096:(half + 1) * 4096] = res.results[c]["out"]
    return outp
```

```python
import numpy as np
from contextlib import ExitStack
import concourse.bass as bass
import concourse.mybir as mybir
from concourse.bass_utils import run_bass_kernel_spmd

F32 = mybir.dt.float32
BF16 = mybir.dt.bfloat16
U32 = mybir.dt.uint32
ALU = mybir.AluOpType
AF = mybir.ActivationFunctionType
AX = mybir.AxisListType

D = 1024
G = 256
HALO = 32
N_CORES = 8
EPS = 1e-6
NS = 10
JB = 4
GM, GF, GO, SCW, CFW, CFB, LNG, LNB, NVEC = 0, 8, 16, 24, 36, 160, 164, 168, 172


class Prog:
    ENGS = ("sync", "scalar", "vector", "gpsimd", "tensor")

    def __init__(self, nc, stack):
        self.nc = nc
        self.stack = stack
        self.q = {e: [] for e in self.ENGS}
        self.cnt = {e: 0 for e in self.ENGS}
        self.sem = {e: stack.enter_context(nc.semaphore("c_" + e)) for e in self.ENGS}
        self.seen = {e: {} for e in self.ENGS}
        self.last_w = {}
        self.readers = {}
        self.dsem = {}
        self.dcnt = {}
        self.alias = {}
        self.defer = None

    def _exp(self, names):
        out = []
        for n in names:
            out.extend(self.alias.get(n, (n,)))
        return out

    def _deps(self, e, reads, writes):
        deps = {}

        def add(ev):
            if ev is None:
                return
            k, s, v = ev
            if k not in deps or deps[k][1] < v:
                deps[k] = (s, v)

        for r in reads:
            add(self.last_w.get(r))
        for w in writes:
            rd = self.readers.get(w, ())
            if not rd:
                add(self.last_w.get(w))
            for ev in rd:
                add(ev)
        for k, (s, v) in deps.items():
            if e == "tensor" and k == "e_tensor":
                continue
            if self.seen[e].get(k, 0) < v:
                self.seen[e][k] = v
                self.q[e].append(lambda eng, s=s, v=v: eng.wait_ge(s, v))

    def _commit(self, ev, reads, writes):
        for w in writes:
            self.last_w[w] = ev
            self.readers[w] = []
        for r in reads:
            if r not in writes:
                self.readers.setdefault(r, []).append(ev)

    def op(self, e, fn, reads=(), writes=(), weak=()):
        if self.defer is not None:
            self.defer.append((e, lambda: self._op(e, fn, reads, writes, weak),
                               set(self._exp(reads)) | set(self._exp(weak)), set(self._exp(writes))))
            return
        self._op(e, fn, reads, writes, weak)

    def _op(self, e, fn, reads=(), writes=(), weak=()):
        reads, writes, weak = self._exp(reads), self._exp(writes), self._exp(weak)
        self._deps(e, list(reads) + list(weak), writes)
        self.cnt[e] += 1
        n = self.cnt[e]
        s = self.sem[e]
        self.q[e].append(lambda eng, fn=fn, s=s: fn(eng).then_inc(s, 1))
        self._commit(("e_" + e, s, n), reads, writes)

    def dma(self, e, fn, key, reads=(), writes=()):
        if self.defer is not None:
            self.defer.append((e, lambda: self._dma(e, fn, key, reads, writes), set(self._exp(reads)), set(self._exp(writes))))
            return
        self._dma(e, fn, key, reads, writes)

    def _dma(self, e, fn, key, reads=(), writes=()):
        reads, writes = self._exp(reads), self._exp(writes)
        self._deps(e, reads, writes)
        if key not in self.dsem:
            self.dsem[key] = self.stack.enter_context(self.nc.semaphore("d_" + key))
            self.dcnt[key] = 0
        s = self.dsem[key]
        self.dcnt[key] += 16
        v = self.dcnt[key]
        self.q[e].append(lambda eng, fn=fn, s=s: fn(eng).then_inc(s, 16))
        self._commit(("d_" + key, s, v), reads, writes)

    def wait_all(self, e, resources):
        self._deps(e, self._exp(resources), ())

    def emit(self):
        with self.nc.Block() as block:
            @block.sync
            def _(eng):
                for f in self.q["sync"]:
                    f(eng)

            @block.scalar
            def _(eng):
                for f in self.q["scalar"]:
                    f(eng)

            @block.vector
            def _(eng):
                for f in self.q["vector"]:
                    f(eng)

            @block.gpsimd
            def _(eng):
                for f in self.q["gpsimd"]:
                    f(eng)

            @block.tensor
            def _(eng):
                for f in self.q["tensor"]:
                    f(eng)


PE_BANK = {"p0": "B2", "p1": "B2", "p2": "B3", "p3": "B3", "b0": "B0", "b1": "B1", "b4": "B4", "b5": "B5", "bT": "BT"}


def reorder(entries, dmin=6):
    units = []
    for (e, th, rd, wr) in entries:
        rd, wr = set(rd), set(wr)
        if e == "tensor":
            wr |= {"pe_" + PE_BANK[w] for w in wr if w in PE_BANK}
            if units and units[-1]["pe"]:
                u = units[-1]
                u["th"].append((e, th)); u["rd"] |= rd; u["wr"] |= wr
                continue
        units.append({"pe": e == "tensor", "th": [(e, th)], "rd": rd, "wr": wr})
    n = len(units)
    preds = [set() for _ in range(n)]
    succs = [set() for _ in range(n)]
    last_w, readers = {}, {}
    for i, u in enumerate(units):
        for r in u["rd"]:
            if r in last_w:
                preds[i].add(last_w[r])
        for w in u["wr"]:
            if w in last_w:
                preds[i].add(last_w[w])
            preds[i].update(readers.get(w, ()))
        for w in u["wr"]:
            last_w[w] = i
            readers[w] = []
        for r in u["rd"]:
            if r not in u["wr"]:
                readers.setdefault(r, []).append(i)
        preds[i].discard(i)
        for p in preds[i]:
            succs[p].add(i)
    cp = [1] * n
    for i in range(n - 1, -1, -1):
        for q in succs[i]:
            cp[i] = max(cp[i], 1 + cp[q])
    npred = [len(p) for p in preds]
    ready = [i for i in range(n) if npred[i] == 0]
    pos = {}
    out = []
    while ready:
        step = len(pos)

        def key(i):
            last = max((pos[p] for p in preds[i]), default=-10 ** 6)
            far = (step - last) >= dmin
            return (0 if far else 1, -cp[i] if far else last, i)

        b = min(ready, key=key)
        ready.remove(b)
        pos[b] = step
        out.extend(units[b]["th"])
        for q in succs[b]:
            npred[q] -= 1
            if npred[q] == 0:
                ready.append(q)
    assert len(pos) == n
    return out


def build(n_groups=16, n_peer_j=128):
    nc = bass.Bass("TRN2", target_bir_lowering=False)
    ntok = HALO + n_groups * G
    dr = lambda name, shape, kind="ExternalInput", dt=F32: nc.dram_tensor(name, shape, dt, kind=kind).ap()
    xh = dr("xh", [ntok, D])
    w_in = dr("w_in", [D, 2560])
    w_out = dr("w_out", [D, D])
    w_q = dr("w_q", [D, 2048])
    keysT = dr("keysT", [128, 2048])
    uv_tab = dr("uv_tab", [16384, 2 * D])
    tab_b = dr("tab_b", [16384, 2 * D], kind="Internal", dt=BF16)
    vecs_d = dr("vecs_h", [128, NVEC])
    gB_d = dr("gB_h", [128, 2048])
    out = dr("out", [n_groups * G, D], kind="ExternalOutput")

    with ExitStack() as st:
        P = Prog(nc, st)
        sb = lambda name, shape, dt=F32: st.enter_context(nc.sbuf_tensor(name, shape, dt))
        Wib = sb("Wib", [128, 8, 2560], BF16)
        Wob = sb("Wob", [128, 8, 1024], BF16)
        Wqb = sb("Wqb", [128, 8, 2048], BF16)
        keysb = sb("keysb", [128, 16, 128], BF16)
        vecs = sb("vecs", [128, NVEC])
        gB = sb("gB", [128, D])
        identb = sb("identb", [128, 128], BF16)
        onesf = sb("onesf", [128, 128])
        iota16 = sb("iota16", [128, 16])
        xt = [sb("xt%d" % i, [128, D]) for i in range(4)]
        hnb = sb("hnb", [128, D], BF16)
        ss = sb("ss", [128, 8])
        hnT = sb("hnT", [128, 8, G], BF16)
        cgs = sb("cgs", [128, G])
        zb = [sb("z%d" % k, [128, HALO + G]) for k in range(4)]
        mixA = sb("mixA", [128, 2048])
        ysc = [mixA[:, k * G:(k + 1) * G] for k in range(4)]
        sq = [sb("sq%d" % k, [128, G]) for k in range(2)]
        ub = [sb("u%d" % k, [128, HALO + G], BF16) for k in range(4)]
        dgs = [sb("dg%d" % k, [128, 128], BF16) for k in range(4)]
        cv = [mixA[:, 1024 + k * G:1024 + (k + 1) * G] for k in range(4)]
        mt = sb("mt", [128, G])
        rt = sb("rt", [128, G])
        qTb = sb("qTb", [128, 1024], BF16)
        Ssb = sb("Ssb", [128, 1024])
        S2 = sb("S2", [128, 256])
        Vt = sb("Vt", [128, 256])
        It = sb("It", [128, 256], U32)
        cand = mixA
        CV = sb("CV", [128, 128])
        CP = sb("CP", [128, 128], U32)
        au = sb("au", [128, 128], U32)
        af = sb("af", [128, 128])
        bf = sb("bf", [128, 128])
        idx2 = [sb("idx%d" % i, [128, 128], U32) for i in range(2)]
        ex = sb("ex", [128, 128])
        Zs = sb("Zs", [128, 8])
        gates2 = [sb("gates%d" % i, [128, 128]) for i in range(2)]
        apre = sb("apre", [128, 128])
        diag = [sb("diag%d" % i, [128, 128], BF16) for i in range(4)]
        GS = [sb("GS%d" % i, [128, 2 * D], BF16) for i in range(NS)]
        yn = [sb("yn%d" % k, [128, G], BF16) for k in range(8)]
        psA = st.enter_context(nc.psum_tensor("psA", [128, 6 * 512], F32))
        psT = st.enter_context(nc.psum_tensor("psT", [128, 1024], BF16))
        pslot = lambda s_, n: psA[:, 1024 + s_ * 256:1024 + s_ * 256 + n]
        bank = lambda i, n=512: psA[:, i * 512:i * 512 + n]
        psT3 = psT[:].rearrange("p (c t) -> p c t", c=8)
        prod = [sb("prod%d" % i, [128, D], BF16) for i in range(2)]
        hbf = [sb("hbf%d" % i, [128, D], BF16) for i in range(2)]
        junk = prod[1][:, :]
        JW = ["prod1"]
        JR = []

        vcol = lambda c: vecs[:, c:c + 1]
        epsc = ss[:, 7:8]
        P.op("vector", lambda e: e.memset(epsc, EPS), writes=["epsc"])
        P.alias["cand"] = ["ysc%d" % k for k in range(4)] + ["cv%d" % k for k in range(4)]
        P.alias["mixlo"] = ["ysc%d" % k for k in range(4)]
        P.alias["mixhi"] = ["cv%d" % k for k in range(4)]
        P.alias["b2"] = ["p0", "p1"]
        P.alias["b3"] = ["p2", "p3"]

        P.dma("sync", lambda e: e.dma_start(out=vecs[:], in_=vecs_d), "ldv", writes=["vecs"])
        P.dma("sync", lambda e: e.dma_start(out=gB[:], in_=gB_d[:, D:2 * D]), "ldg", writes=["gB"])
        P.dma("sync", lambda e: e.dma_start(out=xt[0][:, :], in_=gB_d[:, 0:D]), "ldx0", writes=["xt0"])
        P.op("gpsimd", lambda e: e.memset(apre[:], 1.0), writes=["apre_setup"])
        P.op("gpsimd", lambda e: e.affine_select(out=apre[:], in_=apre[:], pattern=[[-1, 128]], compare_op=ALU.is_equal,
                                                  fill=0.0, base=0, channel_multiplier=1), reads=["apre_setup"], writes=["apre_setup"])
        P.op("vector", lambda e: e.tensor_copy(out=identb[:], in_=apre[:]), reads=["apre_setup"], writes=["identb"])
        P.op("gpsimd", lambda e: e.memset(onesf[:], 1.0), writes=["onesf"])
        P.op("gpsimd", lambda e: e.iota(iota16[:], pattern=[[1, 16]], base=0, channel_multiplier=0,
                                        allow_small_or_imprecise_dtypes=True), writes=["iota16"])
        stage = [(mixA[:, 0:1024], "mixlo"), (mixA[:, 1024:2048], "mixhi"), (xt[1][:, :], "xt1"), (xt[2][:, :], "xt2")]
        si = 0
        for (Wd, Wb, ncol, gcol, nm) in ((w_in, Wib, 2560, GM, "Wib"), (w_out, Wob, 1024, GO, "Wob"), (w_q, Wqb, 2048, GF, "Wqb")):
            for c in range(8):
                for c0 in range(0, ncol, 1024):
                    w = min(1024, ncol - c0)
                    sbuf_, rn = stage[si % 4]
                    si += 1
                    P.dma("sync" if si % 2 else "scalar",
                          lambda e, sbuf_=sbuf_, Wd=Wd, c=c, c0=c0, w=w: e.dma_start(out=sbuf_[:, 0:w], in_=Wd[c * 128:(c + 1) * 128, c0:c0 + w]),
                          "ld" + rn, writes=[rn])
                    if si % 2:
                        P.op("vector", lambda e, sbuf_=sbuf_, Wb=Wb, c=c, c0=c0, w=w, gcol=gcol: e.tensor_scalar(
                            out=Wb[:, c, c0:c0 + w], in0=sbuf_[:, 0:w], scalar1=vcol(gcol + c), scalar2=None, op0=ALU.mult),
                            reads=[rn, "vecs"], writes=[nm])
                    else:
                        P.op("scalar", lambda e, sbuf_=sbuf_, Wb=Wb, c=c, c0=c0, w=w, gcol=gcol: e.activation(
                            out=Wb[:, c, c0:c0 + w], in_=sbuf_[:, 0:w], func=AF.Copy, scale=vcol(gcol + c)),
                            reads=[rn, "vecs"], writes=[nm])
        for c0 in range(0, 2048, 1024):
            sbuf_, rn = stage[si % 4]
            si += 1
            P.dma("sync", lambda e, sbuf_=sbuf_, c0=c0: e.dma_start(out=sbuf_[:, :], in_=keysT[:, c0:c0 + 1024]), "ld" + rn, writes=[rn])
            P.op("vector", lambda e, sbuf_=sbuf_, c0=c0: e.tensor_copy(out=keysb[:].rearrange("p g n -> p (g n)")[:, c0:c0 + 1024], in_=sbuf_[:, :]),
                 reads=[rn], writes=["keysb"])
        GSres = ["GS%d" % i for i in range(NS)]
        TBres = ["tab_b%d" % i for i in range(NS)]
        ustage = [(mixA[:, 0:1024], "mixlo"), (xt[1][:, :], "xt1"), (xt[3][:, :], "xt3")]
        vstage = [(mixA[:, 1024:2048], "mixhi"), (xt[2][:, :], "xt2")]
        for it in range(128):
            su_, ru = ustage[it % 3]
            sv_, rv = vstage[it % 2]
            gs_ = it % NS
            P.dma("sync", lambda e, su_=su_, it=it: e.dma_start(out=su_[:, :], in_=uv_tab[it * 128:(it + 1) * 128, 0:D]), "ldu" + ru, writes=[ru])
            P.dma("sync", lambda e, sv_=sv_, it=it: e.dma_start(out=sv_[:, :], in_=uv_tab[it * 128:(it + 1) * 128, D:2 * D]), "ldv" + rv, writes=[rv])
            P.op("vector", lambda e, su_=su_, gs_=gs_: e.tensor_tensor(out=GS[gs_][:, 0:D], in0=su_[:, :], in1=xt[0][:, :], op=ALU.mult),
                 reads=[ru, "xt0"], writes=[GSres[gs_] + "u"])
            P.op("scalar", lambda e, sv_=sv_, gs_=gs_: e.activation(out=GS[gs_][:, D:2 * D], in_=sv_[:, :], func=AF.Copy), reads=[rv], writes=[GSres[gs_] + "v"])
            P.dma("scalar", lambda e, gs_=gs_, it=it: e.dma_start(out=tab_b[it * 128:(it + 1) * 128, :], in_=GS[gs_][:, :]),
                  "st" + GSres[gs_], reads=[GSres[gs_] + "u", GSres[gs_] + "v", GSres[gs_]], writes=[TBres[gs_]])

        def rstd_from_ss(ss_ap, n, res, scale):
            P.op("scalar", lambda e: e.activation(out=ss_ap, in_=ss_ap, func=AF.Sqrt, scale=scale, bias=epsc[0:n, 0:1]), reads=[res, "epsc"], writes=[res])
            P.op("vector", lambda e: e.reciprocal(out=ss_ap, in_=ss_ap), reads=[res], writes=[res])

        def bcast_rstd(src_bank, src_res, dst, dst_res, N, scale):
            P.op("scalar", lambda e: e.activation(out=dst[:, 0:N], in_=src_bank[:, 0:N], func=AF.Sqrt, scale=scale, bias=epsc[:, 0:1]),
                 reads=[src_res, "epsc"], writes=[dst_res])
            P.op("vector", lambda e: e.reciprocal(out=dst[:, 0:N], in_=dst[:, 0:N]), reads=[dst_res], writes=[dst_res])

        def front(tiles, N, xs, preloaded=False):
            off = 0
            for i, (r0, n) in enumerate(tiles):
                xr = "xt%d" % xs[i]
                xi = xt[xs[i]]
                if not preloaded:
                    P.dma("sync", lambda e, xi=xi, i=i, r0=r0, n=n: e.dma_start(out=xi[0:n, :], in_=xh[r0:r0 + n, :]), "ldx%d" % xs[i], writes=[xr])
                ssr = "ss%d" % i
                P.op("scalar", lambda e, xi=xi, i=i, n=n: e.activation(out=junk[0:n, :], in_=xi[0:n, :], func=AF.Square, accum_out=ss[0:n, i:i + 1]),
                     reads=[xr], writes=[ssr] + JW)
                rstd_from_ss(ss[0:n, i:i + 1], n, ssr, 1.0 / D)
                P.op("scalar", lambda e, xi=xi, i=i, n=n: e.activation(out=hnb[0:n, :], in_=xi[0:n, :], func=AF.Copy, scale=ss[0:n, i:i + 1]),
                     reads=[xr, ssr], writes=["hnb"])
                for c in range(8):
                    P.op("tensor", lambda e, c=c, n=n: e.transpose(out=psT3[:, c, 0:n], in_=hnb[0:n, c * 128:(c + 1) * 128], identity=identb[0:n, 0:n]),
                         reads=["hnb", "identb"], writes=["bT"])
                P.op("vector", lambda e, n=n, off=off: e.tensor_copy(out=hnT[:, :, off:off + n], in_=psT3[:, :, 0:n]), reads=["bT"], writes=["hnT"])
                off += n

        def proj(col, b, N):
            for k in range(8):
                P.op("tensor", lambda e, k=k, col=col, b=b, N=N: e.matmul(pslot(b, N), lhsT=Wib[:, k, col * 128:(col + 1) * 128], rhs=hnT[:, k, 0:N],
                                                                            start=(k == 0), stop=(k == 7)),
                     reads=["Wib", "hnT"], writes=["p%d" % b])

        def mixer_pre():
            N = HALO
            front([(0, HALO)], N, [0])
            for k in range(4):
                proj(k, 0, N)
                proj(8 + k, 1, N)
                P.op("scalar", lambda e: e.activation(out=cgs[:, 0:N], in_=pslot(1, N), func=AF.Copy), reads=["p1"], writes=["cgs"])
                P.op("vector", lambda e, k=k: e.tensor_tensor(out=zb[k][:, 0:N], in0=pslot(0, N), in1=cgs[:, 0:N], op=ALU.mult),
                     reads=["p0", "cgs"], writes=["z%d" % k])
                proj(12 + k, 0, N)
                proj(16 + k, 1, N)
                P.op("scalar", lambda e: e.activation(out=cgs[:, 0:N], in_=pslot(1, N), func=AF.Sigmoid), reads=["p1"], writes=["cgs"])
                P.op("vector", lambda e, k=k: e.tensor_tensor(out=ub[k][:, 0:N], in0=pslot(0, N), in1=cgs[:, 0:N], op=ALU.mult),
                     reads=["p0", "cgs"], writes=["u%d" % k])

        def mixer_group(g):
            N = G
            base = HALO + g * G
            tiles = [(base, 128), (base + 128, 128)]
            xs = [2 * (g % 2), 2 * (g % 2) + 1]
            front(tiles, N, xs, preloaded=(g > 0))
            for k in range(4):
                proj(k, 0, N)
                proj(8 + k, 1, N)
                proj(4 + k, 2, N)
                zr = "z%d" % k
                P.op("scalar", lambda e: e.activation(out=cgs[:, 0:N], in_=pslot(1, N), func=AF.Copy), reads=["p1"], writes=["cgs"])
                if g > 0:
                    P.op("vector", lambda e, k=k: e.tensor_copy(out=zb[k][:, 0:HALO], in_=zb[k][:, G:G + HALO]), reads=[zr], writes=[zr])
                P.op("vector", lambda e, k=k: e.tensor_tensor(out=zb[k][:, HALO:HALO + N], in0=pslot(0, N), in1=cgs[:, 0:N], op=ALU.mult),
                     reads=["p0", "cgs"], writes=[zr])
                P.op("vector", lambda e, k=k: e.tensor_scalar(out=rt[:, 0:N], in0=zb[k][:, HALO - 2:HALO - 2 + N], scalar1=vcol(SCW + 3 * k), scalar2=None, op0=ALU.mult),
                     reads=[zr, "vecs"], writes=["rt"])
                for j in (1, 2):
                    P.op("vector", lambda e, k=k, j=j: e.scalar_tensor_tensor(out=rt[:, 0:N], in0=zb[k][:, HALO - 2 + j:HALO - 2 + j + N], scalar=vcol(SCW + 3 * k + j),
                                                                              in1=rt[:, 0:N], op0=ALU.mult, op1=ALU.add),
                         reads=[zr, "vecs", "rt"], writes=["rt"])
                yr = "ysc%d" % k
                P.op("vector", lambda e, k=k: e.tensor_tensor(out=ysc[k][:, 0:N], in0=pslot(2, N), in1=rt[:, 0:N], op=ALU.mult),
                     reads=["p2", "rt"], writes=[yr])
                sr = "sq%d" % (k % 2)
                P.op("scalar", lambda e, k=k: e.activation(out=sq[k % 2][:, 0:N], in_=ysc[k][:, 0:N], func=AF.Square), reads=[yr], writes=[sr])
                P.op("tensor", lambda e, k=k: e.matmul(bank(4, N), lhsT=onesf[:], rhs=sq[k % 2][:, 0:N], start=(k == 0), stop=(k == 3)),
                     reads=[sr, "onesf"], writes=["b4"])
            bcast_rstd(bank(4), "b4", mt, "mt", N, 1.0 / 512)
            for k in range(4):
                P.op("vector", lambda e, k=k: e.tensor_tensor(out=yn[k][:, 0:N], in0=ysc[k][:, 0:N], in1=mt[:, 0:N], op=ALU.mult),
                     reads=["ysc%d" % k, "mt"], writes=["yn%d" % k])
            if P.defer is not None:
                P.mark = len(P.defer)
            for k in range(4):
                proj(12 + k, 0, N)
                proj(16 + k, 1, N)
                ur = "u%d" % k
                cr = "cv%d" % k
                P.op("scalar", lambda e: e.activation(out=cgs[:, 0:N], in_=pslot(1, N), func=AF.Sigmoid), reads=["p1"], writes=["cgs"])
                if g > 0:
                    P.op("vector", lambda e, k=k: e.tensor_copy(out=ub[k][:, 0:HALO], in_=ub[k][:, G:G + HALO]), reads=[ur], writes=[ur])
                P.op("vector", lambda e, k=k: e.tensor_tensor(out=ub[k][:, HALO:HALO + N], in0=pslot(0, N), in1=cgs[:, 0:N], op=ALU.mult),
                     reads=["p0", "cgs"], writes=[ur])
                for j in range(31):
                    dn = (k * 31 + j) % len(dgs)
                    dr_ = "dg%d" % dn
                    if j % 2:
                        P.op("vector", lambda e, k=k, j=j, dn=dn: e.tensor_tensor(out=dgs[dn][:, :], in0=identb[:, :],
                                                                                 in1=vcol(CFW + 31 * k + j).to_broadcast([128, 128]), op=ALU.mult),
                             reads=["identb", "vecs"], writes=[dr_])
                    else:
                        P.op("scalar", lambda e, k=k, j=j, dn=dn: e.activation(out=dgs[dn][:, :], in_=identb[:, :], func=AF.Copy, scale=vcol(CFW + 31 * k + j)),
                             reads=["identb", "vecs"], writes=[dr_])
                    P.op("tensor", lambda e, k=k, j=j, dn=dn: e.matmul(pslot(3, N), lhsT=dgs[dn][:, :], rhs=ub[k][:, 2 + j:2 + j + N], start=(j == 0), stop=(j == 30)),
                         reads=[dr_, ur], writes=["p3"])
                P.op("vector", lambda e, k=k: e.tensor_scalar(out=cv[k][:, 0:N], in0=pslot(3, N), scalar1=vcol(CFB + k), scalar2=None, op0=ALU.add),
                     reads=["p3", "vecs"], writes=[cr])
                sr = "sq%d" % (k % 2)
                P.op("scalar", lambda e, k=k: e.activation(out=sq[k % 2][:, 0:N], in_=cv[k][:, 0:N], func=AF.Square), reads=[cr], writes=[sr])
                P.op("tensor", lambda e, k=k: e.matmul(bank(4, N), lhsT=onesf[:], rhs=cv[k][:, 0:N], start=(k == 0), stop=(k == 3)),
                     reads=[cr, "onesf"], writes=["b4"])
                P.op("tensor", lambda e, k=k: e.matmul(bank(5, N), lhsT=onesf[:], rhs=sq[k % 2][:, 0:N], start=(k == 0), stop=(k == 3)),
                     reads=[sr, "onesf"], writes=["b5"])
            P.op("vector", lambda e: e.tensor_scalar(out=mt[:, 0:N], in0=bank(4, N), scalar1=1.0 / 512, scalar2=None, op0=ALU.mult), reads=["b4"], writes=["mt"])
            P.op("vector", lambda e: e.tensor_tensor(out=sq[0][:, 0:N], in0=mt[:, 0:N], in1=mt[:, 0:N], op=ALU.mult), reads=["mt"], writes=["sq0"])
            P.op("vector", lambda e: e.scalar_tensor_tensor(out=rt[:, 0:N], in0=bank(5, N), scalar=1.0 / 512, in1=sq[0][:, 0:N], op0=ALU.mult, op1=ALU.subtract),
                 reads=["b5", "sq0"], writes=["rt"])
            P.op("vector", lambda e: e.tensor_scalar(out=rt[:, 0:N], in0=rt[:, 0:N], scalar1=EPS, scalar2=None, op0=ALU.add), reads=["rt"], writes=["rt"])
            P.op("scalar", lambda e: e.activation(out=rt[:, 0:N], in_=rt[:, 0:N], func=AF.Sqrt), reads=["rt"], writes=["rt"])
            P.op("vector", lambda e: e.reciprocal(out=rt[:, 0:N], in_=rt[:, 0:N]), reads=["rt"], writes=["rt"])
            for k in range(4):
                cr = "cv%d" % k
                P.op("vector", lambda e, k=k: e.tensor_tensor(out=cv[k][:, 0:N], in0=cv[k][:, 0:N], in1=mt[:, 0:N], op=ALU.subtract), reads=[cr, "mt"], writes=[cr])
                P.op("vector", lambda e, k=k: e.tensor_tensor(out=cv[k][:, 0:N], in0=cv[k][:, 0:N], in1=rt[:, 0:N], op=ALU.mult), reads=[cr, "rt"], writes=[cr])
                P.op("scalar", lambda e, k=k: e.activation(out=cv[k][:, 0:N], in_=cv[k][:, 0:N], func=AF.Silu, scale=vcol(LNG + k), bias=vcol(LNB + k)),
                     reads=[cr, "vecs"], writes=[cr])
                sr = "sq%d" % (k % 2)
                P.op("scalar", lambda e, k=k: e.activation(out=sq[k % 2][:, 0:N], in_=cv[k][:, 0:N], func=AF.Square), reads=[cr], writes=[sr])
                P.op("tensor", lambda e, k=k: e.matmul(bank(4, N), lhsT=onesf[:], rhs=sq[k % 2][:, 0:N], start=(k == 0), stop=(k == 3)),
                     reads=[sr, "onesf"], writes=["b4"])
            bcast_rstd(bank(4), "b4", mt, "mt", N, 1.0 / 512)
            for k in range(4):
                P.op("vector", lambda e, k=k: e.tensor_tensor(out=yn[4 + k][:, 0:N], in0=cv[k][:, 0:N], in1=mt[:, 0:N], op=ALU.mult),
                     reads=["cv%d" % k, "mt"], writes=["yn%d" % (4 + k)])
            for i in range(2):
                for half in range(2):
                    for kk in range(8):
                        P.op("tensor", lambda e, i=i, half=half, kk=kk: e.matmul(psA[:, (2 + half) * 512:(3 + half) * 512], lhsT=yn[kk][:, i * 128:(i + 1) * 128],
                                                                                  rhs=Wob[:, kk, half * 512:(half + 1) * 512], start=(kk == 0), stop=(kk == 7)),
                             reads=["yn%d" % kk, "Wob"], writes=["b%d" % (2 + half)])
                P.op("vector", lambda e, i=i: e.tensor_tensor(out=xt[xs[i]][:, :], in0=psA[:, 2 * 512:4 * 512], in1=xt[xs[i]][:, :], op=ALU.add),
                     reads=["b2", "b3", "xt%d" % xs[i]], writes=["xt%d" % xs[i]])

        def route(i, x_):
            xr = "xt%d" % x_
            h2 = xt[x_]
            idx = idx2[i]
            gates = gates2[i]
            ssc = ss[:, 2 + i:3 + i]
            ssr2 = "ss2_%d" % i
            idxr = "idx%d" % i
            gatesr = "gates%d" % i
            P.op("scalar", lambda e: e.activation(out=junk[:, :], in_=h2[:, :], func=AF.Square, accum_out=ssc), reads=[xr], writes=[ssr2] + JW)
            rstd_from_ss(ssc, 128, ssr2, 1.0 / D)
            P.op("scalar", lambda e: e.activation(out=hbf[i][:, :], in_=h2[:, :], func=AF.Copy, scale=ssc), reads=[xr, ssr2], writes=["hbf%d" % i])
            for c in range(8):
                P.op("tensor", lambda e, c=c: e.transpose(out=psT3[:, c, :], in_=hbf[i][:, c * 128:(c + 1) * 128], identity=identb[:, :]),
                     reads=["hbf%d" % i, "identb"], writes=["bT"])
            P.op("vector", lambda e: e.tensor_copy(out=hnT[:, :, 0:128], in_=psT3[:, :, :]), reads=["bT"], writes=["hnT"])
            for hf in range(2):
                for gg in range(8):
                    g = hf * 8 + gg
                    for k in range(8):
                        P.op("tensor", lambda e, g=g, gg=gg, k=k: e.matmul(psA[:, 2048 + gg * 128:2048 + (gg + 1) * 128], lhsT=Wqb[:, k, g * 128:(g + 1) * 128],
                                                                        rhs=hnT[:, k, 0:128], start=(k == 0), stop=(k == 7)),
                             reads=["Wqb", "hnT"], writes=["b%d" % (4 + gg // 4)])
                P.op("scalar", lambda e, hf=hf: e.activation(out=qTb[:, :], in_=psA[:, 2048:3072], func=AF.Copy), reads=["b4", "b5"], writes=["qTb"])
                for gg in range(8):
                    g = hf * 8 + gg
                    P.op("tensor", lambda e, g=g, gg=gg: e.matmul(psA[:, 2048 + gg * 128:2048 + (gg + 1) * 128], lhsT=qTb[:, gg * 128:(gg + 1) * 128], rhs=keysb[:, g, :],
                                                               start=True, stop=True),
                         reads=["qTb", "keysb"], writes=["b%d" % (4 + gg // 4)])
                P.op("scalar", lambda e: e.activation(out=Ssb[:, :], in_=psA[:, 2048:3072], func=AF.Copy), reads=["b4", "b5"], writes=["Ssb"])
                for gg in range(8):
                    g = hf * 8 + gg
                    sg_ = Ssb[:, gg * 128:(gg + 1) * 128]
                    v0 = Vt[:, g * 16:g * 16 + 8]
                    v1 = Vt[:, g * 16 + 8:g * 16 + 16]
                    i0 = It[:, g * 16:g * 16 + 8]
                    i1 = It[:, g * 16 + 8:g * 16 + 16]
                    P.op("vector", lambda e, sg_=sg_, v0=v0: e.max(out=v0, in_=sg_), reads=["Ssb"], writes=["Vt"])
                    P.op("vector", lambda e, sg_=sg_, v0=v0, i0=i0: e.max_index(out=i0, in_max=v0, in_values=sg_), reads=["Ssb", "Vt"], writes=["It"])
                    P.op("vector", lambda e, sg_=sg_, v0=v0: e.match_replace(out=S2[:, 0:128], in_to_replace=v0, in_values=sg_, imm_value=-1e30),
                         reads=["Ssb", "Vt"], writes=["S2"])
                    P.op("vector", lambda e, v1=v1: e.max(out=v1, in_=S2[:, 0:128]), reads=["S2"], writes=["Vt"])
                    P.op("vector", lambda e, v1=v1, i1=i1: e.max_index(out=i1, in_max=v1, in_values=S2[:, 0:128]), reads=["S2", "Vt"], writes=["It"])
            Vt4 = Vt[:].rearrange("p (h s a) -> p h s a", h=8, s=2)
            cand4 = cand[:].rearrange("p (h a b) -> p h a b", h=8, a=16)
            P.op("vector", lambda e: e.tensor_tensor(out=cand4, in0=Vt4[:, :, 0, :].unsqueeze(3).to_broadcast([128, 8, 16, 16]),
                                                      in1=Vt4[:, :, 1, :].unsqueeze(2).to_broadcast([128, 8, 16, 16]), op=ALU.add),
                 reads=["Vt"], writes=["cand"])
            for h in range(8):
                ch = cand[:, h * 256:(h + 1) * 256]
                v0 = CV[:, h * 16:h * 16 + 8]
                v1 = CV[:, h * 16 + 8:h * 16 + 16]
                p0 = CP[:, h * 16:h * 16 + 8]
                p1 = CP[:, h * 16 + 8:h * 16 + 16]
                P.op("vector", lambda e, ch=ch, v0=v0: e.max(out=v0, in_=ch), reads=["cand"], writes=["CV"])
                P.op("vector", lambda e, ch=ch, v0=v0, p0=p0: e.max_index(out=p0, in_max=v0, in_values=ch), reads=["cand", "CV"], writes=["CP"])
                P.op("vector", lambda e, ch=ch, v0=v0: e.match_replace(out=S2[:, :], in_to_replace=v0, in_values=ch, imm_value=-1e30),
                     reads=["cand", "CV"], writes=["S2"])
                P.op("vector", lambda e, v1=v1: e.max(out=v1, in_=S2[:, :]), reads=["S2"], writes=["CV"])
                P.op("vector", lambda e, v1=v1, p1=p1: e.max_index(out=p1, in_max=v1, in_values=S2[:, :]), reads=["S2", "CV"], writes=["CP"])
            CV3 = CV[:].rearrange("p (h k) -> p h k", h=8)
            ex3 = ex[:].rearrange("p (h k) -> p h k", h=8)
            g3 = gates[:].rearrange("p (h k) -> p h k", h=8)
            P.op("vector", lambda e: e.tensor_tensor(out=ex3, in0=CV3, in1=CV3[:, :, 0:1].to_broadcast([128, 8, 16]), op=ALU.subtract), reads=["CV"], writes=["ex"])
            P.op("scalar", lambda e: e.activation(out=ex[:, :], in_=ex[:, :], func=AF.Exp), reads=["ex"], writes=["ex"])
            P.op("vector", lambda e: e.tensor_reduce(out=Zs[:, 0:8], in_=ex3, axis=AX.X, op=ALU.add), reads=["ex"], writes=["Zs"])
            P.op("vector", lambda e: e.reciprocal(out=Zs[:, 0:8], in_=Zs[:, 0:8]), reads=["Zs"], writes=["Zs"])
            P.op("vector", lambda e: e.tensor_tensor(out=g3, in0=ex3, in1=Zs[:, 0:8].unsqueeze(2).to_broadcast([128, 8, 16]), op=ALU.mult),
                 reads=["ex", "Zs"], writes=[gatesr])
            P.op("vector", lambda e: e.tensor_single_scalar(out=au[:, :], in_=CP[:, :], scalar=4, op=ALU.logical_shift_right), reads=["CP"], writes=["au"])
            P.op("vector", lambda e: e.tensor_single_scalar(out=CP[:, :], in_=CP[:, :], scalar=15, op=ALU.bitwise_and), reads=["CP", "au"], writes=["CP"])
            P.op("vector", lambda e: e.tensor_copy(out=af[:, :], in_=au[:, :]), reads=["au"], writes=["af"])
            P.op("vector", lambda e: e.tensor_copy(out=bf[:, :], in_=CP[:, :]), reads=["CP"], writes=["bf"])
            P.op("vector", lambda e: e.tensor_copy(out=S2[:, :], in_=It[:, :]), reads=["It"], writes=["S2"])
            Itf4 = S2[:].rearrange("p (h s a) -> p h s a", h=8, s=2)
            oh4 = Ssb[:].rearrange("p (h k a) -> p h k a", h=4, k=16)
            io4 = iota16[:, :].unsqueeze(1).unsqueeze(1).to_broadcast([128, 4, 16, 16])
            for (srcf, s_, sres) in ((af, 0, "af"), (bf, 1, "bf")):
                s3 = srcf[:].rearrange("p (h k) -> p h k", h=8)
                for hh in range(2):
                    hs = slice(hh * 4, hh * 4 + 4)
                    P.op("vector", lambda e, s3=s3, hs=hs: e.tensor_tensor(out=oh4, in0=s3[:, hs, :].unsqueeze(3).to_broadcast([128, 4, 16, 16]), in1=io4, op=ALU.is_equal),
                         reads=[sres, "iota16"], writes=["Ssb"])
                    P.op("vector", lambda e, s_=s_, hs=hs: e.tensor_tensor(out=oh4, in0=oh4, in1=Itf4[:, hs, s_, :].unsqueeze(2).to_broadcast([128, 4, 16, 16]), op=ALU.mult),
                         reads=["Ssb", "S2"], writes=["Ssb"])
                    P.op("vector", lambda e, s3=s3, hs=hs: e.tensor_reduce(out=s3[:, hs, :], in_=oh4, axis=AX.X, op=ALU.add), reads=["Ssb"], writes=[sres])
            P.op("vector", lambda e: e.scalar_tensor_tensor(out=af[:, :], in0=af[:, :], scalar=128.0, in1=bf[:, :], op0=ALU.mult, op1=ALU.add),
                 reads=["af", "bf"], writes=["af"])
            P.op("vector", lambda e: e.tensor_copy(out=idx[:, :], in_=af[:, :]), reads=["af"], writes=[idxr])

        def experts(i, x_, orow, filler=None):
            xr = "xt%d" % x_
            h2 = xt[x_]
            rate = (sum(1 for e_, _ in filler if e_ != "tensor") // n_peer_j + 1) if filler else 0
            idx = idx2[i]
            gates = gates2[i]
            ssc = ss[:, 2 + i:3 + i]
            ssr2 = "ss2_%d" % i
            idxr = "idx%d" % i
            gatesr = "gates%d" % i

            def fill(n):
                while n > 0 and filler:
                    e_, th = filler.pop(0)
                    th()
                    if e_ != "tensor":
                        n -= 1
            nj = n_peer_j
            nb = nj // JB

            nj = n_peer_j
            def gather(j):
                s_ = j % NS
                P.dma("gpsimd", lambda e: e.indirect_dma_start(out=GS[s_][:, :], out_offset=None, in_=tab_b,
                                                               in_offset=bass.IndirectOffsetOnAxis(ap=idx[:, j:j + 1], axis=0)),
                      "g" + GSres[s_], reads=[idxr] + TBres, writes=[GSres[s_]])

            def dot(j):
                s_ = j % NS
                p_ = j % 2
                P.op("vector", lambda e: e.tensor_tensor(out=prod[p_][:, :], in0=GS[s_][:, 0:D], in1=hbf[i][:, :], op=ALU.mult),
                     reads=["hbf%d" % i], writes=["prod%d" % p_], weak=[GSres[s_]])
                P.op("scalar", lambda e: e.activation(out=prod[p_][:, :], in_=prod[p_][:, :], func=AF.Copy, accum_out=apre[:, j:j + 1]),
                     reads=["prod%d" % p_], writes=["prod%d" % p_, "apre%d" % j])

            def gelu(j):
                P.op("scalar", lambda e: e.activation(out=apre[:, j:j + 1], in_=apre[:, j:j + 1], func=AF.Gelu_apprx_tanh),
                     reads=["apre%d" % j], writes=["apre%d" % j])

            def acc(j):
                s_ = j % NS
                d_ = j % len(diag)
                P.op("vector", lambda e: e.scalar_tensor_tensor(out=diag[d_][:, :], in0=identb[:, :], scalar=apre[:, j:j + 1],
                                                                in1=gates[:, j:j + 1].to_broadcast([128, 128]), op0=ALU.mult, op1=ALU.mult),
                     reads=["identb", "apre%d" % j, gatesr], writes=["diag%d" % d_])
                for half in range(2):
                    P.op("tensor", lambda e, half=half: e.matmul(psA[:, half * 512:(half + 1) * 512], lhsT=diag[d_][:, :],
                                                                 rhs=GS[s_][:, D + half * 512:D + (half + 1) * 512], start=(j == 0), stop=(j == nj - 1)),
                         reads=["diag%d" % d_, GSres[s_]], writes=["b%d" % half])

            r1 = rate // 3
            r2 = (rate - r1) // 2
            r3 = rate - r1 - r2
            for t in range(nj + 3):
                if t < nj:
                    gather(t)
                fill(r3)
                if 0 <= t - 1 < nj:
                    dot(t - 1)
                fill(r2)
                if 0 <= t - 2 < nj:
                    gelu(t - 2)
                if 0 <= t - 3 < nj:
                    acc(t - 3)
                fill(r1)
            fill(100000)
            P.op("vector", lambda e: e.tensor_tensor(out=h2[:, :], in0=psA[:, 0:1024], in1=h2[:, :], op=ALU.add), reads=["b0", "b1", xr], writes=[xr])
            P.op("scalar", lambda e: e.activation(out=junk[:, :], in_=h2[:, :], func=AF.Square, accum_out=ss[:, 4 + i:5 + i]), reads=[xr], writes=["ss3_%d" % i] + JW)
            rstd_from_ss(ss[:, 4 + i:5 + i], 128, "ss3_%d" % i, 1.0 / D)
            P.op("vector", lambda e: e.scalar_tensor_tensor(out=h2[:, :], in0=h2[:, :], scalar=ss[:, 4 + i:5 + i], in1=gB[:, :], op0=ALU.mult, op1=ALU.mult),
                 reads=[xr, "ss3_%d" % i, "gB"], writes=[xr])
            P.dma("sync", lambda e: e.dma_start(out=out[orow:orow + 128, :], in_=h2[:, :]), "st%d" % x_, reads=[xr], writes=["out%d" % x_])

        mixer_pre()
        mixer_group(0)
        route(0, 0)
        for g in range(n_groups):
            x0 = 2 * (g % 2)
            P.defer = []
            route(1, x0 + 1)
            R1 = P.defer
            M, R0, ms = [], [], 0
            if g + 1 < n_groups:
                for i_ in range(2):
                    xs_ = 2 * ((g + 1) % 2) + i_
                    r0_ = HALO + (g + 1) * G + i_ * 128
                    P._dma("sync", lambda e, xs_=xs_, r0_=r0_: e.dma_start(out=xt[xs_][:, :], in_=xh[r0_:r0_ + 128, :]), "ldx%d" % xs_, writes=["xt%d" % xs_])
                P.defer = []
                P.mark = 0
                mixer_group(g + 1)
                M, ms = P.defer, P.mark
                P.defer = []
                route(0, 2 * ((g + 1) % 2))
                R0 = P.defer
            P.defer = None
            experts(0, x0, g * G, reorder(R1 + M[:ms]))
            experts(1, x0 + 1, g * G + 128, [(e_, th_) for (e_, th_, _r, _w) in M[ms:] + R0])
        P.wait_all("sync", ["out0", "out1", "out2", "out3"])
        P.emit()
    return nc


def _prep_shared(inp):
    f = lambda a: np.ascontiguousarray(np.asarray(a, dtype=np.float32))
    col = lambda v, n: np.asarray(v, np.float32).reshape(n, 128).T
    vecs = np.zeros((128, NVEC), np.float32)
    vecs[:, GM:GM + 8] = col(inp["norm_mix_g"][0], 8)
    vecs[:, GF:GF + 8] = col(inp["norm_ffn_g"][0], 8)
    vecs[:, GO:GO + 4] = col(inp["out_norm_g_sc"][0], 4)
    vecs[:, GO + 4:GO + 8] = col(inp["out_norm_g_cf"][0], 4)
    scw = np.asarray(inp["sc_conv_w"][0], np.float32)
    cfw = np.asarray(inp["cf_conv_w"][0], np.float32)
    for k in range(4):
        vecs[:, SCW + 3 * k:SCW + 3 * k + 3] = scw[:, k * 128:(k + 1) * 128].T
        vecs[:, CFW + 31 * k:CFW + 31 * k + 31] = cfw[:, k * 128:(k + 1) * 128].T
    vecs[:, CFB:CFB + 4] = col(inp["cf_conv_b"][0], 4)
    vecs[:, LNG:LNG + 4] = col(inp["cf_ln_g"][0], 4)
    vecs[:, LNB:LNB + 4] = col(inp["cf_ln_b"][0], 4)
    gB = np.concatenate([np.broadcast_to(np.asarray(inp["norm_ffn_g"][0], np.float32)[None, :], (128, D)),
                         np.broadcast_to(np.asarray(inp["final_norm_g"], np.float32)[None, :], (128, D))], axis=1)
    sk = np.asarray(inp["peer_sub_keys"][0], np.float32)
    keysT = np.ascontiguousarray(sk.reshape(16, 128, 128).transpose(2, 0, 1).reshape(128, 2048))
    return {
        "w_in": f(inp["w_in"][0]), "w_out": f(inp["w_out"][0]), "w_q": f(inp["peer_w_q"][0]),
        "keysT": keysT, "uv_tab": np.ascontiguousarray(np.concatenate([f(inp["peer_u"][0]), f(inp["peer_v"][0])], axis=1)),
        "vecs_h": vecs, "gB_h": np.ascontiguousarray(gB),
    }


def _core_x(inp, core, n_groups=16):
    x = np.asarray(inp["x"], np.float32)
    meta = np.asarray(inp["meta_tokens"], np.float32)
    b, half = core // 2, core % 2
    ntok = n_groups * G
    if half == 0:
        halo = np.concatenate([np.zeros((HALO - meta.shape[0], D), np.float32), meta], axis=0)
        body = x[b, 0:ntok]
    else:
        halo = x[b, 4096 - HALO:4096]
        body = x[b, 4096:4096 + ntok]
    return np.ascontiguousarray(np.concatenate([halo, body], axis=0))


def kernel(**inputs):
    shared = _prep_shared(inputs)
    nc = build(16)
    in_maps = []
    for c in range(N_CORES):
        m = dict(shared)
        m["xh"] = _core_x(inputs, c)
        in_maps.append(m)
    res = run_bass_kernel_spmd(nc, in_maps, core_ids=list(range(N_CORES)))
    outp = np.empty((4, 8192, D), np.float32)
    for c in range(N_CORES):
        b, half = c // 2, c % 2
        outp[b, half * 4096:(half + 1) * 4096] = res.results[c]["out"]
    return outp
```

```python
import numpy as np
from contextlib import ExitStack
import concourse.bass as bass
import concourse.mybir as mybir
from concourse.bass_utils import run_bass_kernel_spmd

F32 = mybir.dt.float32
BF16 = mybir.dt.bfloat16
U32 = mybir.dt.uint32
ALU = mybir.AluOpType
AF = mybir.ActivationFunctionType
AX = mybir.AxisListType

D = 1024
G = 256
HALO = 32
N_CORES = 8
EPS = 1e-6
NS = 10
JB = 4
GM, GF, GO, SCW, CFW, CFB, LNG, LNB, NVEC = 0, 8, 16, 24, 36, 160, 164, 168, 172


class Prog:
    ENGS = ("sync", "scalar", "vector", "gpsimd", "tensor")

    def __init__(self, nc, stack):
        self.nc = nc
        self.stack = stack
        self.q = {e: [] for e in self.ENGS}
        self.cnt = {e: 0 for e in self.ENGS}
        self.sem = {e: stack.enter_context(nc.semaphore("c_" + e)) for e in self.ENGS}
        self.seen = {e: {} for e in self.ENGS}
        self.last_w = {}
        self.readers = {}
        self.dsem = {}
        self.dcnt = {}
        self.alias = {}
        self.defer = None

    def _exp(self, names):
        out = []
        for n in names:
            out.extend(self.alias.get(n, (n,)))
        return out

    def _deps(self, e, reads, writes):
        deps = {}

        def add(ev):
            if ev is None:
                return
            k, s, v = ev
            if k not in deps or deps[k][1] < v:
                deps[k] = (s, v)

        for r in reads:
            add(self.last_w.get(r))
        for w in writes:
            rd = self.readers.get(w, ())
            if not rd:
                add(self.last_w.get(w))
            for ev in rd:
                add(ev)
        for k, (s, v) in deps.items():
            if e == "tensor" and k == "e_tensor":
                continue
            if self.seen[e].get(k, 0) < v:
                self.seen[e][k] = v
                self.q[e].append(lambda eng, s=s, v=v: eng.wait_ge(s, v))

    def _commit(self, ev, reads, writes):
        for w in writes:
            self.last_w[w] = ev
            self.readers[w] = []
        for r in reads:
            if r not in writes:
                self.readers.setdefault(r, []).append(ev)

    def op(self, e, fn, reads=(), writes=(), weak=()):
        if self.defer is not None:
            self.defer.append((e, lambda: self._op(e, fn, reads, writes, weak),
                               set(self._exp(reads)) | set(self._exp(weak)), set(self._exp(writes))))
            return
        self._op(e, fn, reads, writes, weak)

    def _op(self, e, fn, reads=(), writes=(), weak=()):
        reads, writes, weak = self._exp(reads), self._exp(writes), self._exp(weak)
        self._deps(e, list(reads) + list(weak), writes)
        self.cnt[e] += 1
        n = self.cnt[e]
        s = self.sem[e]
        self.q[e].append(lambda eng, fn=fn, s=s: fn(eng).then_inc(s, 1))
        self._commit(("e_" + e, s, n), reads, writes)

    def dma(self, e, fn, key, reads=(), writes=()):
        if self.defer is not None:
            self.defer.append((e, lambda: self._dma(e, fn, key, reads, writes), set(self._exp(reads)), set(self._exp(writes))))
            return
        self._dma(e, fn, key, reads, writes)

    def _dma(self, e, fn, key, reads=(), writes=()):
        reads, writes = self._exp(reads), self._exp(writes)
        self._deps(e, reads, writes)
        if key not in self.dsem:
            self.dsem[key] = self.stack.enter_context(self.nc.semaphore("d_" + key))
            self.dcnt[key] = 0
        s = self.dsem[key]
        self.dcnt[key] += 16
        v = self.dcnt[key]
        self.q[e].append(lambda eng, fn=fn, s=s: fn(eng).then_inc(s, 16))
        self._commit(("d_" + key, s, v), reads, writes)

    def wait_all(self, e, resources):
        self._deps(e, self._exp(resources), ())

    def emit(self):
        with self.nc.Block() as block:
            @block.sync
            def _(eng):
                for f in self.q["sync"]:
                    f(eng)

            @block.scalar
            def _(eng):
                for f in self.q["scalar"]:
                    f(eng)

            @block.vector
            def _(eng):
                for f in self.q["vector"]:
                    f(eng)

            @block.gpsimd
            def _(eng):
                for f in self.q["gpsimd"]:
                    f(eng)

            @block.tensor
            def _(eng):
                for f in self.q["tensor"]:
                    f(eng)


PE_BANK = {"p0": "B2", "p1": "B2", "p2": "B3", "p3": "B3", "b0": "B0", "b1": "B1", "b4": "B4", "b5": "B5", "bT": "BT"}


def reorder(entries, dmin=6):
    units = []
    for (e, th, rd, wr) in entries:
        rd, wr = set(rd), set(wr)
        if e == "tensor":
            wr |= {"pe_" + PE_BANK[w] for w in wr if w in PE_BANK}
            if units and units[-1]["pe"]:
                u = units[-1]
                u["th"].append((e, th)); u["rd"] |= rd; u["wr"] |= wr
                continue
        units.append({"pe": e == "tensor", "th": [(e, th)], "rd": rd, "wr": wr})
    n = len(units)
    preds = [set() for _ in range(n)]
    succs = [set() for _ in range(n)]
    last_w, readers = {}, {}
    for i, u in enumerate(units):
        for r in u["rd"]:
            if r in last_w:
                preds[i].add(last_w[r])
        for w in u["wr"]:
            if w in last_w:
                preds[i].add(last_w[w])
            preds[i].update(readers.get(w, ()))
        for w in u["wr"]:
            last_w[w] = i
            readers[w] = []
        for r in u["rd"]:
            if r not in u["wr"]:
                readers.setdefault(r, []).append(i)
        preds[i].discard(i)
        for p in preds[i]:
            succs[p].add(i)
    cp = [1] * n
    for i in range(n - 1, -1, -1):
        for q in succs[i]:
            cp[i] = max(cp[i], 1 + cp[q])
    npred = [len(p) for p in preds]
    ready = [i for i in range(n) if npred[i] == 0]
    pos = {}
    out = []
    while ready:
        step = len(pos)

        def key(i):
            last = max((pos[p] for p in preds[i]), default=-10 ** 6)
            far = (step - last) >= dmin
            return (0 if far else 1, -cp[i] if far else last, i)

        b = min(ready, key=key)
        ready.remove(b)
        pos[b] = step
        out.extend(units[b]["th"])
        for q in succs[b]:
            npred[q] -= 1
            if npred[q] == 0:
                ready.append(q)
    assert len(pos) == n
    return out


def build(n_groups=16, n_peer_j=128):
    nc = bass.Bass("TRN2", target_bir_lowering=False)
    ntok = HALO + n_groups * G
    dr = lambda name, shape, kind="ExternalInput", dt=F32: nc.dram_tensor(name, shape, dt, kind=kind).ap()
    xh = dr("xh", [ntok, D])
    w_in = dr("w_in", [D, 2560])
    w_out = dr("w_out", [D, D])
    w_q = dr("w_q", [D, 2048])
    keysT = dr("keysT", [128, 2048])
    uv_tab = dr("uv_tab", [16384, 2 * D])
    tab_b = dr("tab_b", [16384, 2 * D], kind="Internal", dt=BF16)
    vecs_d = dr("vecs_h", [128, NVEC])
    gB_d = dr("gB_h", [128, 2048])
    out = dr("out", [n_groups * G, D], kind="ExternalOutput")

    with ExitStack() as st:
        P = Prog(nc, st)
        sb = lambda name, shape, dt=F32: st.enter_context(nc.sbuf_tensor(name, shape, dt))
        Wib = sb("Wib", [128, 8, 2560], BF16)
        Wob = sb("Wob", [128, 8, 1024], BF16)
        Wqb = sb("Wqb", [128, 8, 2048], BF16)
        keysb = sb("keysb", [128, 16, 128], BF16)
        vecs = sb("vecs", [128, NVEC])
        gB = sb("gB", [128, D])
        identb = sb("identb", [128, 128], BF16)
        onesf = sb("onesf", [128, 128])
        iota16 = sb("iota16", [128, 16])
        xt = [sb("xt%d" % i, [128, D]) for i in range(4)]
        hnb = sb("hnb", [128, D], BF16)
        ss = sb("ss", [128, 8])
        hnT = sb("hnT", [128, 8, G], BF16)
        cgs = sb("cgs", [128, G])
        zb = [sb("z%d" % k, [128, HALO + G]) for k in range(4)]
        mixA = sb("mixA", [128, 2048])
        ysc = [mixA[:, k * G:(k + 1) * G] for k in range(4)]
        sq = [sb("sq%d" % k, [128, G]) for k in range(2)]
        ub = [sb("u%d" % k, [128, HALO + G], BF16) for k in range(4)]
        dgs = [sb("dg%d" % k, [128, 128], BF16) for k in range(4)]
        cv = [mixA[:, 1024 + k * G:1024 + (k + 1) * G] for k in range(4)]
        mt = sb("mt", [128, G])
        rt = sb("rt", [128, G])
        qTb = sb("qTb", [128, 1024], BF16)
        Ssb = sb("Ssb", [128, 1024])
        S2 = sb("S2", [128, 256])
        Vt = sb("Vt", [128, 256])
        It = sb("It", [128, 256], U32)
        cand = mixA
        CV = sb("CV", [128, 128])
        CP = sb("CP", [128, 128], U32)
        au = sb("au", [128, 128], U32)
        af = sb("af", [128, 128])
        bf = sb("bf", [128, 128])
        idx2 = [sb("idx%d" % i, [128, 128], U32) for i in range(2)]
        ex = sb("ex", [128, 128])
        Zs = sb("Zs", [128, 8])
        gates2 = [sb("gates%d" % i, [128, 128]) for i in range(2)]
        apre = sb("apre", [128, 128])
        diag = [sb("diag%d" % i, [128, 128], BF16) for i in range(4)]
        GS = [sb("GS%d" % i, [128, 2 * D], BF16) for i in range(NS)]
        yn = [sb("yn%d" % k, [128, G], BF16) for k in range(8)]
        psA = st.enter_context(nc.psum_tensor("psA", [128, 6 * 512], F32))
        psT = st.enter_context(nc.psum_tensor("psT", [128, 1024], BF16))
        pslot = lambda s_, n: psA[:, 1024 + s_ * 256:1024 + s_ * 256 + n]
        bank = lambda i, n=512: psA[:, i * 512:i * 512 + n]
        psT3 = psT[:].rearrange("p (c t) -> p c t", c=8)
        prod = [sb("prod%d" % i, [128, D], BF16) for i in range(2)]
        hbf = [sb("hbf%d" % i, [128, D], BF16) for i in range(2)]
        junk = prod[1][:, :]
        JW = ["prod1"]
        JR = []

        vcol = lambda c: vecs[:, c:c + 1]
        epsc = ss[:, 7:8]
        P.op("vector", lambda e: e.memset(epsc, EPS), writes=["epsc"])
        P.alias["cand"] = ["ysc%d" % k for k in range(4)] + ["cv%d" % k for k in range(4)]
        P.alias["Vt"] = ["Vt%d" % k for k in range(16)]
        P.alias["It"] = ["It%d" % k for k in range(16)]
        P.alias["CV"] = ["CV%d" % k for k in range(8)]
        P.alias["CP"] = ["CP%d" % k for k in range(8)]
        P.alias["S2"] = ["S2_0", "S2_1"]
        P.alias["Ssb"] = ["Ssb0", "Ssb1", "Ssb2"]
        P.alias["mixlo"] = ["ysc%d" % k for k in range(4)]
        P.alias["mixhi"] = ["cv%d" % k for k in range(4)]
        P.alias["b2"] = ["p0", "p1"]
        P.alias["b3"] = ["p2", "p3"]

        P.dma("sync", lambda e: e.dma_start(out=vecs[:], in_=vecs_d), "ldv", writes=["vecs"])
        P.dma("sync", lambda e: e.dma_start(out=gB[:], in_=gB_d[:, D:2 * D]), "ldg", writes=["gB"])
        P.dma("sync", lambda e: e.dma_start(out=xt[0][:, :], in_=gB_d[:, 0:D]), "ldx0", writes=["xt0"])
        P.op("gpsimd", lambda e: e.memset(apre[:], 1.0), writes=["apre_setup"])
        P.op("gpsimd", lambda e: e.affine_select(out=apre[:], in_=apre[:], pattern=[[-1, 128]], compare_op=ALU.is_equal,
                                                  fill=0.0, base=0, channel_multiplier=1), reads=["apre_setup"], writes=["apre_setup"])
        P.op("vector", lambda e: e.tensor_copy(out=identb[:], in_=apre[:]), reads=["apre_setup"], writes=["identb"])
        P.op("gpsimd", lambda e: e.memset(onesf[:], 1.0), writes=["onesf"])
        P.op("gpsimd", lambda e: e.iota(iota16[:], pattern=[[1, 16]], base=0, channel_multiplier=0,
                                        allow_small_or_imprecise_dtypes=True), writes=["iota16"])
        stage = [(mixA[:, 0:1024], "mixlo"), (mixA[:, 1024:2048], "mixhi"), (xt[1][:, :], "xt1"), (xt[2][:, :], "xt2")]
        si = 0
        for (Wd, Wb, ncol, gcol, nm) in ((w_in, Wib, 2560, GM, "Wib"), (w_out, Wob, 1024, GO, "Wob"), (w_q, Wqb, 2048, GF, "Wqb")):
            for c in range(8):
                for c0 in range(0, ncol, 1024):
                    w = min(1024, ncol - c0)
                    sbuf_, rn = stage[si % 4]
                    si += 1
                    P.dma("sync" if si % 2 else "scalar",
                          lambda e, sbuf_=sbuf_, Wd=Wd, c=c, c0=c0, w=w: e.dma_start(out=sbuf_[:, 0:w], in_=Wd[c * 128:(c + 1) * 128, c0:c0 + w]),
                          "ld" + rn, writes=[rn])
                    if si % 2:
                        P.op("vector", lambda e, sbuf_=sbuf_, Wb=Wb, c=c, c0=c0, w=w, gcol=gcol: e.tensor_scalar(
                            out=Wb[:, c, c0:c0 + w], in0=sbuf_[:, 0:w], scalar1=vcol(gcol + c), scalar2=None, op0=ALU.mult),
                            reads=[rn, "vecs"], writes=[nm])
                    else:
                        P.op("scalar", lambda e, sbuf_=sbuf_, Wb=Wb, c=c, c0=c0, w=w, gcol=gcol: e.activation(
                            out=Wb[:, c, c0:c0 + w], in_=sbuf_[:, 0:w], func=AF.Copy, scale=vcol(gcol + c)),
                            reads=[rn, "vecs"], writes=[nm])
        for c0 in range(0, 2048, 1024):
            sbuf_, rn = stage[si % 4]
            si += 1
            P.dma("sync", lambda e, sbuf_=sbuf_, c0=c0: e.dma_start(out=sbuf_[:, :], in_=keysT[:, c0:c0 + 1024]), "ld" + rn, writes=[rn])
            P.op("vector", lambda e, sbuf_=sbuf_, c0=c0: e.tensor_copy(out=keysb[:].rearrange("p g n -> p (g n)")[:, c0:c0 + 1024], in_=sbuf_[:, :]),
                 reads=[rn], writes=["keysb"])
        GSres = ["GS%d" % i for i in range(NS)]
        TBres = ["tab_b%d" % i for i in range(NS)]
        ustage = [(mixA[:, 0:1024], "mixlo"), (xt[1][:, :], "xt1"), (xt[3][:, :], "xt3")]
        vstage = [(mixA[:, 1024:2048], "mixhi"), (xt[2][:, :], "xt2")]
        for it in range(128):
            su_, ru = ustage[it % 3]
            sv_, rv = vstage[it % 2]
            gs_ = it % NS
            P.dma("sync", lambda e, su_=su_, it=it: e.dma_start(out=su_[:, :], in_=uv_tab[it * 128:(it + 1) * 128, 0:D]), "ldu" + ru, writes=[ru])
            P.dma("sync", lambda e, sv_=sv_, it=it: e.dma_start(out=sv_[:, :], in_=uv_tab[it * 128:(it + 1) * 128, D:2 * D]), "ldv" + rv, writes=[rv])
            P.op("vector", lambda e, su_=su_, gs_=gs_: e.tensor_tensor(out=GS[gs_][:, 0:D], in0=su_[:, :], in1=xt[0][:, :], op=ALU.mult),
                 reads=[ru, "xt0"], writes=[GSres[gs_] + "u"])
            P.op("scalar", lambda e, sv_=sv_, gs_=gs_: e.activation(out=GS[gs_][:, D:2 * D], in_=sv_[:, :], func=AF.Copy), reads=[rv], writes=[GSres[gs_] + "v"])
            P.dma("scalar", lambda e, gs_=gs_, it=it: e.dma_start(out=tab_b[it * 128:(it + 1) * 128, :], in_=GS[gs_][:, :]),
                  "st" + GSres[gs_], reads=[GSres[gs_] + "u", GSres[gs_] + "v", GSres[gs_]], writes=[TBres[gs_]])

        def rstd_from_ss(ss_ap, n, res, scale):
            P.op("scalar", lambda e: e.activation(out=ss_ap, in_=ss_ap, func=AF.Sqrt, scale=scale, bias=epsc[0:n, 0:1]), reads=[res, "epsc"], writes=[res])
            P.op("vector", lambda e: e.reciprocal(out=ss_ap, in_=ss_ap), reads=[res], writes=[res])

        def bcast_rstd(src_bank, src_res, dst, dst_res, N, scale):
            P.op("scalar", lambda e: e.activation(out=dst[:, 0:N], in_=src_bank[:, 0:N], func=AF.Sqrt, scale=scale, bias=epsc[:, 0:1]),
                 reads=[src_res, "epsc"], writes=[dst_res])
            P.op("vector", lambda e: e.reciprocal(out=dst[:, 0:N], in_=dst[:, 0:N]), reads=[dst_res], writes=[dst_res])

        def front(tiles, N, xs, preloaded=False):
            off = 0
            for i, (r0, n) in enumerate(tiles):
                xr = "xt%d" % xs[i]
                xi = xt[xs[i]]
                if not preloaded:
                    P.dma("sync", lambda e, xi=xi, i=i, r0=r0, n=n: e.dma_start(out=xi[0:n, :], in_=xh[r0:r0 + n, :]), "ldx%d" % xs[i], writes=[xr])
                ssr = "ss%d" % i
                P.op("scalar", lambda e, xi=xi, i=i, n=n: e.activation(out=junk[0:n, :], in_=xi[0:n, :], func=AF.Square, accum_out=ss[0:n, i:i + 1]),
                     reads=[xr], writes=[ssr] + JW)
                rstd_from_ss(ss[0:n, i:i + 1], n, ssr, 1.0 / D)
                P.op("scalar", lambda e, xi=xi, i=i, n=n: e.activation(out=hnb[0:n, :], in_=xi[0:n, :], func=AF.Copy, scale=ss[0:n, i:i + 1]),
                     reads=[xr, ssr], writes=["hnb"])
                for c in range(8):
                    P.op("tensor", lambda e, c=c, n=n: e.transpose(out=psT3[:, c, 0:n], in_=hnb[0:n, c * 128:(c + 1) * 128], identity=identb[0:n, 0:n]),
                         reads=["hnb", "identb"], writes=["bT"])
                P.op("vector", lambda e, n=n, off=off: e.tensor_copy(out=hnT[:, :, off:off + n], in_=psT3[:, :, 0:n]), reads=["bT"], writes=["hnT"])
                off += n

        def proj(col, b, N):
            for k in range(8):
                P.op("tensor", lambda e, k=k, col=col, b=b, N=N: e.matmul(pslot(b, N), lhsT=Wib[:, k, col * 128:(col + 1) * 128], rhs=hnT[:, k, 0:N],
                                                                            start=(k == 0), stop=(k == 7)),
                     reads=["Wib", "hnT"], writes=["p%d" % b])

        def mixer_pre():
            N = HALO
            front([(0, HALO)], N, [0])
            for k in range(4):
                proj(k, 0, N)
                proj(8 + k, 1, N)
                P.op("scalar", lambda e: e.activation(out=cgs[:, 0:N], in_=pslot(1, N), func=AF.Copy), reads=["p1"], writes=["cgs"])
                P.op("vector", lambda e, k=k: e.tensor_tensor(out=zb[k][:, 0:N], in0=pslot(0, N), in1=cgs[:, 0:N], op=ALU.mult),
                     reads=["p0", "cgs"], writes=["z%d" % k])
                proj(12 + k, 0, N)
                proj(16 + k, 1, N)
                P.op("scalar", lambda e: e.activation(out=cgs[:, 0:N], in_=pslot(1, N), func=AF.Sigmoid), reads=["p1"], writes=["cgs"])
                P.op("vector", lambda e, k=k: e.tensor_tensor(out=ub[k][:, 0:N], in0=pslot(0, N), in1=cgs[:, 0:N], op=ALU.mult),
                     reads=["p0", "cgs"], writes=["u%d" % k])

        def mixer_group(g):
            N = G
            base = HALO + g * G
            tiles = [(base, 128), (base + 128, 128)]
            xs = [2 * (g % 2), 2 * (g % 2) + 1]
            front(tiles, N, xs, preloaded=(g > 0))
            for k in range(4):
                proj(k, 0, N)
                proj(8 + k, 1, N)
                proj(4 + k, 2, N)
                zr = "z%d" % k
                P.op("scalar", lambda e: e.activation(out=cgs[:, 0:N], in_=pslot(1, N), func=AF.Copy), reads=["p1"], writes=["cgs"])
                if g > 0:
                    P.op("vector", lambda e, k=k: e.tensor_copy(out=zb[k][:, 0:HALO], in_=zb[k][:, G:G + HALO]), reads=[zr], writes=[zr])
                P.op("vector", lambda e, k=k: e.tensor_tensor(out=zb[k][:, HALO:HALO + N], in0=pslot(0, N), in1=cgs[:, 0:N], op=ALU.mult),
                     reads=["p0", "cgs"], writes=[zr])
                P.op("vector", lambda e, k=k: e.tensor_scalar(out=rt[:, 0:N], in0=zb[k][:, HALO - 2:HALO - 2 + N], scalar1=vcol(SCW + 3 * k), scalar2=None, op0=ALU.mult),
                     reads=[zr, "vecs"], writes=["rt"])
                for j in (1, 2):
                    P.op("vector", lambda e, k=k, j=j: e.scalar_tensor_tensor(out=rt[:, 0:N], in0=zb[k][:, HALO - 2 + j:HALO - 2 + j + N], scalar=vcol(SCW + 3 * k + j),
                                                                              in1=rt[:, 0:N], op0=ALU.mult, op1=ALU.add),
                         reads=[zr, "vecs", "rt"], writes=["rt"])
                yr = "ysc%d" % k
                P.op("vector", lambda e, k=k: e.tensor_tensor(out=ysc[k][:, 0:N], in0=pslot(2, N), in1=rt[:, 0:N], op=ALU.mult),
                     reads=["p2", "rt"], writes=[yr])
                sr = "sq%d" % (k % 2)
                P.op("scalar", lambda e, k=k: e.activation(out=sq[k % 2][:, 0:N], in_=ysc[k][:, 0:N], func=AF.Square), reads=[yr], writes=[sr])
                P.op("tensor", lambda e, k=k: e.matmul(bank(4, N), lhsT=onesf[:], rhs=sq[k % 2][:, 0:N], start=(k == 0), stop=(k == 3)),
                     reads=[sr, "onesf"], writes=["b4"])
            bcast_rstd(bank(4), "b4", mt, "mt", N, 1.0 / 512)
            for k in range(4):
                P.op("vector", lambda e, k=k: e.tensor_tensor(out=yn[k][:, 0:N], in0=ysc[k][:, 0:N], in1=mt[:, 0:N], op=ALU.mult),
                     reads=["ysc%d" % k, "mt"], writes=["yn%d" % k])
            if P.defer is not None:
                P.mark = len(P.defer)
            for k in range(4):
                proj(12 + k, 0, N)
                proj(16 + k, 1, N)
                ur = "u%d" % k
                cr = "cv%d" % k
                P.op("scalar", lambda e: e.activation(out=cgs[:, 0:N], in_=pslot(1, N), func=AF.Sigmoid), reads=["p1"], writes=["cgs"])
                if g > 0:
                    P.op("vector", lambda e, k=k: e.tensor_copy(out=ub[k][:, 0:HALO], in_=ub[k][:, G:G + HALO]), reads=[ur], writes=[ur])
                P.op("vector", lambda e, k=k: e.tensor_tensor(out=ub[k][:, HALO:HALO + N], in0=pslot(0, N), in1=cgs[:, 0:N], op=ALU.mult),
                     reads=["p0", "cgs"], writes=[ur])
                for j in range(31):
                    dn = (k * 31 + j) % len(dgs)
                    dr_ = "dg%d" % dn
                    if j % 2:
                        P.op("vector", lambda e, k=k, j=j, dn=dn: e.tensor_tensor(out=dgs[dn][:, :], in0=identb[:, :],
                                                                                 in1=vcol(CFW + 31 * k + j).to_broadcast([128, 128]), op=ALU.mult),
                             reads=["identb", "vecs"], writes=[dr_])
                    else:
                        P.op("scalar", lambda e, k=k, j=j, dn=dn: e.activation(out=dgs[dn][:, :], in_=identb[:, :], func=AF.Copy, scale=vcol(CFW + 31 * k + j)),
                             reads=["identb", "vecs"], writes=[dr_])
                    P.op("tensor", lambda e, k=k, j=j, dn=dn: e.matmul(pslot(3, N), lhsT=dgs[dn][:, :], rhs=ub[k][:, 2 + j:2 + j + N], start=(j == 0), stop=(j == 30)),
                         reads=[dr_, ur], writes=["p3"])
                P.op("vector", lambda e, k=k: e.tensor_scalar(out=cv[k][:, 0:N], in0=pslot(3, N), scalar1=vcol(CFB + k), scalar2=None, op0=ALU.add),
                     reads=["p3", "vecs"], writes=[cr])
                sr = "sq%d" % (k % 2)
                P.op("scalar", lambda e, k=k: e.activation(out=sq[k % 2][:, 0:N], in_=cv[k][:, 0:N], func=AF.Square), reads=[cr], writes=[sr])
                P.op("tensor", lambda e, k=k: e.matmul(bank(4, N), lhsT=onesf[:], rhs=cv[k][:, 0:N], start=(k == 0), stop=(k == 3)),
                     reads=[cr, "onesf"], writes=["b4"])
                P.op("tensor", lambda e, k=k: e.matmul(bank(5, N), lhsT=onesf[:], rhs=sq[k % 2][:, 0:N], start=(k == 0), stop=(k == 3)),
                     reads=[sr, "onesf"], writes=["b5"])
            P.op("vector", lambda e: e.tensor_scalar(out=mt[:, 0:N], in0=bank(4, N), scalar1=1.0 / 512, scalar2=None, op0=ALU.mult), reads=["b4"], writes=["mt"])
            P.op("vector", lambda e: e.tensor_tensor(out=sq[0][:, 0:N], in0=mt[:, 0:N], in1=mt[:, 0:N], op=ALU.mult), reads=["mt"], writes=["sq0"])
            P.op("vector", lambda e: e.scalar_tensor_tensor(out=rt[:, 0:N], in0=bank(5, N), scalar=1.0 / 512, in1=sq[0][:, 0:N], op0=ALU.mult, op1=ALU.subtract),
                 reads=["b5", "sq0"], writes=["rt"])
            P.op("vector", lambda e: e.tensor_scalar(out=rt[:, 0:N], in0=rt[:, 0:N], scalar1=EPS, scalar2=None, op0=ALU.add), reads=["rt"], writes=["rt"])
            P.op("scalar", lambda e: e.activation(out=rt[:, 0:N], in_=rt[:, 0:N], func=AF.Sqrt), reads=["rt"], writes=["rt"])
            P.op("vector", lambda e: e.reciprocal(out=rt[:, 0:N], in_=rt[:, 0:N]), reads=["rt"], writes=["rt"])
            for k in range(4):
                cr = "cv%d" % k
                P.op("vector", lambda e, k=k: e.tensor_tensor(out=cv[k][:, 0:N], in0=cv[k][:, 0:N], in1=mt[:, 0:N], op=ALU.subtract), reads=[cr, "mt"], writes=[cr])
                P.op("vector", lambda e, k=k: e.tensor_tensor(out=cv[k][:, 0:N], in0=cv[k][:, 0:N], in1=rt[:, 0:N], op=ALU.mult), reads=[cr, "rt"], writes=[cr])
                P.op("scalar", lambda e, k=k: e.activation(out=cv[k][:, 0:N], in_=cv[k][:, 0:N], func=AF.Silu, scale=vcol(LNG + k), bias=vcol(LNB + k)),
                     reads=[cr, "vecs"], writes=[cr])
                sr = "sq%d" % (k % 2)
                P.op("scalar", lambda e, k=k: e.activation(out=sq[k % 2][:, 0:N], in_=cv[k][:, 0:N], func=AF.Square), reads=[cr], writes=[sr])
                P.op("tensor", lambda e, k=k: e.matmul(bank(4, N), lhsT=onesf[:], rhs=sq[k % 2][:, 0:N], start=(k == 0), stop=(k == 3)),
                     reads=[sr, "onesf"], writes=["b4"])
            bcast_rstd(bank(4), "b4", mt, "mt", N, 1.0 / 512)
            for k in range(4):
                P.op("vector", lambda e, k=k: e.tensor_tensor(out=yn[4 + k][:, 0:N], in0=cv[k][:, 0:N], in1=mt[:, 0:N], op=ALU.mult),
                     reads=["cv%d" % k, "mt"], writes=["yn%d" % (4 + k)])
            for i in range(2):
                for half in range(2):
                    for kk in range(8):
                        P.op("tensor", lambda e, i=i, half=half, kk=kk: e.matmul(psA[:, (2 + half) * 512:(3 + half) * 512], lhsT=yn[kk][:, i * 128:(i + 1) * 128],
                                                                                  rhs=Wob[:, kk, half * 512:(half + 1) * 512], start=(kk == 0), stop=(kk == 7)),
                             reads=["yn%d" % kk, "Wob"], writes=["b%d" % (2 + half)])
                P.op("vector", lambda e, i=i: e.tensor_tensor(out=xt[xs[i]][:, :], in0=psA[:, 2 * 512:4 * 512], in1=xt[xs[i]][:, :], op=ALU.add),
                     reads=["b2", "b3", "xt%d" % xs[i]], writes=["xt%d" % xs[i]])

        def route(i, x_):
            xr = "xt%d" % x_
            h2 = xt[x_]
            idx = idx2[i]
            gates = gates2[i]
            ssc = ss[:, 2 + i:3 + i]
            ssr2 = "ss2_%d" % i
            idxr = "idx%d" % i
            gatesr = "gates%d" % i
            P.op("scalar", lambda e: e.activation(out=junk[:, :], in_=h2[:, :], func=AF.Square, accum_out=ssc), reads=[xr], writes=[ssr2] + JW)
            rstd_from_ss(ssc, 128, ssr2, 1.0 / D)
            P.op("scalar", lambda e: e.activation(out=hbf[i][:, :], in_=h2[:, :], func=AF.Copy, scale=ssc), reads=[xr, ssr2], writes=["hbf%d" % i])
            for c in range(8):
                P.op("tensor", lambda e, c=c: e.transpose(out=psT3[:, c, :], in_=hbf[i][:, c * 128:(c + 1) * 128], identity=identb[:, :]),
                     reads=["hbf%d" % i, "identb"], writes=["bT"])
            P.op("vector", lambda e: e.tensor_copy(out=hnT[:, :, 0:128], in_=psT3[:, :, :]), reads=["bT"], writes=["hnT"])
            for hf in range(2):
                for gg in range(8):
                    g = hf * 8 + gg
                    for k in range(8):
                        P.op("tensor", lambda e, g=g, gg=gg, k=k: e.matmul(psA[:, 2048 + gg * 128:2048 + (gg + 1) * 128], lhsT=Wqb[:, k, g * 128:(g + 1) * 128],
                                                                        rhs=hnT[:, k, 0:128], start=(k == 0), stop=(k == 7)),
                             reads=["Wqb", "hnT"], writes=["b%d" % (4 + gg // 4)])
                P.op("scalar", lambda e, hf=hf: e.activation(out=qTb[:, :], in_=psA[:, 2048:3072], func=AF.Copy), reads=["b4", "b5"], writes=["qTb"])
                for gg in range(8):
                    g = hf * 8 + gg
                    P.op("tensor", lambda e, g=g, gg=gg: e.matmul(psA[:, 2048 + gg * 128:2048 + (gg + 1) * 128], lhsT=qTb[:, gg * 128:(gg + 1) * 128], rhs=keysb[:, g, :],
                                                               start=True, stop=True),
                         reads=["qTb", "keysb"], writes=["b%d" % (4 + gg // 4)])
                P.op("scalar", lambda e: e.activation(out=Ssb[:, :], in_=psA[:, 2048:3072], func=AF.Copy), reads=["b4", "b5"], writes=["Ssb"])
                for gg in range(8):
                    g = hf * 8 + gg
                    sg_ = Ssb[:, gg * 128:(gg + 1) * 128]
                    ssn = "Ssb%d" % (0 if gg < 2 else 1 if gg < 4 else 2)
                    sc_ = S2[:, (gg % 2) * 128:(gg % 2) * 128 + 128]
                    scn = "S2_%d" % (gg % 2)
                    vn, inn = "Vt%d" % g, "It%d" % g
                    v0 = Vt[:, g * 16:g * 16 + 8]
                    v1 = Vt[:, g * 16 + 8:g * 16 + 16]
                    i0 = It[:, g * 16:g * 16 + 8]
                    i1 = It[:, g * 16 + 8:g * 16 + 16]
                    P.op("vector", lambda e, sg_=sg_, v0=v0: e.max(out=v0, in_=sg_), reads=[ssn], writes=[vn])
                    P.op("vector", lambda e, sg_=sg_, v0=v0, i0=i0: e.max_index(out=i0, in_max=v0, in_values=sg_), reads=[ssn, vn], writes=[inn])
                    P.op("vector", lambda e, sg_=sg_, v0=v0, sc_=sc_: e.match_replace(out=sc_, in_to_replace=v0, in_values=sg_, imm_value=-1e30),
                         reads=[ssn, vn], writes=[scn])
                    P.op("vector", lambda e, v1=v1, sc_=sc_: e.max(out=v1, in_=sc_), reads=[scn], writes=[vn])
                    P.op("vector", lambda e, v1=v1, i1=i1, sc_=sc_: e.max_index(out=i1, in_max=v1, in_values=sc_), reads=[scn, vn], writes=[inn])
            Vt4 = Vt[:].rearrange("p (h s a) -> p h s a", h=8, s=2)
            cand4 = cand[:].rearrange("p (h a b) -> p h a b", h=8, a=16)
            P.op("vector", lambda e: e.tensor_tensor(out=cand4, in0=Vt4[:, :, 0, :].unsqueeze(3).to_broadcast([128, 8, 16, 16]),
                                                      in1=Vt4[:, :, 1, :].unsqueeze(2).to_broadcast([128, 8, 16, 16]), op=ALU.add),
                 reads=["Vt"], writes=["cand"])
            for h in range(8):
                ch = cand[:, h * 256:(h + 1) * 256]
                chn = ("ysc%d" % h) if h < 4 else ("cv%d" % (h - 4))
                sc_ = Ssb[:, (h % 2) * 256:(h % 2) * 256 + 256]
                scn = "Ssb%d" % (h % 2)
                cvn, cpn = "CV%d" % h, "CP%d" % h
                v0 = CV[:, h * 16:h * 16 + 8]
                v1 = CV[:, h * 16 + 8:h * 16 + 16]
                p0 = CP[:, h * 16:h * 16 + 8]
                p1 = CP[:, h * 16 + 8:h * 16 + 16]
                P.op("vector", lambda e, ch=ch, v0=v0: e.max(out=v0, in_=ch), reads=[chn], writes=[cvn])
                P.op("vector", lambda e, ch=ch, v0=v0, p0=p0: e.max_index(out=p0, in_max=v0, in_values=ch), reads=[chn, cvn], writes=[cpn])
                P.op("vector", lambda e, ch=ch, v0=v0, sc_=sc_: e.match_replace(out=sc_, in_to_replace=v0, in_values=ch, imm_value=-1e30),
                     reads=[chn, cvn], writes=[scn])
                P.op("vector", lambda e, v1=v1, sc_=sc_: e.max(out=v1, in_=sc_), reads=[scn], writes=[cvn])
                P.op("vector", lambda e, v1=v1, p1=p1, sc_=sc_: e.max_index(out=p1, in_max=v1, in_values=sc_), reads=[scn, cvn], writes=[cpn])
            CV3 = CV[:].rearrange("p (h k) -> p h k", h=8)
            ex3 = ex[:].rearrange("p (h k) -> p h k", h=8)
            g3 = gates[:].rearrange("p (h k) -> p h k", h=8)
            P.op("vector", lambda e: e.tensor_tensor(out=ex3, in0=CV3, in1=CV3[:, :, 0:1].to_broadcast([128, 8, 16]), op=ALU.subtract), reads=["CV"], writes=["ex"])
            P.op("scalar", lambda e: e.activation(out=ex[:, :], in_=ex[:, :], func=AF.Exp), reads=["ex"], writes=["ex"])
            P.op("vector", lambda e: e.tensor_reduce(out=Zs[:, 0:8], in_=ex3, axis=AX.X, op=ALU.add), reads=["ex"], writes=["Zs"])
            P.op("vector", lambda e: e.reciprocal(out=Zs[:, 0:8], in_=Zs[:, 0:8]), reads=["Zs"], writes=["Zs"])
            P.op("vector", lambda e: e.tensor_tensor(out=g3, in0=ex3, in1=Zs[:, 0:8].unsqueeze(2).to_broadcast([128, 8, 16]), op=ALU.mult),
                 reads=["ex", "Zs"], writes=[gatesr])
            P.op("vector", lambda e: e.tensor_single_scalar(out=au[:, :], in_=CP[:, :], scalar=4, op=ALU.logical_shift_right), reads=["CP"], writes=["au"])
            P.op("vector", lambda e: e.tensor_single_scalar(out=CP[:, :], in_=CP[:, :], scalar=15, op=ALU.bitwise_and), reads=["CP", "au"], writes=["CP"])
            P.op("vector", lambda e: e.tensor_copy(out=af[:, :], in_=au[:, :]), reads=["au"], writes=["af"])
            P.op("vector", lambda e: e.tensor_copy(out=bf[:, :], in_=CP[:, :]), reads=["CP"], writes=["bf"])
            P.op("vector", lambda e: e.tensor_copy(out=S2[:, :], in_=It[:, :]), reads=["It"], writes=["S2"])
            Itf4 = S2[:].rearrange("p (h s a) -> p h s a", h=8, s=2)
            oh4 = Ssb[:].rearrange("p (h k a) -> p h k a", h=4, k=16)
            io4 = iota16[:, :].unsqueeze(1).unsqueeze(1).to_broadcast([128, 4, 16, 16])
            for (srcf, s_, sres) in ((af, 0, "af"), (bf, 1, "bf")):
                s3 = srcf[:].rearrange("p (h k) -> p h k", h=8)
                for hh in range(2):
                    hs = slice(hh * 4, hh * 4 + 4)
                    P.op("vector", lambda e, s3=s3, hs=hs: e.tensor_tensor(out=oh4, in0=s3[:, hs, :].unsqueeze(3).to_broadcast([128, 4, 16, 16]), in1=io4, op=ALU.is_equal),
                         reads=[sres, "iota16"], writes=["Ssb"])
                    P.op("vector", lambda e, s_=s_, hs=hs: e.tensor_tensor(out=oh4, in0=oh4, in1=Itf4[:, hs, s_, :].unsqueeze(2).to_broadcast([128, 4, 16, 16]), op=ALU.mult),
                         reads=["Ssb", "S2"], writes=["Ssb"])
                    P.op("vector", lambda e, s3=s3, hs=hs: e.tensor_reduce(out=s3[:, hs, :], in_=oh4, axis=AX.X, op=ALU.add), reads=["Ssb"], writes=[sres])
            P.op("vector", lambda e: e.scalar_tensor_tensor(out=af[:, :], in0=af[:, :], scalar=128.0, in1=bf[:, :], op0=ALU.mult, op1=ALU.add),
                 reads=["af", "bf"], writes=["af"])
            P.op("vector", lambda e: e.tensor_copy(out=idx[:, :], in_=af[:, :]), reads=["af"], writes=[idxr])

        def experts(i, x_, orow, filler=None):
            xr = "xt%d" % x_
            h2 = xt[x_]
            rate = (sum(1 for e_, _ in filler if e_ != "tensor") // n_peer_j + 1) if filler else 0
            idx = idx2[i]
            gates = gates2[i]
            ssc = ss[:, 2 + i:3 + i]
            ssr2 = "ss2_%d" % i
            idxr = "idx%d" % i
            gatesr = "gates%d" % i

            def fill(n):
                while n > 0 and filler:
                    e_, th = filler.pop(0)
                    th()
                    if e_ != "tensor":
                        n -= 1
            nj = n_peer_j
            nb = nj // JB

            nj = n_peer_j
            def gather(j):
                s_ = j % NS
                P.dma("gpsimd", lambda e: e.indirect_dma_start(out=GS[s_][:, :], out_offset=None, in_=tab_b,
                                                               in_offset=bass.IndirectOffsetOnAxis(ap=idx[:, j:j + 1], axis=0)),
                      "g" + GSres[s_], reads=[idxr] + TBres, writes=[GSres[s_]])

            def dot(j):
                s_ = j % NS
                p_ = j % 2
                P.op("vector", lambda e: e.tensor_tensor(out=prod[p_][:, :], in0=GS[s_][:, 0:D], in1=hbf[i][:, :], op=ALU.mult),
                     reads=["hbf%d" % i], writes=["prod%d" % p_], weak=[GSres[s_]])
                P.op("scalar", lambda e: e.activation(out=prod[p_][:, :], in_=prod[p_][:, :], func=AF.Copy, accum_out=apre[:, j:j + 1]),
                     reads=["prod%d" % p_], writes=["prod%d" % p_, "apre%d" % j])

            def gelu(j):
                P.op("scalar", lambda e: e.activation(out=apre[:, j:j + 1], in_=apre[:, j:j + 1], func=AF.Gelu_apprx_tanh),
                     reads=["apre%d" % j], writes=["apre%d" % j])

            def acc(j):
                s_ = j % NS
                d_ = j % len(diag)
                P.op("vector", lambda e: e.scalar_tensor_tensor(out=diag[d_][:, :], in0=identb[:, :], scalar=apre[:, j:j + 1],
                                                                in1=gates[:, j:j + 1].to_broadcast([128, 128]), op0=ALU.mult, op1=ALU.mult),
                     reads=["identb", "apre%d" % j, gatesr], writes=["diag%d" % d_])
                for half in range(2):
                    P.op("tensor", lambda e, half=half: e.matmul(psA[:, half * 512:(half + 1) * 512], lhsT=diag[d_][:, :],
                                                                 rhs=GS[s_][:, D + half * 512:D + (half + 1) * 512], start=(j == 0), stop=(j == nj - 1)),
                         reads=["diag%d" % d_, GSres[s_]], writes=["b%d" % half])

            r1 = rate // 3
            r2 = (rate - r1) // 2
            r3 = rate - r1 - r2
            for t in range(nj + 3):
                if t < nj:
                    gather(t)
                fill(r3)
                if 0 <= t - 1 < nj:
                    dot(t - 1)
                fill(r2)
                if 0 <= t - 2 < nj:
                    gelu(t - 2)
                if 0 <= t - 3 < nj:
                    acc(t - 3)
                fill(r1)
            fill(100000)
            P.op("vector", lambda e: e.tensor_tensor(out=h2[:, :], in0=psA[:, 0:1024], in1=h2[:, :], op=ALU.add), reads=["b0", "b1", xr], writes=[xr])
            P.op("scalar", lambda e: e.activation(out=junk[:, :], in_=h2[:, :], func=AF.Square, accum_out=ss[:, 4 + i:5 + i]), reads=[xr], writes=["ss3_%d" % i] + JW)
            rstd_from_ss(ss[:, 4 + i:5 + i], 128, "ss3_%d" % i, 1.0 / D)
            P.op("vector", lambda e: e.scalar_tensor_tensor(out=h2[:, :], in0=h2[:, :], scalar=ss[:, 4 + i:5 + i], in1=gB[:, :], op0=ALU.mult, op1=ALU.mult),
                 reads=[xr, "ss3_%d" % i, "gB"], writes=[xr])
            P.dma("sync", lambda e: e.dma_start(out=out[orow:orow + 128, :], in_=h2[:, :]), "st%d" % x_, reads=[xr], writes=["out%d" % x_])

        mixer_pre()
        mixer_group(0)
        route(0, 0)
        for g in range(n_groups):
            x0 = 2 * (g % 2)
            P.defer = []
            route(1, x0 + 1)
            R1 = P.defer
            M, R0, ms = [], [], 0
            if g + 1 < n_groups:
                for i_ in range(2):
                    xs_ = 2 * ((g + 1) % 2) + i_
                    r0_ = HALO + (g + 1) * G + i_ * 128
                    P._dma("sync", lambda e, xs_=xs_, r0_=r0_: e.dma_start(out=xt[xs_][:, :], in_=xh[r0_:r0_ + 128, :]), "ldx%d" % xs_, writes=["xt%d" % xs_])
                P.defer = []
                P.mark = 0
                mixer_group(g + 1)
                M, ms = P.defer, P.mark
                P.defer = []
                route(0, 2 * ((g + 1) % 2))
                R0 = P.defer
            P.defer = None
            experts(0, x0, g * G, reorder(R1 + M[:ms]))
            experts(1, x0 + 1, g * G + 128, [(e_, th_) for (e_, th_, _r, _w) in M[ms:]] + reorder(R0))
        P.wait_all("sync", ["out0", "out1", "out2", "out3"])
        P.emit()
    return nc


def _prep_shared(inp):
    f = lambda a: np.ascontiguousarray(np.asarray(a, dtype=np.float32))
    col = lambda v, n: np.asarray(v, np.float32).reshape(n, 128).T
    vecs = np.zeros((128, NVEC), np.float32)
    vecs[:, GM:GM + 8] = col(inp["norm_mix_g"][0], 8)
    vecs[:, GF:GF + 8] = col(inp["norm_ffn_g"][0], 8)
    vecs[:, GO:GO + 4] = col(inp["out_norm_g_sc"][0], 4)
    vecs[:, GO + 4:GO + 8] = col(inp["out_norm_g_cf"][0], 4)
    scw = np.asarray(inp["sc_conv_w"][0], np.float32)
    cfw = np.asarray(inp["cf_conv_w"][0], np.float32)
    for k in range(4):
        vecs[:, SCW + 3 * k:SCW + 3 * k + 3] = scw[:, k * 128:(k + 1) * 128].T
        vecs[:, CFW + 31 * k:CFW + 31 * k + 31] = cfw[:, k * 128:(k + 1) * 128].T
    vecs[:, CFB:CFB + 4] = col(inp["cf_conv_b"][0], 4)
    vecs[:, LNG:LNG + 4] = col(inp["cf_ln_g"][0], 4)
    vecs[:, LNB:LNB + 4] = col(inp["cf_ln_b"][0], 4)
    gB = np.concatenate([np.broadcast_to(np.asarray(inp["norm_ffn_g"][0], np.float32)[None, :], (128, D)),
                         np.broadcast_to(np.asarray(inp["final_norm_g"], np.float32)[None, :], (128, D))], axis=1)
    sk = np.asarray(inp["peer_sub_keys"][0], np.float32)
    keysT = np.ascontiguousarray(sk.reshape(16, 128, 128).transpose(2, 0, 1).reshape(128, 2048))
    return {
        "w_in": f(inp["w_in"][0]), "w_out": f(inp["w_out"][0]), "w_q": f(inp["peer_w_q"][0]),
        "keysT": keysT, "uv_tab": np.ascontiguousarray(np.concatenate([f(inp["peer_u"][0]), f(inp["peer_v"][0])], axis=1)),
        "vecs_h": vecs, "gB_h": np.ascontiguousarray(gB),
    }


def _core_x(inp, core, n_groups=16):
    x = np.asarray(inp["x"], np.float32)
    meta = np.asarray(inp["meta_tokens"], np.float32)
    b, half = core // 2, core % 2
    ntok = n_groups * G
    if half == 0:
        halo = np.concatenate([np.zeros((HALO - meta.shape[0], D), np.float32), meta], axis=0)
        body = x[b, 0:ntok]
    else:
        halo = x[b, 4096 - HALO:4096]
        body = x[b, 4096:4096 + ntok]
    return np.ascontiguousarray(np.concatenate([halo, body], axis=0))


def kernel(**inputs):
    shared = _prep_shared(inputs)
    nc = build(16)
    in_maps = []
    for c in range(N_CORES):
        m = dict(shared)
        m["xh"] = _core_x(inputs, c)
        in_maps.append(m)
    res = run_bass_kernel_spmd(nc, in_maps, core_ids=list(range(N_CORES)))
    outp = np.empty((4, 8192, D), np.float32)
    for c in range(N_CORES):
        b, half = c // 2, c % 2
        outp[b, half * 4096:(half + 1) * 4096] = res.results[c]["out"]
    return outp
```

```python
import numpy as np
from contextlib import ExitStack
import concourse.bass as bass
import concourse.mybir as mybir
from concourse.bass_utils import run_bass_kernel_spmd

F32 = mybir.dt.float32
BF16 = mybir.dt.bfloat16
U32 = mybir.dt.uint32
ALU = mybir.AluOpType
AF = mybir.ActivationFunctionType
AX = mybir.AxisListType

D = 1024
G = 256
HALO = 32
N_CORES = 8
EPS = 1e-6
NS = 10
JB = 4
GM, GF, GO, SCW, CFW, CFB, LNG, LNB, NVEC = 0, 8, 16, 24, 36, 160, 164, 168, 172


class Prog:
    ENGS = ("sync", "scalar", "vector", "gpsimd", "tensor")

    def __init__(self, nc, stack):
        self.nc = nc
        self.stack = stack
        self.q = {e: [] for e in self.ENGS}
        self.cnt = {e: 0 for e in self.ENGS}
        self.sem = {e: stack.enter_context(nc.semaphore("c_" + e)) for e in self.ENGS}
        self.seen = {e: {} for e in self.ENGS}
        self.last_w = {}
        self.readers = {}
        self.dsem = {}
        self.dcnt = {}
        self.alias = {}
        self.defer = None

    def _exp(self, names):
        out = []
        for n in names:
            out.extend(self.alias.get(n, (n,)))
        return out

    def _deps(self, e, reads, writes):
        deps = {}

        def add(ev):
            if ev is None:
                return
            k, s, v = ev
            if k not in deps or deps[k][1] < v:
                deps[k] = (s, v)

        for r in reads:
            add(self.last_w.get(r))
        for w in writes:
            rd = self.readers.get(w, ())
            if not rd:
                add(self.last_w.get(w))
            for ev in rd:
                add(ev)
        for k, (s, v) in deps.items():
            if e == "tensor" and k == "e_tensor":
                continue
            if self.seen[e].get(k, 0) < v:
                self.seen[e][k] = v
                self.q[e].append(lambda eng, s=s, v=v: eng.wait_ge(s, v))

    def _commit(self, ev, reads, writes):
        for w in writes:
            self.last_w[w] = ev
            self.readers[w] = []
        for r in reads:
            if r not in writes:
                self.readers.setdefault(r, []).append(ev)

    def op(self, e, fn, reads=(), writes=(), weak=()):
        if self.defer is not None:
            self.defer.append((e, lambda: self._op(e, fn, reads, writes, weak),
                               set(self._exp(reads)) | set(self._exp(weak)), set(self._exp(writes))))
            return
        self._op(e, fn, reads, writes, weak)

    def _op(self, e, fn, reads=(), writes=(), weak=()):
        reads, writes, weak = self._exp(reads), self._exp(writes), self._exp(weak)
        self._deps(e, list(reads) + list(weak), writes)
        self.cnt[e] += 1
        n = self.cnt[e]
        s = self.sem[e]
        self.q[e].append(lambda eng, fn=fn, s=s: fn(eng).then_inc(s, 1))
        self._commit(("e_" + e, s, n), reads, writes)

    def dma(self, e, fn, key, reads=(), writes=()):
        if self.defer is not None:
            self.defer.append((e, lambda: self._dma(e, fn, key, reads, writes), set(self._exp(reads)), set(self._exp(writes))))
            return
        self._dma(e, fn, key, reads, writes)

    def _dma(self, e, fn, key, reads=(), writes=()):
        reads, writes = self._exp(reads), self._exp(writes)
        self._deps(e, reads, writes)
        if key not in self.dsem:
            self.dsem[key] = self.stack.enter_context(self.nc.semaphore("d_" + key))
            self.dcnt[key] = 0
        s = self.dsem[key]
        self.dcnt[key] += 16
        v = self.dcnt[key]
        self.q[e].append(lambda eng, fn=fn, s=s: fn(eng).then_inc(s, 16))
        self._commit(("d_" + key, s, v), reads, writes)

    def wait_all(self, e, resources):
        self._deps(e, self._exp(resources), ())

    def emit(self):
        with self.nc.Block() as block:
            @block.sync
            def _(eng):
                for f in self.q["sync"]:
                    f(eng)

            @block.scalar
            def _(eng):
                for f in self.q["scalar"]:
                    f(eng)

            @block.vector
            def _(eng):
                for f in self.q["vector"]:
                    f(eng)

            @block.gpsimd
            def _(eng):
                for f in self.q["gpsimd"]:
                    f(eng)

            @block.tensor
            def _(eng):
                for f in self.q["tensor"]:
                    f(eng)


PE_BANK = {"p0": "B2", "p1": "B2", "p2": "B3", "p3": "B3", "b0": "B0", "b1": "B1", "b4": "B4", "b5": "B5", "bT": "BT"}


def reorder(entries, dmin=2):
    units = []
    for (e, th, rd, wr) in entries:
        rd, wr = set(rd), set(wr)
        if e == "tensor":
            wr |= {"pe_" + PE_BANK[w] for w in wr if w in PE_BANK}
            if units and units[-1]["pe"]:
                u = units[-1]
                u["th"].append((e, th)); u["rd"] |= rd; u["wr"] |= wr
                continue
        units.append({"pe": e == "tensor", "th": [(e, th)], "rd": rd, "wr": wr})
    n = len(units)
    preds = [set() for _ in range(n)]
    succs = [set() for _ in range(n)]
    last_w, readers = {}, {}
    for i, u in enumerate(units):
        for r in u["rd"]:
            if r in last_w:
                preds[i].add(last_w[r])
        for w in u["wr"]:
            if w in last_w:
                preds[i].add(last_w[w])
            preds[i].update(readers.get(w, ()))
        for w in u["wr"]:
            last_w[w] = i
            readers[w] = []
        for r in u["rd"]:
            if r not in u["wr"]:
                readers.setdefault(r, []).append(i)
        preds[i].discard(i)
        for p in preds[i]:
            succs[p].add(i)
    cp = [1] * n
    for i in range(n - 1, -1, -1):
        for q in succs[i]:
            cp[i] = max(cp[i], 1 + cp[q])
    npred = [len(p) for p in preds]
    ready = [i for i in range(n) if npred[i] == 0]
    pos = {}
    out = []
    while ready:
        step = len(pos)

        def key(i):
            last = max((pos[p] for p in preds[i]), default=-10 ** 6)
            far = (step - last) >= dmin
            return (0 if far else 1, -cp[i] if far else last, i)

        b = min(ready, key=key)
        ready.remove(b)
        pos[b] = step
        out.extend(units[b]["th"])
        for q in succs[b]:
            npred[q] -= 1
            if npred[q] == 0:
                ready.append(q)
    assert len(pos) == n
    return out


def build(n_groups=16, n_peer_j=128):
    nc = bass.Bass("TRN2", target_bir_lowering=False)
    ntok = HALO + n_groups * G
    dr = lambda name, shape, kind="ExternalInput", dt=F32: nc.dram_tensor(name, shape, dt, kind=kind).ap()
    xh = dr("xh", [ntok, D])
    w_in = dr("w_in", [D, 2560])
    w_out = dr("w_out", [D, D])
    w_q = dr("w_q", [D, 2048])
    keysT = dr("keysT", [128, 2048])
    uv_tab = dr("uv_tab", [16384, 2 * D])
    tab_b = dr("tab_b", [16384, 2 * D], kind="Internal", dt=BF16)
    vecs_d = dr("vecs_h", [128, NVEC])
    gB_d = dr("gB_h", [128, 2048])
    out = dr("out", [n_groups * G, D], kind="ExternalOutput")

    with ExitStack() as st:
        P = Prog(nc, st)
        sb = lambda name, shape, dt=F32: st.enter_context(nc.sbuf_tensor(name, shape, dt))
        Wib = sb("Wib", [128, 8, 2560], BF16)
        Wob = sb("Wob", [128, 8, 1024], BF16)
        Wqb = sb("Wqb", [128, 8, 2048], BF16)
        keysb = sb("keysb", [128, 16, 128], BF16)
        vecs = sb("vecs", [128, NVEC])
        gB = sb("gB", [128, D])
        identb = sb("identb", [128, 128], BF16)
        onesf = sb("onesf", [128, 128])
        iota16 = sb("iota16", [128, 16])
        xt = [sb("xt%d" % i, [128, D]) for i in range(4)]
        hnb = sb("hnb", [128, D], BF16)
        ss = sb("ss", [128, 8])
        hnT = sb("hnT", [128, 8, G], BF16)
        cgs = sb("cgs", [128, G])
        zb = [sb("z%d" % k, [128, HALO + G]) for k in range(4)]
        mixA = sb("mixA", [128, 2048])
        ysc = [mixA[:, k * G:(k + 1) * G] for k in range(4)]
        sq = [sb("sq%d" % k, [128, G]) for k in range(2)]
        ub = [sb("u%d" % k, [128, HALO + G], BF16) for k in range(4)]
        dgs = [sb("dg%d" % k, [128, 128], BF16) for k in range(4)]
        cv = [mixA[:, 1024 + k * G:1024 + (k + 1) * G] for k in range(4)]
        mt = sb("mt", [128, G])
        rt = sb("rt", [128, G])
        qTb = sb("qTb", [128, 1024], BF16)
        Ssb = sb("Ssb", [128, 1024])
        S2 = sb("S2", [128, 256])
        Vt = sb("Vt", [128, 256])
        It = sb("It", [128, 256], U32)
        cand = mixA
        CV = sb("CV", [128, 128])
        CP = sb("CP", [128, 128], U32)
        au = sb("au", [128, 128], U32)
        af = sb("af", [128, 128])
        bf = sb("bf", [128, 128])
        idx2 = [sb("idx%d" % i, [128, 128], U32) for i in range(2)]
        ex = sb("ex", [128, 128])
        Zs = sb("Zs", [128, 8])
        gates2 = [sb("gates%d" % i, [128, 128]) for i in range(2)]
        apre = sb("apre", [128, 128])
        diag = [sb("diag%d" % i, [128, 128], BF16) for i in range(4)]
        GS = [sb("GS%d" % i, [128, 2 * D], BF16) for i in range(NS)]
        yn = [sb("yn%d" % k, [128, G], BF16) for k in range(8)]
        psA = st.enter_context(nc.psum_tensor("psA", [128, 6 * 512], F32))
        psT = st.enter_context(nc.psum_tensor("psT", [128, 1024], BF16))
        pslot = lambda s_, n: psA[:, 1024 + s_ * 256:1024 + s_ * 256 + n]
        bank = lambda i, n=512: psA[:, i * 512:i * 512 + n]
        psT3 = psT[:].rearrange("p (c t) -> p c t", c=8)
        prod = [sb("prod%d" % i, [128, D], BF16) for i in range(2)]
        hbf = [sb("hbf%d" % i, [128, D], BF16) for i in range(2)]
        junk = prod[1][:, :]
        JW = ["prod1"]
        JR = []

        vcol = lambda c: vecs[:, c:c + 1]
        epsc = ss[:, 7:8]
        P.op("vector", lambda e: e.memset(epsc, EPS), writes=["epsc"])
        P.alias["cand"] = ["ysc%d" % k for k in range(4)] + ["cv%d" % k for k in range(4)]
        P.alias["Vt"] = ["Vt%d" % k for k in range(16)]
        P.alias["It"] = ["It%d" % k for k in range(16)]
        P.alias["CV"] = ["CV%d" % k for k in range(8)]
        P.alias["CP"] = ["CP%d" % k for k in range(8)]
        P.alias["S2"] = ["S2_0", "S2_1"]
        P.alias["Ssb"] = ["Ssb0", "Ssb1", "Ssb2"]
        P.alias["mixlo"] = ["ysc%d" % k for k in range(4)]
        P.alias["mixhi"] = ["cv%d" % k for k in range(4)]
        P.alias["b2"] = ["p0", "p1"]
        P.alias["b3"] = ["p2", "p3"]

        P.dma("sync", lambda e: e.dma_start(out=vecs[:], in_=vecs_d), "ldv", writes=["vecs"])
        P.dma("sync", lambda e: e.dma_start(out=gB[:], in_=gB_d[:, D:2 * D]), "ldg", writes=["gB"])
        P.dma("sync", lambda e: e.dma_start(out=xt[0][:, :], in_=gB_d[:, 0:D]), "ldx0", writes=["xt0"])
        P.op("gpsimd", lambda e: e.memset(apre[:], 1.0), writes=["apre_setup"])
        P.op("gpsimd", lambda e: e.affine_select(out=apre[:], in_=apre[:], pattern=[[-1, 128]], compare_op=ALU.is_equal,
                                                  fill=0.0, base=0, channel_multiplier=1), reads=["apre_setup"], writes=["apre_setup"])
        P.op("vector", lambda e: e.tensor_copy(out=identb[:], in_=apre[:]), reads=["apre_setup"], writes=["identb"])
        P.op("gpsimd", lambda e: e.memset(onesf[:], 1.0), writes=["onesf"])
        P.op("gpsimd", lambda e: e.iota(iota16[:], pattern=[[1, 16]], base=0, channel_multiplier=0,
                                        allow_small_or_imprecise_dtypes=True), writes=["iota16"])
        stage = [(mixA[:, 0:1024], "mixlo"), (mixA[:, 1024:2048], "mixhi"), (xt[1][:, :], "xt1"), (xt[2][:, :], "xt2")]
        si = 0
        for (Wd, Wb, ncol, gcol, nm) in ((w_in, Wib, 2560, GM, "Wib"), (w_out, Wob, 1024, GO, "Wob"), (w_q, Wqb, 2048, GF, "Wqb")):
            for c in range(8):
                for c0 in range(0, ncol, 1024):
                    w = min(1024, ncol - c0)
                    sbuf_, rn = stage[si % 4]
                    si += 1
                    P.dma("sync" if si % 2 else "scalar",
                          lambda e, sbuf_=sbuf_, Wd=Wd, c=c, c0=c0, w=w: e.dma_start(out=sbuf_[:, 0:w], in_=Wd[c * 128:(c + 1) * 128, c0:c0 + w]),
                          "ld" + rn, writes=[rn])
                    if si % 2:
                        P.op("vector", lambda e, sbuf_=sbuf_, Wb=Wb, c=c, c0=c0, w=w, gcol=gcol: e.tensor_scalar(
                            out=Wb[:, c, c0:c0 + w], in0=sbuf_[:, 0:w], scalar1=vcol(gcol + c), scalar2=None, op0=ALU.mult),
                            reads=[rn, "vecs"], writes=[nm])
                    else:
                        P.op("scalar", lambda e, sbuf_=sbuf_, Wb=Wb, c=c, c0=c0, w=w, gcol=gcol: e.activation(
                            out=Wb[:, c, c0:c0 + w], in_=sbuf_[:, 0:w], func=AF.Copy, scale=vcol(gcol + c)),
                            reads=[rn, "vecs"], writes=[nm])
        for c0 in range(0, 2048, 1024):
            sbuf_, rn = stage[si % 4]
            si += 1
            P.dma("sync", lambda e, sbuf_=sbuf_, c0=c0: e.dma_start(out=sbuf_[:, :], in_=keysT[:, c0:c0 + 1024]), "ld" + rn, writes=[rn])
            P.op("vector", lambda e, sbuf_=sbuf_, c0=c0: e.tensor_copy(out=keysb[:].rearrange("p g n -> p (g n)")[:, c0:c0 + 1024], in_=sbuf_[:, :]),
                 reads=[rn], writes=["keysb"])
        GSres = ["GS%d" % i for i in range(NS)]
        TBres = ["tab_b%d" % i for i in range(NS)]
        ustage = [(mixA[:, 0:1024], "mixlo"), (xt[1][:, :], "xt1"), (xt[3][:, :], "xt3")]
        vstage = [(mixA[:, 1024:2048], "mixhi"), (xt[2][:, :], "xt2")]
        for it in range(128):
            su_, ru = ustage[it % 3]
            sv_, rv = vstage[it % 2]
            gs_ = it % NS
            P.dma("sync", lambda e, su_=su_, it=it: e.dma_start(out=su_[:, :], in_=uv_tab[it * 128:(it + 1) * 128, 0:D]), "ldu" + ru, writes=[ru])
            P.dma("sync", lambda e, sv_=sv_, it=it: e.dma_start(out=sv_[:, :], in_=uv_tab[it * 128:(it + 1) * 128, D:2 * D]), "ldv" + rv, writes=[rv])
            P.op("vector", lambda e, su_=su_, gs_=gs_: e.tensor_tensor(out=GS[gs_][:, 0:D], in0=su_[:, :], in1=xt[0][:, :], op=ALU.mult),
                 reads=[ru, "xt0"], writes=[GSres[gs_] + "u"])
            P.op("scalar", lambda e, sv_=sv_, gs_=gs_: e.activation(out=GS[gs_][:, D:2 * D], in_=sv_[:, :], func=AF.Copy), reads=[rv], writes=[GSres[gs_] + "v"])
            P.dma("scalar", lambda e, gs_=gs_, it=it: e.dma_start(out=tab_b[it * 128:(it + 1) * 128, :], in_=GS[gs_][:, :]),
                  "st" + GSres[gs_], reads=[GSres[gs_] + "u", GSres[gs_] + "v", GSres[gs_]], writes=[TBres[gs_]])

        def rstd_from_ss(ss_ap, n, res, scale):
            P.op("scalar", lambda e: e.activation(out=ss_ap, in_=ss_ap, func=AF.Sqrt, scale=scale, bias=epsc[0:n, 0:1]), reads=[res, "epsc"], writes=[res])
            P.op("vector", lambda e: e.reciprocal(out=ss_ap, in_=ss_ap), reads=[res], writes=[res])

        def bcast_rstd(src_bank, src_res, dst, dst_res, N, scale):
            P.op("scalar", lambda e: e.activation(out=dst[:, 0:N], in_=src_bank[:, 0:N], func=AF.Sqrt, scale=scale, bias=epsc[:, 0:1]),
                 reads=[src_res, "epsc"], writes=[dst_res])
            P.op("vector", lambda e: e.reciprocal(out=dst[:, 0:N], in_=dst[:, 0:N]), reads=[dst_res], writes=[dst_res])

        def front(tiles, N, xs, preloaded=False):
            off = 0
            for i, (r0, n) in enumerate(tiles):
                xr = "xt%d" % xs[i]
                xi = xt[xs[i]]
                if not preloaded:
                    P.dma("sync", lambda e, xi=xi, i=i, r0=r0, n=n: e.dma_start(out=xi[0:n, :], in_=xh[r0:r0 + n, :]), "ldx%d" % xs[i], writes=[xr])
                ssr = "ss%d" % i
                P.op("scalar", lambda e, xi=xi, i=i, n=n: e.activation(out=junk[0:n, :], in_=xi[0:n, :], func=AF.Square, accum_out=ss[0:n, i:i + 1]),
                     reads=[xr], writes=[ssr] + JW)
                rstd_from_ss(ss[0:n, i:i + 1], n, ssr, 1.0 / D)
                P.op("scalar", lambda e, xi=xi, i=i, n=n: e.activation(out=hnb[0:n, :], in_=xi[0:n, :], func=AF.Copy, scale=ss[0:n, i:i + 1]),
                     reads=[xr, ssr], writes=["hnb"])
                for c in range(8):
                    P.op("tensor", lambda e, c=c, n=n: e.transpose(out=psT3[:, c, 0:n], in_=hnb[0:n, c * 128:(c + 1) * 128], identity=identb[0:n, 0:n]),
                         reads=["hnb", "identb"], writes=["bT"])
                P.op("vector", lambda e, n=n, off=off: e.tensor_copy(out=hnT[:, :, off:off + n], in_=psT3[:, :, 0:n]), reads=["bT"], writes=["hnT"])
                off += n

        def proj(col, b, N):
            for k in range(8):
                P.op("tensor", lambda e, k=k, col=col, b=b, N=N: e.matmul(pslot(b, N), lhsT=Wib[:, k, col * 128:(col + 1) * 128], rhs=hnT[:, k, 0:N],
                                                                            start=(k == 0), stop=(k == 7)),
                     reads=["Wib", "hnT"], writes=["p%d" % b])

        def mixer_pre():
            N = HALO
            front([(0, HALO)], N, [0])
            for k in range(4):
                proj(k, 0, N)
                proj(8 + k, 1, N)
                P.op("scalar", lambda e: e.activation(out=cgs[:, 0:N], in_=pslot(1, N), func=AF.Copy), reads=["p1"], writes=["cgs"])
                P.op("vector", lambda e, k=k: e.tensor_tensor(out=zb[k][:, 0:N], in0=pslot(0, N), in1=cgs[:, 0:N], op=ALU.mult),
                     reads=["p0", "cgs"], writes=["z%d" % k])
                proj(12 + k, 0, N)
                proj(16 + k, 1, N)
                P.op("scalar", lambda e: e.activation(out=cgs[:, 0:N], in_=pslot(1, N), func=AF.Sigmoid), reads=["p1"], writes=["cgs"])
                P.op("vector", lambda e, k=k: e.tensor_tensor(out=ub[k][:, 0:N], in0=pslot(0, N), in1=cgs[:, 0:N], op=ALU.mult),
                     reads=["p0", "cgs"], writes=["u%d" % k])

        def mixer_group(g):
            N = G
            base = HALO + g * G
            tiles = [(base, 128), (base + 128, 128)]
            xs = [2 * (g % 2), 2 * (g % 2) + 1]
            front(tiles, N, xs, preloaded=(g > 0))
            for k in range(4):
                proj(k, 0, N)
                proj(8 + k, 1, N)
                proj(4 + k, 2, N)
                zr = "z%d" % k
                P.op("scalar", lambda e: e.activation(out=cgs[:, 0:N], in_=pslot(1, N), func=AF.Copy), reads=["p1"], writes=["cgs"])
                if g > 0:
                    P.op("vector", lambda e, k=k: e.tensor_copy(out=zb[k][:, 0:HALO], in_=zb[k][:, G:G + HALO]), reads=[zr], writes=[zr])
                P.op("vector", lambda e, k=k: e.tensor_tensor(out=zb[k][:, HALO:HALO + N], in0=pslot(0, N), in1=cgs[:, 0:N], op=ALU.mult),
                     reads=["p0", "cgs"], writes=[zr])
                P.op("vector", lambda e, k=k: e.tensor_scalar(out=rt[:, 0:N], in0=zb[k][:, HALO - 2:HALO - 2 + N], scalar1=vcol(SCW + 3 * k), scalar2=None, op0=ALU.mult),
                     reads=[zr, "vecs"], writes=["rt"])
                for j in (1, 2):
                    P.op("vector", lambda e, k=k, j=j: e.scalar_tensor_tensor(out=rt[:, 0:N], in0=zb[k][:, HALO - 2 + j:HALO - 2 + j + N], scalar=vcol(SCW + 3 * k + j),
                                                                              in1=rt[:, 0:N], op0=ALU.mult, op1=ALU.add),
                         reads=[zr, "vecs", "rt"], writes=["rt"])
                yr = "ysc%d" % k
                P.op("vector", lambda e, k=k: e.tensor_tensor(out=ysc[k][:, 0:N], in0=pslot(2, N), in1=rt[:, 0:N], op=ALU.mult),
                     reads=["p2", "rt"], writes=[yr])
                sr = "sq%d" % (k % 2)
                P.op("scalar", lambda e, k=k: e.activation(out=sq[k % 2][:, 0:N], in_=ysc[k][:, 0:N], func=AF.Square), reads=[yr], writes=[sr])
                P.op("tensor", lambda e, k=k: e.matmul(bank(4, N), lhsT=onesf[:], rhs=sq[k % 2][:, 0:N], start=(k == 0), stop=(k == 3)),
                     reads=[sr, "onesf"], writes=["b4"])
            bcast_rstd(bank(4), "b4", mt, "mt", N, 1.0 / 512)
            for k in range(4):
                P.op("vector", lambda e, k=k: e.tensor_tensor(out=yn[k][:, 0:N], in0=ysc[k][:, 0:N], in1=mt[:, 0:N], op=ALU.mult),
                     reads=["ysc%d" % k, "mt"], writes=["yn%d" % k])
            if P.defer is not None:
                P.mark = len(P.defer)
            for k in range(4):
                proj(12 + k, 0, N)
                proj(16 + k, 1, N)
                ur = "u%d" % k
                cr = "cv%d" % k
                P.op("scalar", lambda e: e.activation(out=cgs[:, 0:N], in_=pslot(1, N), func=AF.Sigmoid), reads=["p1"], writes=["cgs"])
                if g > 0:
                    P.op("vector", lambda e, k=k: e.tensor_copy(out=ub[k][:, 0:HALO], in_=ub[k][:, G:G + HALO]), reads=[ur], writes=[ur])
                P.op("vector", lambda e, k=k: e.tensor_tensor(out=ub[k][:, HALO:HALO + N], in0=pslot(0, N), in1=cgs[:, 0:N], op=ALU.mult),
                     reads=["p0", "cgs"], writes=[ur])
                for j in range(31):
                    dn = (k * 31 + j) % len(dgs)
                    dr_ = "dg%d" % dn
                    if j % 2:
                        P.op("vector", lambda e, k=k, j=j, dn=dn: e.tensor_tensor(out=dgs[dn][:, :], in0=identb[:, :],
                                                                                 in1=vcol(CFW + 31 * k + j).to_broadcast([128, 128]), op=ALU.mult),
                             reads=["identb", "vecs"], writes=[dr_])
                    else:
                        P.op("scalar", lambda e, k=k, j=j, dn=dn: e.activation(out=dgs[dn][:, :], in_=identb[:, :], func=AF.Copy, scale=vcol(CFW + 31 * k + j)),
                             reads=["identb", "vecs"], writes=[dr_])
                    P.op("tensor", lambda e, k=k, j=j, dn=dn: e.matmul(pslot(3, N), lhsT=dgs[dn][:, :], rhs=ub[k][:, 2 + j:2 + j + N], start=(j == 0), stop=(j == 30)),
                         reads=[dr_, ur], writes=["p3"])
                P.op("vector", lambda e, k=k: e.tensor_scalar(out=cv[k][:, 0:N], in0=pslot(3, N), scalar1=vcol(CFB + k), scalar2=None, op0=ALU.add),
                     reads=["p3", "vecs"], writes=[cr])
                sr = "sq%d" % (k % 2)
                P.op("scalar", lambda e, k=k: e.activation(out=sq[k % 2][:, 0:N], in_=cv[k][:, 0:N], func=AF.Square), reads=[cr], writes=[sr])
                P.op("tensor", lambda e, k=k: e.matmul(bank(4, N), lhsT=onesf[:], rhs=cv[k][:, 0:N], start=(k == 0), stop=(k == 3)),
                     reads=[cr, "onesf"], writes=["b4"])
                P.op("tensor", lambda e, k=k: e.matmul(bank(5, N), lhsT=onesf[:], rhs=sq[k % 2][:, 0:N], start=(k == 0), stop=(k == 3)),
                     reads=[sr, "onesf"], writes=["b5"])
            P.op("vector", lambda e: e.tensor_scalar(out=mt[:, 0:N], in0=bank(4, N), scalar1=1.0 / 512, scalar2=None, op0=ALU.mult), reads=["b4"], writes=["mt"])
            P.op("vector", lambda e: e.tensor_tensor(out=sq[0][:, 0:N], in0=mt[:, 0:N], in1=mt[:, 0:N], op=ALU.mult), reads=["mt"], writes=["sq0"])
            P.op("vector", lambda e: e.scalar_tensor_tensor(out=rt[:, 0:N], in0=bank(5, N), scalar=1.0 / 512, in1=sq[0][:, 0:N], op0=ALU.mult, op1=ALU.subtract),
                 reads=["b5", "sq0"], writes=["rt"])
            P.op("vector", lambda e: e.tensor_scalar(out=rt[:, 0:N], in0=rt[:, 0:N], scalar1=EPS, scalar2=None, op0=ALU.add), reads=["rt"], writes=["rt"])
            P.op("scalar", lambda e: e.activation(out=rt[:, 0:N], in_=rt[:, 0:N], func=AF.Sqrt), reads=["rt"], writes=["rt"])
            P.op("vector", lambda e: e.reciprocal(out=rt[:, 0:N], in_=rt[:, 0:N]), reads=["rt"], writes=["rt"])
            for k in range(4):
                cr = "cv%d" % k
                P.op("vector", lambda e, k=k: e.tensor_tensor(out=cv[k][:, 0:N], in0=cv[k][:, 0:N], in1=mt[:, 0:N], op=ALU.subtract), reads=[cr, "mt"], writes=[cr])
                P.op("vector", lambda e, k=k: e.tensor_tensor(out=cv[k][:, 0:N], in0=cv[k][:, 0:N], in1=rt[:, 0:N], op=ALU.mult), reads=[cr, "rt"], writes=[cr])
                P.op("scalar", lambda e, k=k: e.activation(out=cv[k][:, 0:N], in_=cv[k][:, 0:N], func=AF.Silu, scale=vcol(LNG + k), bias=vcol(LNB + k)),
                     reads=[cr, "vecs"], writes=[cr])
                sr = "sq%d" % (k % 2)
                P.op("scalar", lambda e, k=k: e.activation(out=sq[k % 2][:, 0:N], in_=cv[k][:, 0:N], func=AF.Square), reads=[cr], writes=[sr])
                P.op("tensor", lambda e, k=k: e.matmul(bank(4, N), lhsT=onesf[:], rhs=sq[k % 2][:, 0:N], start=(k == 0), stop=(k == 3)),
                     reads=[sr, "onesf"], writes=["b4"])
            bcast_rstd(bank(4), "b4", mt, "mt", N, 1.0 / 512)
            for k in range(4):
                P.op("vector", lambda e, k=k: e.tensor_tensor(out=yn[4 + k][:, 0:N], in0=cv[k][:, 0:N], in1=mt[:, 0:N], op=ALU.mult),
                     reads=["cv%d" % k, "mt"], writes=["yn%d" % (4 + k)])
            for i in range(2):
                for half in range(2):
                    for kk in range(8):
                        P.op("tensor", lambda e, i=i, half=half, kk=kk: e.matmul(psA[:, (2 + half) * 512:(3 + half) * 512], lhsT=yn[kk][:, i * 128:(i + 1) * 128],
                                                                                  rhs=Wob[:, kk, half * 512:(half + 1) * 512], start=(kk == 0), stop=(kk == 7)),
                             reads=["yn%d" % kk, "Wob"], writes=["b%d" % (2 + half)])
                P.op("vector", lambda e, i=i: e.tensor_tensor(out=xt[xs[i]][:, :], in0=psA[:, 2 * 512:4 * 512], in1=xt[xs[i]][:, :], op=ALU.add),
                     reads=["b2", "b3", "xt%d" % xs[i]], writes=["xt%d" % xs[i]])

        def route(i, x_):
            xr = "xt%d" % x_
            h2 = xt[x_]
            idx = idx2[i]
            gates = gates2[i]
            ssc = ss[:, 2 + i:3 + i]
            ssr2 = "ss2_%d" % i
            idxr = "idx%d" % i
            gatesr = "gates%d" % i
            P.op("scalar", lambda e: e.activation(out=junk[:, :], in_=h2[:, :], func=AF.Square, accum_out=ssc), reads=[xr], writes=[ssr2] + JW)
            rstd_from_ss(ssc, 128, ssr2, 1.0 / D)
            P.op("scalar", lambda e: e.activation(out=hbf[i][:, :], in_=h2[:, :], func=AF.Copy, scale=ssc), reads=[xr, ssr2], writes=["hbf%d" % i])
            for c in range(8):
                P.op("tensor", lambda e, c=c: e.transpose(out=psT3[:, c, :], in_=hbf[i][:, c * 128:(c + 1) * 128], identity=identb[:, :]),
                     reads=["hbf%d" % i, "identb"], writes=["bT"])
            P.op("vector", lambda e: e.tensor_copy(out=hnT[:, :, 0:128], in_=psT3[:, :, :]), reads=["bT"], writes=["hnT"])
            for hf in range(2):
                for gg in range(8):
                    g = hf * 8 + gg
                    for k in range(8):
                        P.op("tensor", lambda e, g=g, gg=gg, k=k: e.matmul(psA[:, 2048 + gg * 128:2048 + (gg + 1) * 128], lhsT=Wqb[:, k, g * 128:(g + 1) * 128],
                                                                        rhs=hnT[:, k, 0:128], start=(k == 0), stop=(k == 7)),
                             reads=["Wqb", "hnT"], writes=["b%d" % (4 + gg // 4)])
                P.op("scalar", lambda e, hf=hf: e.activation(out=qTb[:, :], in_=psA[:, 2048:3072], func=AF.Copy), reads=["b4", "b5"], writes=["qTb"])
                for gg in range(8):
                    g = hf * 8 + gg
                    P.op("tensor", lambda e, g=g, gg=gg: e.matmul(psA[:, 2048 + gg * 128:2048 + (gg + 1) * 128], lhsT=qTb[:, gg * 128:(gg + 1) * 128], rhs=keysb[:, g, :],
                                                               start=True, stop=True),
                         reads=["qTb", "keysb"], writes=["b%d" % (4 + gg // 4)])
                P.op("scalar", lambda e: e.activation(out=Ssb[:, :], in_=psA[:, 2048:3072], func=AF.Copy), reads=["b4", "b5"], writes=["Ssb"])
                for gg in range(8):
                    g = hf * 8 + gg
                    sg_ = Ssb[:, gg * 128:(gg + 1) * 128]
                    ssn = "Ssb%d" % (0 if gg < 2 else 1 if gg < 4 else 2)
                    sc_ = S2[:, (gg % 2) * 128:(gg % 2) * 128 + 128]
                    scn = "S2_%d" % (gg % 2)
                    vn, inn = "Vt%d" % g, "It%d" % g
                    v0 = Vt[:, g * 16:g * 16 + 8]
                    v1 = Vt[:, g * 16 + 8:g * 16 + 16]
                    i0 = It[:, g * 16:g * 16 + 8]
                    i1 = It[:, g * 16 + 8:g * 16 + 16]
                    P.op("vector", lambda e, sg_=sg_, v0=v0: e.max(out=v0, in_=sg_), reads=[ssn], writes=[vn])
                    P.op("vector", lambda e, sg_=sg_, v0=v0, i0=i0: e.max_index(out=i0, in_max=v0, in_values=sg_), reads=[ssn, vn], writes=[inn])
                    P.op("vector", lambda e, sg_=sg_, v0=v0, sc_=sc_: e.match_replace(out=sc_, in_to_replace=v0, in_values=sg_, imm_value=-1e30),
                         reads=[ssn, vn], writes=[scn])
                    P.op("vector", lambda e, v1=v1, sc_=sc_: e.max(out=v1, in_=sc_), reads=[scn], writes=[vn])
                    P.op("vector", lambda e, v1=v1, i1=i1, sc_=sc_: e.max_index(out=i1, in_max=v1, in_values=sc_), reads=[scn, vn], writes=[inn])
            Vt4 = Vt[:].rearrange("p (h s a) -> p h s a", h=8, s=2)
            cand4 = cand[:].rearrange("p (h a b) -> p h a b", h=8, a=16)
            P.op("vector", lambda e: e.tensor_tensor(out=cand4, in0=Vt4[:, :, 0, :].unsqueeze(3).to_broadcast([128, 8, 16, 16]),
                                                      in1=Vt4[:, :, 1, :].unsqueeze(2).to_broadcast([128, 8, 16, 16]), op=ALU.add),
                 reads=["Vt"], writes=["cand"])
            for h in range(8):
                ch = cand[:, h * 256:(h + 1) * 256]
                chn = ("ysc%d" % h) if h < 4 else ("cv%d" % (h - 4))
                sc_ = Ssb[:, (h % 2) * 256:(h % 2) * 256 + 256]
                scn = "Ssb%d" % (h % 2)
                cvn, cpn = "CV%d" % h, "CP%d" % h
                v0 = CV[:, h * 16:h * 16 + 8]
                v1 = CV[:, h * 16 + 8:h * 16 + 16]
                p0 = CP[:, h * 16:h * 16 + 8]
                p1 = CP[:, h * 16 + 8:h * 16 + 16]
                P.op("vector", lambda e, ch=ch, v0=v0: e.max(out=v0, in_=ch), reads=[chn], writes=[cvn])
                P.op("vector", lambda e, ch=ch, v0=v0, p0=p0: e.max_index(out=p0, in_max=v0, in_values=ch), reads=[chn, cvn], writes=[cpn])
                P.op("vector", lambda e, ch=ch, v0=v0, sc_=sc_: e.match_replace(out=sc_, in_to_replace=v0, in_values=ch, imm_value=-1e30),
                     reads=[chn, cvn], writes=[scn])
                P.op("vector", lambda e, v1=v1, sc_=sc_: e.max(out=v1, in_=sc_), reads=[scn], writes=[cvn])
                P.op("vector", lambda e, v1=v1, p1=p1, sc_=sc_: e.max_index(out=p1, in_max=v1, in_values=sc_), reads=[scn, cvn], writes=[cpn])
            CV3 = CV[:].rearrange("p (h k) -> p h k", h=8)
            ex3 = ex[:].rearrange("p (h k) -> p h k", h=8)
            g3 = gates[:].rearrange("p (h k) -> p h k", h=8)
            P.op("vector", lambda e: e.tensor_tensor(out=ex3, in0=CV3, in1=CV3[:, :, 0:1].to_broadcast([128, 8, 16]), op=ALU.subtract), reads=["CV"], writes=["ex"])
            P.op("scalar", lambda e: e.activation(out=ex[:, :], in_=ex[:, :], func=AF.Exp), reads=["ex"], writes=["ex"])
            P.op("vector", lambda e: e.tensor_reduce(out=Zs[:, 0:8], in_=ex3, axis=AX.X, op=ALU.add), reads=["ex"], writes=["Zs"])
            P.op("vector", lambda e: e.reciprocal(out=Zs[:, 0:8], in_=Zs[:, 0:8]), reads=["Zs"], writes=["Zs"])
            P.op("vector", lambda e: e.tensor_tensor(out=g3, in0=ex3, in1=Zs[:, 0:8].unsqueeze(2).to_broadcast([128, 8, 16]), op=ALU.mult),
                 reads=["ex", "Zs"], writes=[gatesr])
            P.op("vector", lambda e: e.tensor_single_scalar(out=au[:, :], in_=CP[:, :], scalar=4, op=ALU.logical_shift_right), reads=["CP"], writes=["au"])
            P.op("vector", lambda e: e.tensor_single_scalar(out=CP[:, :], in_=CP[:, :], scalar=15, op=ALU.bitwise_and), reads=["CP", "au"], writes=["CP"])
            P.op("vector", lambda e: e.tensor_copy(out=af[:, :], in_=au[:, :]), reads=["au"], writes=["af"])
            P.op("vector", lambda e: e.tensor_copy(out=bf[:, :], in_=CP[:, :]), reads=["CP"], writes=["bf"])
            P.op("vector", lambda e: e.tensor_copy(out=S2[:, :], in_=It[:, :]), reads=["It"], writes=["S2"])
            Itf4 = S2[:].rearrange("p (h s a) -> p h s a", h=8, s=2)
            oh4 = Ssb[:].rearrange("p (h k a) -> p h k a", h=4, k=16)
            io4 = iota16[:, :].unsqueeze(1).unsqueeze(1).to_broadcast([128, 4, 16, 16])
            for (srcf, s_, sres) in ((af, 0, "af"), (bf, 1, "bf")):
                s3 = srcf[:].rearrange("p (h k) -> p h k", h=8)
                for hh in range(2):
                    hs = slice(hh * 4, hh * 4 + 4)
                    P.op("vector", lambda e, s3=s3, hs=hs: e.tensor_tensor(out=oh4, in0=s3[:, hs, :].unsqueeze(3).to_broadcast([128, 4, 16, 16]), in1=io4, op=ALU.is_equal),
                         reads=[sres, "iota16"], writes=["Ssb"])
                    P.op("vector", lambda e, s_=s_, hs=hs: e.tensor_tensor(out=oh4, in0=oh4, in1=Itf4[:, hs, s_, :].unsqueeze(2).to_broadcast([128, 4, 16, 16]), op=ALU.mult),
                         reads=["Ssb", "S2"], writes=["Ssb"])
                    P.op("vector", lambda e, s3=s3, hs=hs: e.tensor_reduce(out=s3[:, hs, :], in_=oh4, axis=AX.X, op=ALU.add), reads=["Ssb"], writes=[sres])
            P.op("vector", lambda e: e.scalar_tensor_tensor(out=af[:, :], in0=af[:, :], scalar=128.0, in1=bf[:, :], op0=ALU.mult, op1=ALU.add),
                 reads=["af", "bf"], writes=["af"])
            P.op("vector", lambda e: e.tensor_copy(out=idx[:, :], in_=af[:, :]), reads=["af"], writes=[idxr])

        def experts(i, x_, orow, filler=None):
            xr = "xt%d" % x_
            h2 = xt[x_]
            rate = (sum(1 for e_, _ in filler if e_ != "tensor") // n_peer_j + 1) if filler else 0
            idx = idx2[i]
            gates = gates2[i]
            ssc = ss[:, 2 + i:3 + i]
            ssr2 = "ss2_%d" % i
            idxr = "idx%d" % i
            gatesr = "gates%d" % i

            def fill(n):
                while n > 0 and filler:
                    e_, th = filler.pop(0)
                    th()
                    if e_ != "tensor":
                        n -= 1
            nj = n_peer_j
            nb = nj // JB

            nj = n_peer_j
            def gather(j):
                s_ = j % NS
                P.dma("gpsimd", lambda e: e.indirect_dma_start(out=GS[s_][:, :], out_offset=None, in_=tab_b,
                                                               in_offset=bass.IndirectOffsetOnAxis(ap=idx[:, j:j + 1], axis=0)),
                      "g" + GSres[s_], reads=[idxr] + TBres, writes=[GSres[s_]])

            def dot(j):
                s_ = j % NS
                p_ = j % 2
                P.op("vector", lambda e: e.tensor_tensor(out=prod[p_][:, :], in0=GS[s_][:, 0:D], in1=hbf[i][:, :], op=ALU.mult),
                     reads=["hbf%d" % i], writes=["prod%d" % p_], weak=[GSres[s_]])
                P.op("scalar", lambda e: e.activation(out=prod[p_][:, :], in_=prod[p_][:, :], func=AF.Copy, accum_out=apre[:, j:j + 1]),
                     reads=["prod%d" % p_], writes=["prod%d" % p_, "apre%d" % j])

            def gelu(j):
                P.op("scalar", lambda e: e.activation(out=apre[:, j:j + 1], in_=apre[:, j:j + 1], func=AF.Gelu_apprx_tanh),
                     reads=["apre%d" % j], writes=["apre%d" % j])

            def acc(j):
                s_ = j % NS
                d_ = j % len(diag)
                P.op("vector", lambda e: e.scalar_tensor_tensor(out=diag[d_][:, :], in0=identb[:, :], scalar=apre[:, j:j + 1],
                                                                in1=gates[:, j:j + 1].to_broadcast([128, 128]), op0=ALU.mult, op1=ALU.mult),
                     reads=["identb", "apre%d" % j, gatesr], writes=["diag%d" % d_])
                for half in range(2):
                    P.op("tensor", lambda e, half=half: e.matmul(psA[:, half * 512:(half + 1) * 512], lhsT=diag[d_][:, :],
                                                                 rhs=GS[s_][:, D + half * 512:D + (half + 1) * 512], start=(j == 0), stop=(j == nj - 1)),
                         reads=["diag%d" % d_, GSres[s_]], writes=["b%d" % half])

            r1 = rate // 3
            r2 = (rate - r1) // 2
            r3 = rate - r1 - r2
            for t in range(nj + 3):
                if t < nj:
                    gather(t)
                fill(r3)
                if 0 <= t - 1 < nj:
                    dot(t - 1)
                fill(r2)
                if 0 <= t - 2 < nj:
                    gelu(t - 2)
                if 0 <= t - 3 < nj:
                    acc(t - 3)
                fill(r1)
            fill(100000)
            P.op("vector", lambda e: e.tensor_tensor(out=h2[:, :], in0=psA[:, 0:1024], in1=h2[:, :], op=ALU.add), reads=["b0", "b1", xr], writes=[xr])
            P.op("scalar", lambda e: e.activation(out=junk[:, :], in_=h2[:, :], func=AF.Square, accum_out=ss[:, 4 + i:5 + i]), reads=[xr], writes=["ss3_%d" % i] + JW)
            rstd_from_ss(ss[:, 4 + i:5 + i], 128, "ss3_%d" % i, 1.0 / D)
            P.op("vector", lambda e: e.scalar_tensor_tensor(out=h2[:, :], in0=h2[:, :], scalar=ss[:, 4 + i:5 + i], in1=gB[:, :], op0=ALU.mult, op1=ALU.mult),
                 reads=[xr, "ss3_%d" % i, "gB"], writes=[xr])
            P.dma("sync", lambda e: e.dma_start(out=out[orow:orow + 128, :], in_=h2[:, :]), "st%d" % x_, reads=[xr], writes=["out%d" % x_])

        mixer_pre()
        mixer_group(0)
        route(0, 0)
        for g in range(n_groups):
            x0 = 2 * (g % 2)
            P.defer = []
            route(1, x0 + 1)
            R1 = P.defer
            M, R0, ms = [], [], 0
            if g + 1 < n_groups:
                for i_ in range(2):
                    xs_ = 2 * ((g + 1) % 2) + i_
                    r0_ = HALO + (g + 1) * G + i_ * 128
                    P._dma("sync", lambda e, xs_=xs_, r0_=r0_: e.dma_start(out=xt[xs_][:, :], in_=xh[r0_:r0_ + 128, :]), "ldx%d" % xs_, writes=["xt%d" % xs_])
                P.defer = []
                P.mark = 0
                mixer_group(g + 1)
                M, ms = P.defer, P.mark
                P.defer = []
                route(0, 2 * ((g + 1) % 2))
                R0 = P.defer
            P.defer = None
            experts(0, x0, g * G, reorder(R1 + M[:ms]))
            experts(1, x0 + 1, g * G + 128, [(e_, th_) for (e_, th_, _r, _w) in M[ms:]] + reorder(R0))
        P.wait_all("sync", ["out0", "out1", "out2", "out3"])
        P.emit()
    return nc


def _prep_shared(inp):
    f = lambda a: np.ascontiguousarray(np.asarray(a, dtype=np.float32))
    col = lambda v, n: np.asarray(v, np.float32).reshape(n, 128).T
    vecs = np.zeros((128, NVEC), np.float32)
    vecs[:, GM:GM + 8] = col(inp["norm_mix_g"][0], 8)
    vecs[:, GF:GF + 8] = col(inp["norm_ffn_g"][0], 8)
    vecs[:, GO:GO + 4] = col(inp["out_norm_g_sc"][0], 4)
    vecs[:, GO + 4:GO + 8] = col(inp["out_norm_g_cf"][0], 4)
    scw = np.asarray(inp["sc_conv_w"][0], np.float32)
    cfw = np.asarray(inp["cf_conv_w"][0], np.float32)
    for k in range(4):
        vecs[:, SCW + 3 * k:SCW + 3 * k + 3] = scw[:, k * 128:(k + 1) * 128].T
        vecs[:, CFW + 31 * k:CFW + 31 * k + 31] = cfw[:, k * 128:(k + 1) * 128].T
    vecs[:, CFB:CFB + 4] = col(inp["cf_conv_b"][0], 4)
    vecs[:, LNG:LNG + 4] = col(inp["cf_ln_g"][0], 4)
    vecs[:, LNB:LNB + 4] = col(inp["cf_ln_b"][0], 4)
    gB = np.concatenate([np.broadcast_to(np.asarray(inp["norm_ffn_g"][0], np.float32)[None, :], (128, D)),
                         np.broadcast_to(np.asarray(inp["final_norm_g"], np.float32)[None, :], (128, D))], axis=1)
    sk = np.asarray(inp["peer_sub_keys"][0], np.float32)
    keysT = np.ascontiguousarray(sk.reshape(16, 128, 128).transpose(2, 0, 1).reshape(128, 2048))
    return {
        "w_in": f(inp["w_in"][0]), "w_out": f(inp["w_out"][0]), "w_q": f(inp["peer_w_q"][0]),
        "keysT": keysT, "uv_tab": np.ascontiguousarray(np.concatenate([f(inp["peer_u"][0]), f(inp["peer_v"][0])], axis=1)),
        "vecs_h": vecs, "gB_h": np.ascontiguousarray(gB),
    }


def _core_x(inp, core, n_groups=16):
    x = np.asarray(inp["x"], np.float32)
    meta = np.asarray(inp["meta_tokens"], np.float32)
    b, half = core // 2, core % 2
    ntok = n_groups * G
    if half == 0:
        halo = np.concatenate([np.zeros((HALO - meta.shape[0], D), np.float32), meta], axis=0)
        body = x[b, 0:ntok]
    else:
        halo = x[b, 4096 - HALO:4096]
        body = x[b, 4096:4096 + ntok]
    return np.ascontiguousarray(np.concatenate([halo, body], axis=0))


def kernel(**inputs):
    shared = _prep_shared(inputs)
    nc = build(16)
    in_maps = []
    for c in range(N_CORES):
        m = dict(shared)
        m["xh"] = _core_x(inputs, c)
        in_maps.append(m)
    res = run_bass_kernel_spmd(nc, in_maps, core_ids=list(range(N_CORES)))
    outp = np.empty((4, 8192, D), np.float32)
    for c in range(N_CORES):
        b, half = c // 2, c % 2
        outp[b, half * 4096:(half + 1) * 4096] = res.results[c]["out"]
    return outp
```

```python
import numpy as np
from contextlib import ExitStack
import concourse.bass as bass
import concourse.mybir as mybir
from concourse.bass_utils import run_bass_kernel_spmd

F32 = mybir.dt.float32
BF16 = mybir.dt.bfloat16
U32 = mybir.dt.uint32
ALU = mybir.AluOpType
AF = mybir.ActivationFunctionType
AX = mybir.AxisListType

D = 1024
G = 256
HALO = 32
N_CORES = 8
EPS = 1e-6
NS = 10
JB = 4
GM, GF, GO, SCW, CFW, CFB, LNG, LNB, NVEC = 0, 8, 16, 24, 36, 160, 164, 168, 172


class Prog:
    ENGS = ("sync", "scalar", "vector", "gpsimd", "tensor")

    def __init__(self, nc, stack):
        self.nc = nc
        self.stack = stack
        self.q = {e: [] for e in self.ENGS}
        self.cnt = {e: 0 for e in self.ENGS}
        self.sem = {e: stack.enter_context(nc.semaphore("c_" + e)) for e in self.ENGS}
        self.seen = {e: {} for e in self.ENGS}
        self.last_w = {}
        self.readers = {}
        self.dsem = {}
        self.dcnt = {}
        self.alias = {}
        self.defer = None

    def _exp(self, names):
        out = []
        for n in names:
            out.extend(self.alias.get(n, (n,)))
        return out

    def _deps(self, e, reads, writes):
        deps = {}

        def add(ev):
            if ev is None:
                return
            k, s, v = ev
            if k not in deps or deps[k][1] < v:
                deps[k] = (s, v)

        for r in reads:
            add(self.last_w.get(r))
        for w in writes:
            rd = self.readers.get(w, ())
            if not rd:
                add(self.last_w.get(w))
            for ev in rd:
                add(ev)
        for k, (s, v) in deps.items():
            if e == "tensor" and k == "e_tensor":
                continue
            if self.seen[e].get(k, 0) < v:
                self.seen[e][k] = v
                self.q[e].append(lambda eng, s=s, v=v: eng.wait_ge(s, v))

    def _commit(self, ev, reads, writes):
        for w in writes:
            self.last_w[w] = ev
            self.readers[w] = []
        for r in reads:
            if r not in writes:
                self.readers.setdefault(r, []).append(ev)

    def op(self, e, fn, reads=(), writes=(), weak=()):
        if self.defer is not None:
            self.defer.append((e, lambda: self._op(e, fn, reads, writes, weak),
                               set(self._exp(reads)) | set(self._exp(weak)), set(self._exp(writes))))
            return
        self._op(e, fn, reads, writes, weak)

    def _op(self, e, fn, reads=(), writes=(), weak=()):
        reads, writes, weak = self._exp(reads), self._exp(writes), self._exp(weak)
        self._deps(e, list(reads) + list(weak), writes)
        self.cnt[e] += 1
        n = self.cnt[e]
        s = self.sem[e]
        self.q[e].append(lambda eng, fn=fn, s=s: fn(eng).then_inc(s, 1))
        self._commit(("e_" + e, s, n), reads, writes)

    def dma(self, e, fn, key, reads=(), writes=()):
        if self.defer is not None:
            self.defer.append((e, lambda: self._dma(e, fn, key, reads, writes), set(self._exp(reads)), set(self._exp(writes))))
            return
        self._dma(e, fn, key, reads, writes)

    def _dma(self, e, fn, key, reads=(), writes=()):
        reads, writes = self._exp(reads), self._exp(writes)
        self._deps(e, reads, writes)
        if key not in self.dsem:
            self.dsem[key] = self.stack.enter_context(self.nc.semaphore("d_" + key))
            self.dcnt[key] = 0
        s = self.dsem[key]
        self.dcnt[key] += 16
        v = self.dcnt[key]
        self.q[e].append(lambda eng, fn=fn, s=s: fn(eng).then_inc(s, 16))
        self._commit(("d_" + key, s, v), reads, writes)

    def wait_all(self, e, resources):
        self._deps(e, self._exp(resources), ())

    def emit(self):
        with self.nc.Block() as block:
            @block.sync
            def _(eng):
                for f in self.q["sync"]:
                    f(eng)

            @block.scalar
            def _(eng):
                for f in self.q["scalar"]:
                    f(eng)

            @block.vector
            def _(eng):
                for f in self.q["vector"]:
                    f(eng)

            @block.gpsimd
            def _(eng):
                for f in self.q["gpsimd"]:
                    f(eng)

            @block.tensor
            def _(eng):
                for f in self.q["tensor"]:
                    f(eng)


PE_BANK = {"p0": "B2", "p1": "B2", "p2": "B3", "p3": "B3", "b0": "B0", "b1": "B1", "b4": "B4", "b5": "B5", "bT": "BT"}


def reorder(entries, dmin=2):
    units = []
    for (e, th, rd, wr) in entries:
        rd, wr = set(rd), set(wr)
        if e == "tensor":
            wr |= {"pe_" + PE_BANK[w] for w in wr if w in PE_BANK}
            if units and units[-1]["pe"]:
                u = units[-1]
                u["th"].append((e, th)); u["rd"] |= rd; u["wr"] |= wr
                continue
        units.append({"pe": e == "tensor", "th": [(e, th)], "rd": rd, "wr": wr})
    n = len(units)
    preds = [set() for _ in range(n)]
    succs = [set() for _ in range(n)]
    last_w, readers = {}, {}
    for i, u in enumerate(units):
        for r in u["rd"]:
            if r in last_w:
                preds[i].add(last_w[r])
        for w in u["wr"]:
            if w in last_w:
                preds[i].add(last_w[w])
            preds[i].update(readers.get(w, ()))
        for w in u["wr"]:
            last_w[w] = i
            readers[w] = []
        for r in u["rd"]:
            if r not in u["wr"]:
                readers.setdefault(r, []).append(i)
        preds[i].discard(i)
        for p in preds[i]:
            succs[p].add(i)
    cp = [1] * n
    for i in range(n - 1, -1, -1):
        for q in succs[i]:
            cp[i] = max(cp[i], 1 + cp[q])
    npred = [len(p) for p in preds]
    ready = [i for i in range(n) if npred[i] == 0]
    pos = {}
    out = []
    while ready:
        step = len(pos)

        def key(i):
            last = max((pos[p] for p in preds[i]), default=-10 ** 6)
            far = (step - last) >= dmin
            return (0 if far else 1, -cp[i] if far else last, i)

        b = min(ready, key=key)
        ready.remove(b)
        pos[b] = step
        out.extend(units[b]["th"])
        for q in succs[b]:
            npred[q] -= 1
            if npred[q] == 0:
                ready.append(q)
    assert len(pos) == n
    return out


def build(n_groups=16, n_peer_j=128):
    nc = bass.Bass("TRN2", target_bir_lowering=False)
    ntok = HALO + n_groups * G
    dr = lambda name, shape, kind="ExternalInput", dt=F32: nc.dram_tensor(name, shape, dt, kind=kind).ap()
    xh = dr("xh", [ntok, D])
    w_in = dr("w_in", [D, 2560])
    w_out = dr("w_out", [D, D])
    w_q = dr("w_q", [D, 2048])
    keysT = dr("keysT", [128, 2048])
    uv_tab = dr("uv_tab", [16384, 2 * D])
    tab_b = dr("tab_b", [16384, 2 * D], kind="Internal", dt=BF16)
    vecs_d = dr("vecs_h", [128, NVEC])
    gB_d = dr("gB_h", [128, 2048])
    out = dr("out", [n_groups * G, D], kind="ExternalOutput")

    with ExitStack() as st:
        P = Prog(nc, st)
        sb = lambda name, shape, dt=F32: st.enter_context(nc.sbuf_tensor(name, shape, dt))
        Wib = sb("Wib", [128, 8, 2560], BF16)
        Wob = sb("Wob", [128, 8, 1024], BF16)
        Wqb = sb("Wqb", [128, 8, 2048], BF16)
        keysb = sb("keysb", [128, 16, 128], BF16)
        vecs = sb("vecs", [128, NVEC])
        gB = sb("gB", [128, D])
        identb = sb("identb", [128, 128], BF16)
        onesf = sb("onesf", [128, 128])
        iota16 = sb("iota16", [128, 16])
        xt = [sb("xt%d" % i, [128, D]) for i in range(4)]
        hnb = sb("hnb", [128, D], BF16)
        ss = sb("ss", [128, 8])
        hnT = sb("hnT", [128, 8, G], BF16)
        cgs = sb("cgs", [128, G])
        zb = [sb("z%d" % k, [128, HALO + G]) for k in range(4)]
        mixA = sb("mixA", [128, 2048])
        ysc = [mixA[:, k * G:(k + 1) * G] for k in range(4)]
        sq = [sb("sq%d" % k, [128, G]) for k in range(2)]
        ub = [sb("u%d" % k, [128, HALO + G], BF16) for k in range(4)]
        dgs = [sb("dg%d" % k, [128, 128], BF16) for k in range(4)]
        cv = [mixA[:, 1024 + k * G:1024 + (k + 1) * G] for k in range(4)]
        mt = sb("mt", [128, G])
        rt = sb("rt", [128, G])
        qTb = sb("qTb", [128, 1024], BF16)
        Ssb = sb("Ssb", [128, 1024])
        S2 = sb("S2", [128, 256])
        Vt = sb("Vt", [128, 256])
        It = sb("It", [128, 256], U32)
        cand = mixA
        CV = sb("CV", [128, 128])
        CP = sb("CP", [128, 128], U32)
        au = sb("au", [128, 128], U32)
        af = sb("af", [128, 128])
        bf = sb("bf", [128, 128])
        idx2 = [sb("idx%d" % i, [128, 128], U32) for i in range(2)]
        ex = sb("ex", [128, 128])
        Zs = sb("Zs", [128, 8])
        gates2 = [sb("gates%d" % i, [128, 128]) for i in range(2)]
        apre = sb("apre", [128, 128])
        diag = [sb("diag%d" % i, [128, 128], BF16) for i in range(4)]
        GS = [sb("GS%d" % i, [128, 2 * D], BF16) for i in range(NS)]
        yn = [sb("yn%d" % k, [128, G], BF16) for k in range(8)]
        psA = st.enter_context(nc.psum_tensor("psA", [128, 6 * 512], F32))
        psT = st.enter_context(nc.psum_tensor("psT", [128, 1024], BF16))
        pslot = lambda s_, n: psA[:, 1024 + s_ * 256:1024 + s_ * 256 + n]
        bank = lambda i, n=512: psA[:, i * 512:i * 512 + n]
        psT3 = psT[:].rearrange("p (c t) -> p c t", c=8)
        prod = [sb("prod%d" % i, [128, D], BF16) for i in range(2)]
        hbf = [sb("hbf%d" % i, [128, D], BF16) for i in range(2)]
        junk = prod[1][:, :]
        JW = ["prod1"]
        JR = []

        vcol = lambda c: vecs[:, c:c + 1]
        epsc = ss[:, 7:8]
        P.op("vector", lambda e: e.memset(epsc, EPS), writes=["epsc"])
        P.alias["cand"] = ["ysc%d" % k for k in range(4)] + ["cv%d" % k for k in range(4)]
        P.alias["Vt"] = ["Vt%d" % k for k in range(16)]
        P.alias["It"] = ["It%d" % k for k in range(16)]
        P.alias["CV"] = ["CV%d" % k for k in range(8)]
        P.alias["CP"] = ["CP%d" % k for k in range(8)]
        P.alias["S2"] = ["S2_0", "S2_1"]
        P.alias["Ssb"] = ["Ssb0", "Ssb1", "Ssb2"]
        P.alias["mixlo"] = ["ysc%d" % k for k in range(4)]
        P.alias["mixhi"] = ["cv%d" % k for k in range(4)]
        P.alias["b2"] = ["p0", "p1"]
        P.alias["b3"] = ["p2", "p3"]

        P.dma("sync", lambda e: e.dma_start(out=vecs[:], in_=vecs_d), "ldv", writes=["vecs"])
        P.dma("sync", lambda e: e.dma_start(out=gB[:], in_=gB_d[:, D:2 * D]), "ldg", writes=["gB"])
        P.dma("sync", lambda e: e.dma_start(out=xt[0][:, :], in_=gB_d[:, 0:D]), "ldx0", writes=["xt0"])
        P.op("gpsimd", lambda e: e.memset(apre[:], 1.0), writes=["apre_setup"])
        P.op("gpsimd", lambda e: e.affine_select(out=apre[:], in_=apre[:], pattern=[[-1, 128]], compare_op=ALU.is_equal,
                                                  fill=0.0, base=0, channel_multiplier=1), reads=["apre_setup"], writes=["apre_setup"])
        P.op("vector", lambda e: e.tensor_copy(out=identb[:], in_=apre[:]), reads=["apre_setup"], writes=["identb"])
        P.op("gpsimd", lambda e: e.memset(onesf[:], 1.0), writes=["onesf"])
        P.op("gpsimd", lambda e: e.iota(iota16[:], pattern=[[1, 16]], base=0, channel_multiplier=0,
                                        allow_small_or_imprecise_dtypes=True), writes=["iota16"])
        stage = [(mixA[:, 0:1024], "mixlo"), (mixA[:, 1024:2048], "mixhi"), (xt[1][:, :], "xt1"), (xt[2][:, :], "xt2")]
        si = 0
        for (Wd, Wb, ncol, gcol, nm) in ((w_in, Wib, 2560, GM, "Wib"), (w_out, Wob, 1024, GO, "Wob"), (w_q, Wqb, 2048, GF, "Wqb")):
            for c in range(8):
                for c0 in range(0, ncol, 1024):
                    w = min(1024, ncol - c0)
                    sbuf_, rn = stage[si % 4]
                    si += 1
                    P.dma("sync" if si % 2 else "scalar",
                          lambda e, sbuf_=sbuf_, Wd=Wd, c=c, c0=c0, w=w: e.dma_start(out=sbuf_[:, 0:w], in_=Wd[c * 128:(c + 1) * 128, c0:c0 + w]),
                          "ld" + rn, writes=[rn])
                    if si % 2:
                        P.op("vector", lambda e, sbuf_=sbuf_, Wb=Wb, c=c, c0=c0, w=w, gcol=gcol: e.tensor_scalar(
                            out=Wb[:, c, c0:c0 + w], in0=sbuf_[:, 0:w], scalar1=vcol(gcol + c), scalar2=None, op0=ALU.mult),
                            reads=[rn, "vecs"], writes=[nm])
                    else:
                        P.op("scalar", lambda e, sbuf_=sbuf_, Wb=Wb, c=c, c0=c0, w=w, gcol=gcol: e.activation(
                            out=Wb[:, c, c0:c0 + w], in_=sbuf_[:, 0:w], func=AF.Copy, scale=vcol(gcol + c)),
                            reads=[rn, "vecs"], writes=[nm])
        for c0 in range(0, 2048, 1024):
            sbuf_, rn = stage[si % 4]
            si += 1
            P.dma("sync", lambda e, sbuf_=sbuf_, c0=c0: e.dma_start(out=sbuf_[:, :], in_=keysT[:, c0:c0 + 1024]), "ld" + rn, writes=[rn])
            P.op("vector", lambda e, sbuf_=sbuf_, c0=c0: e.tensor_copy(out=keysb[:].rearrange("p g n -> p (g n)")[:, c0:c0 + 1024], in_=sbuf_[:, :]),
                 reads=[rn], writes=["keysb"])
        GSres = ["GS%d" % i for i in range(NS)]
        TBres = ["tab_b%d" % i for i in range(NS)]
        ustage = [(mixA[:, 0:1024], "mixlo"), (xt[1][:, :], "xt1"), (xt[3][:, :], "xt3")]
        vstage = [(mixA[:, 1024:2048], "mixhi"), (xt[2][:, :], "xt2")]
        for it in range(128):
            su_, ru = ustage[it % 3]
            sv_, rv = vstage[it % 2]
            gs_ = it % NS
            P.dma("sync", lambda e, su_=su_, it=it: e.dma_start(out=su_[:, :], in_=uv_tab[it * 128:(it + 1) * 128, 0:D]), "ldu" + ru, writes=[ru])
            P.dma("sync", lambda e, sv_=sv_, it=it: e.dma_start(out=sv_[:, :], in_=uv_tab[it * 128:(it + 1) * 128, D:2 * D]), "ldv" + rv, writes=[rv])
            P.op("vector", lambda e, su_=su_, gs_=gs_: e.tensor_tensor(out=GS[gs_][:, 0:D], in0=su_[:, :], in1=xt[0][:, :], op=ALU.mult),
                 reads=[ru, "xt0"], writes=[GSres[gs_] + "u"])
            P.op("scalar", lambda e, sv_=sv_, gs_=gs_: e.activation(out=GS[gs_][:, D:2 * D], in_=sv_[:, :], func=AF.Copy), reads=[rv], writes=[GSres[gs_] + "v"])
            P.dma("scalar", lambda e, gs_=gs_, it=it: e.dma_start(out=tab_b[it * 128:(it + 1) * 128, :], in_=GS[gs_][:, :]),
                  "st" + GSres[gs_], reads=[GSres[gs_] + "u", GSres[gs_] + "v", GSres[gs_]], writes=[TBres[gs_]])

        def rstd_from_ss(ss_ap, n, res, scale):
            P.op("scalar", lambda e: e.activation(out=ss_ap, in_=ss_ap, func=AF.Sqrt, scale=scale, bias=epsc[0:n, 0:1]), reads=[res, "epsc"], writes=[res])
            P.op("vector", lambda e: e.reciprocal(out=ss_ap, in_=ss_ap), reads=[res], writes=[res])

        def bcast_rstd(src_bank, src_res, dst, dst_res, N, scale):
            P.op("scalar", lambda e: e.activation(out=dst[:, 0:N], in_=src_bank[:, 0:N], func=AF.Sqrt, scale=scale, bias=epsc[:, 0:1]),
                 reads=[src_res, "epsc"], writes=[dst_res])
            P.op("vector", lambda e: e.reciprocal(out=dst[:, 0:N], in_=dst[:, 0:N]), reads=[dst_res], writes=[dst_res])

        def front(tiles, N, xs, preloaded=False):
            off = 0
            for i, (r0, n) in enumerate(tiles):
                xr = "xt%d" % xs[i]
                xi = xt[xs[i]]
                if not preloaded:
                    P.dma("sync", lambda e, xi=xi, i=i, r0=r0, n=n: e.dma_start(out=xi[0:n, :], in_=xh[r0:r0 + n, :]), "ldx%d" % xs[i], writes=[xr])
                ssr = "ss%d" % i
                P.op("scalar", lambda e, xi=xi, i=i, n=n: e.activation(out=junk[0:n, :], in_=xi[0:n, :], func=AF.Square, accum_out=ss[0:n, i:i + 1]),
                     reads=[xr], writes=[ssr] + JW)
                rstd_from_ss(ss[0:n, i:i + 1], n, ssr, 1.0 / D)
                P.op("scalar", lambda e, xi=xi, i=i, n=n: e.activation(out=hnb[0:n, :], in_=xi[0:n, :], func=AF.Copy, scale=ss[0:n, i:i + 1]),
                     reads=[xr, ssr], writes=["hnb"])
                for c in range(8):
                    P.op("tensor", lambda e, c=c, n=n: e.transpose(out=psT3[:, c, 0:n], in_=hnb[0:n, c * 128:(c + 1) * 128], identity=identb[0:n, 0:n]),
                         reads=["hnb", "identb"], writes=["bT"])
                P.op("vector", lambda e, n=n, off=off: e.tensor_copy(out=hnT[:, :, off:off + n], in_=psT3[:, :, 0:n]), reads=["bT"], writes=["hnT"])
                off += n

        def proj(col, b, N):
            for k in range(8):
                P.op("tensor", lambda e, k=k, col=col, b=b, N=N: e.matmul(pslot(b, N), lhsT=Wib[:, k, col * 128:(col + 1) * 128], rhs=hnT[:, k, 0:N],
                                                                            start=(k == 0), stop=(k == 7)),
                     reads=["Wib", "hnT"], writes=["p%d" % b])

        def mixer_pre():
            N = HALO
            front([(0, HALO)], N, [0])
            for k in range(4):
                proj(k, 0, N)
                proj(8 + k, 1, N)
                P.op("scalar", lambda e: e.activation(out=cgs[:, 0:N], in_=pslot(1, N), func=AF.Copy), reads=["p1"], writes=["cgs"])
                P.op("vector", lambda e, k=k: e.tensor_tensor(out=zb[k][:, 0:N], in0=pslot(0, N), in1=cgs[:, 0:N], op=ALU.mult),
                     reads=["p0", "cgs"], writes=["z%d" % k])
                proj(12 + k, 0, N)
                proj(16 + k, 1, N)
                P.op("scalar", lambda e: e.activation(out=cgs[:, 0:N], in_=pslot(1, N), func=AF.Sigmoid), reads=["p1"], writes=["cgs"])
                P.op("vector", lambda e, k=k: e.tensor_tensor(out=ub[k][:, 0:N], in0=pslot(0, N), in1=cgs[:, 0:N], op=ALU.mult),
                     reads=["p0", "cgs"], writes=["u%d" % k])

        def mixer_group(g):
            N = G
            base = HALO + g * G
            tiles = [(base, 128), (base + 128, 128)]
            xs = [2 * (g % 2), 2 * (g % 2) + 1]
            front(tiles, N, xs, preloaded=(g > 0))
            for k in range(4):
                proj(k, 0, N)
                proj(8 + k, 1, N)
                proj(4 + k, 2, N)
                zr = "z%d" % k
                P.op("scalar", lambda e: e.activation(out=cgs[:, 0:N], in_=pslot(1, N), func=AF.Copy), reads=["p1"], writes=["cgs"])
                if g > 0:
                    P.op("vector", lambda e, k=k: e.tensor_copy(out=zb[k][:, 0:HALO], in_=zb[k][:, G:G + HALO]), reads=[zr], writes=[zr])
                P.op("vector", lambda e, k=k: e.tensor_tensor(out=zb[k][:, HALO:HALO + N], in0=pslot(0, N), in1=cgs[:, 0:N], op=ALU.mult),
                     reads=["p0", "cgs"], writes=[zr])
                P.op("vector", lambda e, k=k: e.tensor_scalar(out=rt[:, 0:N], in0=zb[k][:, HALO - 2:HALO - 2 + N], scalar1=vcol(SCW + 3 * k), scalar2=None, op0=ALU.mult),
                     reads=[zr, "vecs"], writes=["rt"])
                for j in (1, 2):
                    P.op("vector", lambda e, k=k, j=j: e.scalar_tensor_tensor(out=rt[:, 0:N], in0=zb[k][:, HALO - 2 + j:HALO - 2 + j + N], scalar=vcol(SCW + 3 * k + j),
                                                                              in1=rt[:, 0:N], op0=ALU.mult, op1=ALU.add),
                         reads=[zr, "vecs", "rt"], writes=["rt"])
                yr = "ysc%d" % k
                P.op("vector", lambda e, k=k: e.tensor_tensor(out=ysc[k][:, 0:N], in0=pslot(2, N), in1=rt[:, 0:N], op=ALU.mult),
                     reads=["p2", "rt"], writes=[yr])
                sr = "sq%d" % (k % 2)
                P.op("scalar", lambda e, k=k: e.activation(out=sq[k % 2][:, 0:N], in_=ysc[k][:, 0:N], func=AF.Square), reads=[yr], writes=[sr])
                P.op("tensor", lambda e, k=k: e.matmul(bank(4, N), lhsT=onesf[:], rhs=sq[k % 2][:, 0:N], start=(k == 0), stop=(k == 3)),
                     reads=[sr, "onesf"], writes=["b4"])
            bcast_rstd(bank(4), "b4", mt, "mt", N, 1.0 / 512)
            for k in range(4):
                P.op("vector", lambda e, k=k: e.tensor_tensor(out=yn[k][:, 0:N], in0=ysc[k][:, 0:N], in1=mt[:, 0:N], op=ALU.mult),
                     reads=["ysc%d" % k, "mt"], writes=["yn%d" % k])
            if P.defer is not None:
                P.mark = len(P.defer)
            for k in range(4):
                if k == 1 and P.defer is not None:
                    P.mark = len(P.defer)
                proj(12 + k, 0, N)
                proj(16 + k, 1, N)
                ur = "u%d" % k
                cr = "cv%d" % k
                P.op("scalar", lambda e: e.activation(out=cgs[:, 0:N], in_=pslot(1, N), func=AF.Sigmoid), reads=["p1"], writes=["cgs"])
                if g > 0:
                    P.op("vector", lambda e, k=k: e.tensor_copy(out=ub[k][:, 0:HALO], in_=ub[k][:, G:G + HALO]), reads=[ur], writes=[ur])
                P.op("vector", lambda e, k=k: e.tensor_tensor(out=ub[k][:, HALO:HALO + N], in0=pslot(0, N), in1=cgs[:, 0:N], op=ALU.mult),
                     reads=["p0", "cgs"], writes=[ur])
                for j in range(31):
                    dn = (k * 31 + j) % len(dgs)
                    dr_ = "dg%d" % dn
                    if j % 2:
                        P.op("vector", lambda e, k=k, j=j, dn=dn: e.tensor_tensor(out=dgs[dn][:, :], in0=identb[:, :],
                                                                                 in1=vcol(CFW + 31 * k + j).to_broadcast([128, 128]), op=ALU.mult),
                             reads=["identb", "vecs"], writes=[dr_])
                    else:
                        P.op("scalar", lambda e, k=k, j=j, dn=dn: e.activation(out=dgs[dn][:, :], in_=identb[:, :], func=AF.Copy, scale=vcol(CFW + 31 * k + j)),
                             reads=["identb", "vecs"], writes=[dr_])
                    P.op("tensor", lambda e, k=k, j=j, dn=dn: e.matmul(pslot(3, N), lhsT=dgs[dn][:, :], rhs=ub[k][:, 2 + j:2 + j + N], start=(j == 0), stop=(j == 30)),
                         reads=[dr_, ur], writes=["p3"])
                P.op("vector", lambda e, k=k: e.tensor_scalar(out=cv[k][:, 0:N], in0=pslot(3, N), scalar1=vcol(CFB + k), scalar2=None, op0=ALU.add),
                     reads=["p3", "vecs"], writes=[cr])
                sr = "sq%d" % (k % 2)
                P.op("scalar", lambda e, k=k: e.activation(out=sq[k % 2][:, 0:N], in_=cv[k][:, 0:N], func=AF.Square), reads=[cr], writes=[sr])
                P.op("tensor", lambda e, k=k: e.matmul(bank(4, N), lhsT=onesf[:], rhs=cv[k][:, 0:N], start=(k == 0), stop=(k == 3)),
                     reads=[cr, "onesf"], writes=["b4"])
                P.op("tensor", lambda e, k=k: e.matmul(bank(5, N), lhsT=onesf[:], rhs=sq[k % 2][:, 0:N], start=(k == 0), stop=(k == 3)),
                     reads=[sr, "onesf"], writes=["b5"])
            P.op("vector", lambda e: e.tensor_scalar(out=mt[:, 0:N], in0=bank(4, N), scalar1=1.0 / 512, scalar2=None, op0=ALU.mult), reads=["b4"], writes=["mt"])
            P.op("vector", lambda e: e.tensor_tensor(out=sq[0][:, 0:N], in0=mt[:, 0:N], in1=mt[:, 0:N], op=ALU.mult), reads=["mt"], writes=["sq0"])
            P.op("vector", lambda e: e.scalar_tensor_tensor(out=rt[:, 0:N], in0=bank(5, N), scalar=1.0 / 512, in1=sq[0][:, 0:N], op0=ALU.mult, op1=ALU.subtract),
                 reads=["b5", "sq0"], writes=["rt"])
            P.op("vector", lambda e: e.tensor_scalar(out=rt[:, 0:N], in0=rt[:, 0:N], scalar1=EPS, scalar2=None, op0=ALU.add), reads=["rt"], writes=["rt"])
            P.op("scalar", lambda e: e.activation(out=rt[:, 0:N], in_=rt[:, 0:N], func=AF.Sqrt), reads=["rt"], writes=["rt"])
            P.op("vector", lambda e: e.reciprocal(out=rt[:, 0:N], in_=rt[:, 0:N]), reads=["rt"], writes=["rt"])
            for k in range(4):
                cr = "cv%d" % k
                P.op("vector", lambda e, k=k: e.tensor_tensor(out=cv[k][:, 0:N], in0=cv[k][:, 0:N], in1=mt[:, 0:N], op=ALU.subtract), reads=[cr, "mt"], writes=[cr])
                P.op("vector", lambda e, k=k: e.tensor_tensor(out=cv[k][:, 0:N], in0=cv[k][:, 0:N], in1=rt[:, 0:N], op=ALU.mult), reads=[cr, "rt"], writes=[cr])
                P.op("scalar", lambda e, k=k: e.activation(out=cv[k][:, 0:N], in_=cv[k][:, 0:N], func=AF.Silu, scale=vcol(LNG + k), bias=vcol(LNB + k)),
                     reads=[cr, "vecs"], writes=[cr])
                sr = "sq%d" % (k % 2)
                P.op("scalar", lambda e, k=k: e.activation(out=sq[k % 2][:, 0:N], in_=cv[k][:, 0:N], func=AF.Square), reads=[cr], writes=[sr])
                P.op("tensor", lambda e, k=k: e.matmul(bank(4, N), lhsT=onesf[:], rhs=sq[k % 2][:, 0:N], start=(k == 0), stop=(k == 3)),
                     reads=[sr, "onesf"], writes=["b4"])
            bcast_rstd(bank(4), "b4", mt, "mt", N, 1.0 / 512)
            for k in range(4):
                P.op("vector", lambda e, k=k: e.tensor_tensor(out=yn[4 + k][:, 0:N], in0=cv[k][:, 0:N], in1=mt[:, 0:N], op=ALU.mult),
                     reads=["cv%d" % k, "mt"], writes=["yn%d" % (4 + k)])
            for i in range(2):
                for half in range(2):
                    for kk in range(8):
                        P.op("tensor", lambda e, i=i, half=half, kk=kk: e.matmul(psA[:, (2 + half) * 512:(3 + half) * 512], lhsT=yn[kk][:, i * 128:(i + 1) * 128],
                                                                                  rhs=Wob[:, kk, half * 512:(half + 1) * 512], start=(kk == 0), stop=(kk == 7)),
                             reads=["yn%d" % kk, "Wob"], writes=["b%d" % (2 + half)])
                P.op("vector", lambda e, i=i: e.tensor_tensor(out=xt[xs[i]][:, :], in0=psA[:, 2 * 512:4 * 512], in1=xt[xs[i]][:, :], op=ALU.add),
                     reads=["b2", "b3", "xt%d" % xs[i]], writes=["xt%d" % xs[i]])

        def route(i, x_):
            xr = "xt%d" % x_
            h2 = xt[x_]
            idx = idx2[i]
            gates = gates2[i]
            ssc = ss[:, 2 + i:3 + i]
            ssr2 = "ss2_%d" % i
            idxr = "idx%d" % i
            gatesr = "gates%d" % i
            P.op("scalar", lambda e: e.activation(out=junk[:, :], in_=h2[:, :], func=AF.Square, accum_out=ssc), reads=[xr], writes=[ssr2] + JW)
            rstd_from_ss(ssc, 128, ssr2, 1.0 / D)
            P.op("scalar", lambda e: e.activation(out=hbf[i][:, :], in_=h2[:, :], func=AF.Copy, scale=ssc), reads=[xr, ssr2], writes=["hbf%d" % i])
            for c in range(8):
                P.op("tensor", lambda e, c=c: e.transpose(out=psT3[:, c, :], in_=hbf[i][:, c * 128:(c + 1) * 128], identity=identb[:, :]),
                     reads=["hbf%d" % i, "identb"], writes=["bT"])
            P.op("vector", lambda e: e.tensor_copy(out=hnT[:, :, 0:128], in_=psT3[:, :, :]), reads=["bT"], writes=["hnT"])
            for hf in range(2):
                for gg in range(8):
                    g = hf * 8 + gg
                    for k in range(8):
                        P.op("tensor", lambda e, g=g, gg=gg, k=k: e.matmul(psA[:, 2048 + gg * 128:2048 + (gg + 1) * 128], lhsT=Wqb[:, k, g * 128:(g + 1) * 128],
                                                                        rhs=hnT[:, k, 0:128], start=(k == 0), stop=(k == 7)),
                             reads=["Wqb", "hnT"], writes=["b%d" % (4 + gg // 4)])
                P.op("scalar", lambda e, hf=hf: e.activation(out=qTb[:, :], in_=psA[:, 2048:3072], func=AF.Copy), reads=["b4", "b5"], writes=["qTb"])
                for gg in range(8):
                    g = hf * 8 + gg
                    P.op("tensor", lambda e, g=g, gg=gg: e.matmul(psA[:, 2048 + gg * 128:2048 + (gg + 1) * 128], lhsT=qTb[:, gg * 128:(gg + 1) * 128], rhs=keysb[:, g, :],
                                                               start=True, stop=True),
                         reads=["qTb", "keysb"], writes=["b%d" % (4 + gg // 4)])
                P.op("scalar", lambda e: e.activation(out=Ssb[:, :], in_=psA[:, 2048:3072], func=AF.Copy), reads=["b4", "b5"], writes=["Ssb"])
                for gg in range(8):
                    g = hf * 8 + gg
                    sg_ = Ssb[:, gg * 128:(gg + 1) * 128]
                    ssn = "Ssb%d" % (0 if gg < 2 else 1 if gg < 4 else 2)
                    sc_ = S2[:, (gg % 2) * 128:(gg % 2) * 128 + 128]
                    scn = "S2_%d" % (gg % 2)
                    vn, inn = "Vt%d" % g, "It%d" % g
                    v0 = Vt[:, g * 16:g * 16 + 8]
                    v1 = Vt[:, g * 16 + 8:g * 16 + 16]
                    i0 = It[:, g * 16:g * 16 + 8]
                    i1 = It[:, g * 16 + 8:g * 16 + 16]
                    P.op("vector", lambda e, sg_=sg_, v0=v0: e.max(out=v0, in_=sg_), reads=[ssn], writes=[vn])
                    P.op("vector", lambda e, sg_=sg_, v0=v0, i0=i0: e.max_index(out=i0, in_max=v0, in_values=sg_), reads=[ssn, vn], writes=[inn])
                    P.op("vector", lambda e, sg_=sg_, v0=v0, sc_=sc_: e.match_replace(out=sc_, in_to_replace=v0, in_values=sg_, imm_value=-1e30),
                         reads=[ssn, vn], writes=[scn])
                    P.op("vector", lambda e, v1=v1, sc_=sc_: e.max(out=v1, in_=sc_), reads=[scn], writes=[vn])
                    P.op("vector", lambda e, v1=v1, i1=i1, sc_=sc_: e.max_index(out=i1, in_max=v1, in_values=sc_), reads=[scn, vn], writes=[inn])
            Vt4 = Vt[:].rearrange("p (h s a) -> p h s a", h=8, s=2)
            cand4 = cand[:].rearrange("p (h a b) -> p h a b", h=8, a=16)
            P.op("vector", lambda e: e.tensor_tensor(out=cand4, in0=Vt4[:, :, 0, :].unsqueeze(3).to_broadcast([128, 8, 16, 16]),
                                                      in1=Vt4[:, :, 1, :].unsqueeze(2).to_broadcast([128, 8, 16, 16]), op=ALU.add),
                 reads=["Vt"], writes=["cand"])
            for h in range(8):
                ch = cand[:, h * 256:(h + 1) * 256]
                chn = ("ysc%d" % h) if h < 4 else ("cv%d" % (h - 4))
                sc_ = Ssb[:, (h % 2) * 256:(h % 2) * 256 + 256]
                scn = "Ssb%d" % (h % 2)
                cvn, cpn = "CV%d" % h, "CP%d" % h
                v0 = CV[:, h * 16:h * 16 + 8]
                v1 = CV[:, h * 16 + 8:h * 16 + 16]
                p0 = CP[:, h * 16:h * 16 + 8]
                p1 = CP[:, h * 16 + 8:h * 16 + 16]
                P.op("vector", lambda e, ch=ch, v0=v0: e.max(out=v0, in_=ch), reads=[chn], writes=[cvn])
                P.op("vector", lambda e, ch=ch, v0=v0, p0=p0: e.max_index(out=p0, in_max=v0, in_values=ch), reads=[chn, cvn], writes=[cpn])
                P.op("vector", lambda e, ch=ch, v0=v0, sc_=sc_: e.match_replace(out=sc_, in_to_replace=v0, in_values=ch, imm_value=-1e30),
                     reads=[chn, cvn], writes=[scn])
                P.op("vector", lambda e, v1=v1, sc_=sc_: e.max(out=v1, in_=sc_), reads=[scn], writes=[cvn])
                P.op("vector", lambda e, v1=v1, p1=p1, sc_=sc_: e.max_index(out=p1, in_max=v1, in_values=sc_), reads=[scn, cvn], writes=[cpn])
            CV3 = CV[:].rearrange("p (h k) -> p h k", h=8)
            ex3 = ex[:].rearrange("p (h k) -> p h k", h=8)
            g3 = gates[:].rearrange("p (h k) -> p h k", h=8)
            P.op("vector", lambda e: e.tensor_tensor(out=ex3, in0=CV3, in1=CV3[:, :, 0:1].to_broadcast([128, 8, 16]), op=ALU.subtract), reads=["CV"], writes=["ex"])
            P.op("scalar", lambda e: e.activation(out=ex[:, :], in_=ex[:, :], func=AF.Exp), reads=["ex"], writes=["ex"])
            P.op("vector", lambda e: e.tensor_reduce(out=Zs[:, 0:8], in_=ex3, axis=AX.X, op=ALU.add), reads=["ex"], writes=["Zs"])
            P.op("vector", lambda e: e.reciprocal(out=Zs[:, 0:8], in_=Zs[:, 0:8]), reads=["Zs"], writes=["Zs"])
            P.op("vector", lambda e: e.tensor_tensor(out=g3, in0=ex3, in1=Zs[:, 0:8].unsqueeze(2).to_broadcast([128, 8, 16]), op=ALU.mult),
                 reads=["ex", "Zs"], writes=[gatesr])
            P.op("vector", lambda e: e.tensor_single_scalar(out=au[:, :], in_=CP[:, :], scalar=4, op=ALU.logical_shift_right), reads=["CP"], writes=["au"])
            P.op("vector", lambda e: e.tensor_single_scalar(out=CP[:, :], in_=CP[:, :], scalar=15, op=ALU.bitwise_and), reads=["CP", "au"], writes=["CP"])
            P.op("vector", lambda e: e.tensor_copy(out=af[:, :], in_=au[:, :]), reads=["au"], writes=["af"])
            P.op("vector", lambda e: e.tensor_copy(out=bf[:, :], in_=CP[:, :]), reads=["CP"], writes=["bf"])
            P.op("vector", lambda e: e.tensor_copy(out=S2[:, :], in_=It[:, :]), reads=["It"], writes=["S2"])
            Itf4 = S2[:].rearrange("p (h s a) -> p h s a", h=8, s=2)
            oh4 = Ssb[:].rearrange("p (h k a) -> p h k a", h=4, k=16)
            io4 = iota16[:, :].unsqueeze(1).unsqueeze(1).to_broadcast([128, 4, 16, 16])
            for (srcf, s_, sres) in ((af, 0, "af"), (bf, 1, "bf")):
                s3 = srcf[:].rearrange("p (h k) -> p h k", h=8)
                for hh in range(2):
                    hs = slice(hh * 4, hh * 4 + 4)
                    P.op("vector", lambda e, s3=s3, hs=hs: e.tensor_tensor(out=oh4, in0=s3[:, hs, :].unsqueeze(3).to_broadcast([128, 4, 16, 16]), in1=io4, op=ALU.is_equal),
                         reads=[sres, "iota16"], writes=["Ssb"])
                    P.op("vector", lambda e, s_=s_, hs=hs: e.tensor_tensor(out=oh4, in0=oh4, in1=Itf4[:, hs, s_, :].unsqueeze(2).to_broadcast([128, 4, 16, 16]), op=ALU.mult),
                         reads=["Ssb", "S2"], writes=["Ssb"])
                    P.op("vector", lambda e, s3=s3, hs=hs: e.tensor_reduce(out=s3[:, hs, :], in_=oh4, axis=AX.X, op=ALU.add), reads=["Ssb"], writes=[sres])
            P.op("vector", lambda e: e.scalar_tensor_tensor(out=af[:, :], in0=af[:, :], scalar=128.0, in1=bf[:, :], op0=ALU.mult, op1=ALU.add),
                 reads=["af", "bf"], writes=["af"])
            P.op("vector", lambda e: e.tensor_copy(out=idx[:, :], in_=af[:, :]), reads=["af"], writes=[idxr])

        def experts(i, x_, orow, filler=None):
            xr = "xt%d" % x_
            h2 = xt[x_]
            rate = (sum(1 for e_, _ in filler if e_ != "tensor") // n_peer_j + 1) if filler else 0
            idx = idx2[i]
            gates = gates2[i]
            ssc = ss[:, 2 + i:3 + i]
            ssr2 = "ss2_%d" % i
            idxr = "idx%d" % i
            gatesr = "gates%d" % i

            def fill(n):
                while n > 0 and filler:
                    e_, th = filler.pop(0)
                    th()
                    if e_ != "tensor":
                        n -= 1
            nj = n_peer_j
            nb = nj // JB

            nj = n_peer_j
            def gather(j):
                s_ = j % NS
                P.dma("gpsimd", lambda e: e.indirect_dma_start(out=GS[s_][:, :], out_offset=None, in_=tab_b,
                                                               in_offset=bass.IndirectOffsetOnAxis(ap=idx[:, j:j + 1], axis=0)),
                      "g" + GSres[s_], reads=[idxr] + TBres, writes=[GSres[s_]])

            def dot(j):
                s_ = j % NS
                p_ = j % 2
                P.op("vector", lambda e: e.tensor_tensor(out=prod[p_][:, :], in0=GS[s_][:, 0:D], in1=hbf[i][:, :], op=ALU.mult),
                     reads=["hbf%d" % i], writes=["prod%d" % p_], weak=[GSres[s_]])
                P.op("scalar", lambda e: e.activation(out=prod[p_][:, :], in_=prod[p_][:, :], func=AF.Copy, accum_out=apre[:, j:j + 1]),
                     reads=["prod%d" % p_], writes=["prod%d" % p_, "apre%d" % j])

            def gelu(j):
                P.op("scalar", lambda e: e.activation(out=apre[:, j:j + 1], in_=apre[:, j:j + 1], func=AF.Gelu_apprx_tanh),
                     reads=["apre%d" % j], writes=["apre%d" % j])

            def acc(j):
                s_ = j % NS
                d_ = j % len(diag)
                P.op("vector", lambda e: e.scalar_tensor_tensor(out=diag[d_][:, :], in0=identb[:, :], scalar=apre[:, j:j + 1],
                                                                in1=gates[:, j:j + 1].to_broadcast([128, 128]), op0=ALU.mult, op1=ALU.mult),
                     reads=["identb", "apre%d" % j, gatesr], writes=["diag%d" % d_])
                for half in range(2):
                    P.op("tensor", lambda e, half=half: e.matmul(psA[:, half * 512:(half + 1) * 512], lhsT=diag[d_][:, :],
                                                                 rhs=GS[s_][:, D + half * 512:D + (half + 1) * 512], start=(j == 0), stop=(j == nj - 1)),
                         reads=["diag%d" % d_, GSres[s_]], writes=["b%d" % half])

            r1 = rate // 3
            r2 = (rate - r1) // 2
            r3 = rate - r1 - r2
            for t in range(nj + 3):
                if t < nj:
                    gather(t)
                fill(r3)
                if 0 <= t - 1 < nj:
                    dot(t - 1)
                fill(r2)
                if 0 <= t - 2 < nj:
                    gelu(t - 2)
                if 0 <= t - 3 < nj:
                    acc(t - 3)
                fill(r1)
            fill(100000)
            P.op("vector", lambda e: e.tensor_tensor(out=h2[:, :], in0=psA[:, 0:1024], in1=h2[:, :], op=ALU.add), reads=["b0", "b1", xr], writes=[xr])
            P.op("scalar", lambda e: e.activation(out=junk[:, :], in_=h2[:, :], func=AF.Square, accum_out=ss[:, 4 + i:5 + i]), reads=[xr], writes=["ss3_%d" % i] + JW)
            rstd_from_ss(ss[:, 4 + i:5 + i], 128, "ss3_%d" % i, 1.0 / D)
            P.op("vector", lambda e: e.scalar_tensor_tensor(out=h2[:, :], in0=h2[:, :], scalar=ss[:, 4 + i:5 + i], in1=gB[:, :], op0=ALU.mult, op1=ALU.mult),
                 reads=[xr, "ss3_%d" % i, "gB"], writes=[xr])
            P.dma("sync", lambda e: e.dma_start(out=out[orow:orow + 128, :], in_=h2[:, :]), "st%d" % x_, reads=[xr], writes=["out%d" % x_])

        mixer_pre()
        mixer_group(0)
        route(0, 0)
        for g in range(n_groups):
            x0 = 2 * (g % 2)
            P.defer = []
            route(1, x0 + 1)
            R1 = P.defer
            M, R0, ms = [], [], 0
            if g + 1 < n_groups:
                for i_ in range(2):
                    xs_ = 2 * ((g + 1) % 2) + i_
                    r0_ = HALO + (g + 1) * G + i_ * 128
                    P._dma("sync", lambda e, xs_=xs_, r0_=r0_: e.dma_start(out=xt[xs_][:, :], in_=xh[r0_:r0_ + 128, :]), "ldx%d" % xs_, writes=["xt%d" % xs_])
                P.defer = []
                P.mark = 0
                mixer_group(g + 1)
                M, ms = P.defer, P.mark
                P.defer = []
                route(0, 2 * ((g + 1) % 2))
                R0 = P.defer
            P.defer = None
            experts(0, x0, g * G, reorder(R1 + M[:ms]))
            experts(1, x0 + 1, g * G + 128, [(e_, th_) for (e_, th_, _r, _w) in M[ms:]] + reorder(R0))
        P.wait_all("sync", ["out0", "out1", "out2", "out3"])
        P.emit()
    return nc


def _prep_shared(inp):
    f = lambda a: np.ascontiguousarray(np.asarray(a, dtype=np.float32))
    col = lambda v, n: np.asarray(v, np.float32).reshape(n, 128).T
    vecs = np.zeros((128, NVEC), np.float32)
    vecs[:, GM:GM + 8] = col(inp["norm_mix_g"][0], 8)
    vecs[:, GF:GF + 8] = col(inp["norm_ffn_g"][0], 8)
    vecs[:, GO:GO + 4] = col(inp["out_norm_g_sc"][0], 4)
    vecs[:, GO + 4:GO + 8] = col(inp["out_norm_g_cf"][0], 4)
    scw = np.asarray(inp["sc_conv_w"][0], np.float32)
    cfw = np.asarray(inp["cf_conv_w"][0], np.float32)
    for k in range(4):
        vecs[:, SCW + 3 * k:SCW + 3 * k + 3] = scw[:, k * 128:(k + 1) * 128].T
        vecs[:, CFW + 31 * k:CFW + 31 * k + 31] = cfw[:, k * 128:(k + 1) * 128].T
    vecs[:, CFB:CFB + 4] = col(inp["cf_conv_b"][0], 4)
    vecs[:, LNG:LNG + 4] = col(inp["cf_ln_g"][0], 4)
    vecs[:, LNB:LNB + 4] = col(inp["cf_ln_b"][0], 4)
    gB = np.concatenate([np.broadcast_to(np.asarray(inp["norm_ffn_g"][0], np.float32)[None, :], (128, D)),
                         np.broadcast_to(np.asarray(inp["final_norm_g"], np.float32)[None, :], (128, D))], axis=1)
    sk = np.asarray(inp["peer_sub_keys"][0], np.float32)
    keysT = np.ascontiguousarray(sk.reshape(16, 128, 128).transpose(2, 0, 1).reshape(128, 2048))
    return {
        "w_in": f(inp["w_in"][0]), "w_out": f(inp["w_out"][0]), "w_q": f(inp["peer_w_q"][0]),
        "keysT": keysT, "uv_tab": np.ascontiguousarray(np.concatenate([f(inp["peer_u"][0]), f(inp["peer_v"][0])], axis=1)),
        "vecs_h": vecs, "gB_h": np.ascontiguousarray(gB),
    }


def _core_x(inp, core, n_groups=16):
    x = np.asarray(inp["x"], np.float32)
    meta = np.asarray(inp["meta_tokens"], np.float32)
    b, half = core // 2, core % 2
    ntok = n_groups * G
    if half == 0:
        halo = np.concatenate([np.zeros((HALO - meta.shape[0], D), np.float32), meta], axis=0)
        body = x[b, 0:ntok]
    else:
        halo = x[b, 4096 - HALO:4096]
        body = x[b, 4096:4096 + ntok]
    return np.ascontiguousarray(np.concatenate([halo, body], axis=0))


def kernel(**inputs):
    shared = _prep_shared(inputs)
    nc = build(16)
    in_maps = []
    for c in range(N_CORES):
        m = dict(shared)
        m["xh"] = _core_x(inputs, c)
        in_maps.append(m)
    res = run_bass_kernel_spmd(nc, in_maps, core_ids=list(range(N_CORES)))
    outp = np.empty((4, 8192, D), np.float32)
    for c in range(N_CORES):
        b, half = c // 2, c % 2
        outp[b, half * 4096:(half + 1) * 4096] = res.results[c]["out"]
    return outp
```
